# Optimizing a Trainium2 kernel written in Bass

```python
import math
import jax
import jax.numpy as jnp
from jax import lax
import numpy as np

D_MODEL = 1024
BATCH = 4
SEQ = 4096
DEPTH = 2

GRID_W = 64
CTX_LEN = 256
N_MIXERS = 2

MLA_HEADS = 8
Q_LORA = 384
KV_LORA = 256
QK_NOPE = 128
QK_ROPE = 64
QK_DIM = QK_NOPE + QK_ROPE
V_DIM = 128
ROPE_PAIRS = QK_ROPE // 4
ROPE_THETA = 10000.0
Q_BLOCK = 128

GROUP_SIZE = 16
N_GROUPS = D_MODEL // GROUP_SIZE
STATE = 64
STEP_MIN = 0.001
STEP_MAX = 0.1

D_FF = 2816
N_EXPERTS = 8
TOP_K = 2
D_FF_EXPERT = 3584

N_EVEN = (DEPTH + 1) // 2
N_ODD = DEPTH // 2
N_MOD = 6
DEEPNORM_ALPHA = (2 * DEPTH) ** 0.25
DEEPNORM_BETA = (8 * DEPTH) ** -0.25
EPS = 1e-6

kernel_name = "hybrid_mla_s5_moe_diffusion_block"


def rms_norm(t, g):
    tf = t.astype(jnp.float32)
    tf = tf * lax.rsqrt(jnp.mean(tf * tf, axis=-1, keepdims=True) + EPS)
    return tf.astype(t.dtype) * g


def layer_norm(t, g, b):
    tf = t.astype(jnp.float32)
    mu = jnp.mean(tf, axis=-1, keepdims=True)
    var = jnp.mean(jnp.square(tf - mu), axis=-1, keepdims=True)
    return ((tf - mu) * lax.rsqrt(var + EPS)).astype(t.dtype) * g + b


def modulate(t, shift, scale):
    return t * (1 + scale) + shift


def axial_rope_tables(rows):
    row = jnp.repeat(jnp.arange(rows, dtype=jnp.float32), GRID_W)
    col = jnp.tile(jnp.arange(GRID_W, dtype=jnp.float32), rows)
    freqs = ROPE_THETA ** (-jnp.arange(ROPE_PAIRS, dtype=jnp.float32) / ROPE_PAIRS)
    ang = jnp.stack([row[:, None] * freqs, col[:, None] * freqs], axis=1)
    return jnp.cos(ang), jnp.sin(ang)


def axial_rope(t, cos, sin):
    shp = t.shape
    t = t.reshape(shp[:-1] + (2, 2, ROPE_PAIRS))
    t1, t2 = t[..., 0, :], t[..., 1, :]
    cos = cos.astype(t.dtype)
    sin = sin.astype(t.dtype)
    out = jnp.stack([t1 * cos - t2 * sin, t1 * sin + t2 * cos], axis=-2)
    return out.reshape(shp)


def mla_queries(q_feat, g_q, w_uq, cos, sin):
    cq = rms_norm(q_feat, g_q)
    q = jnp.einsum("btr,rhd->bthd", cq, w_uq)
    if cos is not None:
        q = jnp.concatenate([q[..., :QK_NOPE], axial_rope(q[..., QK_NOPE:], cos[:, None], sin[:, None])], axis=-1)
    return q


def mla_keys_values(kv_feat, g_kv, w_uk, w_uv, cos, sin):
    ckv = rms_norm(kv_feat[..., :KV_LORA], g_kv)
    k_rope = kv_feat[..., KV_LORA:]
    if cos is not None:
        k_rope = axial_rope(k_rope, cos, sin)
    k_nope = jnp.einsum("btr,rhd->bthd", ckv, w_uk)
    v = jnp.einsum("btr,rhd->bthd", ckv, w_uv)
    k_rope = jnp.broadcast_to(k_rope[:, :, None, :], k_rope.shape[:2] + (MLA_HEADS, QK_ROPE))
    return jnp.concatenate([k_nope, k_rope], axis=-1), v


def attend(q, k, v):
    s = jnp.einsum("bqhd,bkhd->bhqk", q, k).astype(jnp.float32) * (QK_DIM ** -0.5)
    p = jax.nn.softmax(s, axis=-1).astype(v.dtype)
    return jnp.einsum("bhqk,bkhd->bqhd", p, v)


def mla_mixer(u, uc, cos, sin, w_down, g_q, g_kv, w_uq, w_uk, w_uv, w_o, need_ctx):
    bsz, n_lat, _ = u.shape
    n_ctx = uc.shape[1]
    down = u @ w_down
    q_lat = mla_queries(down[..., :Q_LORA], g_q, w_uq, cos, sin)
    k_lat, v_lat = mla_keys_values(down[..., Q_LORA:], g_kv, w_uk, w_uv, cos, sin)
    if need_ctx:
        down_c = uc @ w_down
        kv_feat_c = down_c[..., Q_LORA:]
    else:
        kv_feat_c = uc @ w_down[:, Q_LORA:]
    k_ctx, v_ctx = mla_keys_values(kv_feat_c, g_kv, w_uk, w_uv, None, None)
    k_all = jnp.concatenate([k_ctx, k_lat], axis=1)
    v_all = jnp.concatenate([v_ctx, v_lat], axis=1)
    n_blocks = n_lat // Q_BLOCK
    q_blocks = q_lat.reshape(bsz, n_blocks, Q_BLOCK, MLA_HEADS, QK_DIM).transpose(1, 0, 2, 3, 4)
    o = lax.map(lambda qb: attend(qb, k_all, v_all), q_blocks)
    o = o.transpose(1, 0, 2, 3, 4).reshape(bsz, n_lat, MLA_HEADS * V_DIM)
    y = o @ w_o
    if need_ctx:
        q_ctx = mla_queries(down_c[..., :Q_LORA], g_q, w_uq, None, None)
        yc = attend(q_ctx, k_ctx, v_ctx).reshape(bsz, n_ctx, MLA_HEADS * V_DIM) @ w_o
    else:
        yc = None
    return y, yc


def cmul(ar, ai, br, bi):
    return ar * br - ai * bi, ar * bi + ai * br


def zoh_discretise(a_re, a_im, log_step, b_re, b_im):
    delta = jnp.exp(log_step)[:, None]
    mag = jnp.exp(delta * a_re)
    abar_r = mag * jnp.cos(delta * a_im)
    abar_i = mag * jnp.sin(delta * a_im)
    nr, ni = abar_r - 1, abar_i
    den = a_re * a_re + a_im * a_im
    fr = (nr * a_re + ni * a_im) / den
    fi = (ni * a_re - nr * a_im) / den
    bbar_r, bbar_i = cmul(fr[..., None], fi[..., None], b_re, b_im)
    return (abar_r, abar_i), (bbar_r, bbar_i)


def scan_combine(e1, e2):
    a1r, a1i, b1r, b1i = e1
    a2r, a2i, b2r, b2i = e2
    ar, ai = cmul(a2r, a2i, a1r, a1i)
    br, bi = cmul(a2r, a2i, b1r, b1i)
    return ar, ai, br + b2r, bi + b2i


def diag_scan(ug, abar, bbar, h0, reverse):
    abar_r, abar_i = abar
    bu_r = jnp.einsum("btgh,gph->btgp", ug, bbar[0])
    bu_i = jnp.einsum("btgh,gph->btgp", ug, bbar[1])
    n_tok = ug.shape[1]
    if h0 is not None:
        first = n_tok - 1 if reverse else 0
        inj_r, inj_i = cmul(abar_r, abar_i, h0[0], h0[1])
        bu_r = bu_r.at[:, first].add(inj_r)
        bu_i = bu_i.at[:, first].add(inj_i)
    ar = jnp.broadcast_to(abar_r, (1, n_tok) + abar_r.shape)
    ai = jnp.broadcast_to(abar_i, (1, n_tok) + abar_i.shape)
    _, _, hr, hi = lax.associative_scan(scan_combine, (ar, ai, bu_r, bu_i), reverse=reverse, axis=1)
    return hr, hi


def readout(h, c_re, c_im):
    return jnp.einsum("btgp,ghp->btgh", h[0], c_re) - jnp.einsum("btgp,ghp->btgh", h[1], c_im)


def glu(y, w_a, w_b):
    z = jax.nn.gelu(y)
    return (z @ w_a) * jax.nn.sigmoid(z @ w_b)


def s5_mixer(u, uc, a_re, a_im, log_step, b_re, b_im, c_re, c_im, d_skip, w_glu_a, w_glu_b, need_ctx):
    bsz, n_lat, _ = u.shape
    n_ctx = uc.shape[1]
    ug = u.reshape(bsz, n_lat, N_GROUPS, GROUP_SIZE)
    ucg = uc.reshape(bsz, n_ctx, N_GROUPS, GROUP_SIZE)
    y = d_skip * u
    yc = d_skip * uc if need_ctx else None
    for direction in range(2):
        reverse = direction == 1
        abar, bbar = zoh_discretise(a_re[direction], a_im[direction], log_step[direction],
                                    b_re[direction], b_im[direction])
        hc = diag_scan(ucg, abar, bbar, None, reverse)
        end = 0 if reverse else n_ctx - 1
        h = diag_scan(ug, abar, bbar, (hc[0][:, end], hc[1][:, end]), reverse)
        y = y + readout(h, c_re[direction], c_im[direction]).reshape(bsz, n_lat, D_MODEL)
        if need_ctx:
            yc = yc + readout(hc, c_re[direction], c_im[direction]).reshape(bsz, n_ctx, D_MODEL)
    out = glu(y, w_glu_a, w_glu_b)
    out_c = glu(yc, w_glu_a, w_glu_b) if need_ctx else None
    return out, out_c


def swiglu(t, w1, w3, w2):
    return (jax.nn.silu(t @ w1) * (t @ w3)) @ w2


def moe_ffn(t, w_router, w1, w3, w2):
    logits = (t @ w_router).astype(jnp.float32)
    top_val, top_idx = lax.top_k(logits, TOP_K)
    gates = jax.nn.softmax(top_val, axis=-1)
    combine = jnp.sum(gates[..., None] * (top_idx[..., None] == jnp.arange(N_EXPERTS)), axis=-2)
    combine = combine.astype(t.dtype)
    out = jnp.zeros_like(t)
    for e in range(N_EXPERTS):
        out = out + combine[..., e:e + 1] * swiglu(t, w1[e], w3[e], w2[e])
    return out


def setup_inputs(seed: int = 0) -> dict:
    key = jax.random.key(seed)
    ks = iter(jax.random.split(key, 40))
    f32 = jnp.float32

    def nrm(shape, scale):
        return jax.random.normal(next(ks), shape, f32) * scale

    beta = DEEPNORM_BETA
    d = D_MODEL
    inp = {}
    inp["x"] = nrm((BATCH, SEQ, d), 1.0)
    inp["c"] = nrm((BATCH, d), 1.0)
    inp["ctx"] = nrm((BATCH, CTX_LEN, d), 1.0)
    inp["c_ctx"] = nrm((d,), 1.0)
    inp["ada_w"] = nrm((DEPTH, d, N_MOD * d), d ** -0.5)
    inp["ada_b"] = nrm((DEPTH, N_MOD * d), 0.01)
    inp["ln_g"] = 1.0 + nrm((DEPTH, 2, d), 0.02)
    inp["ln_b"] = nrm((DEPTH, 2, d), 0.02)
    inp["mla_w_down"] = nrm((N_EVEN, d, Q_LORA + KV_LORA + QK_ROPE), d ** -0.5)
    inp["mla_g_q"] = 1.0 + nrm((N_EVEN, Q_LORA), 0.02)
    inp["mla_g_kv"] = 1.0 + nrm((N_EVEN, KV_LORA), 0.02)
    inp["mla_w_uq"] = nrm((N_EVEN, Q_LORA, MLA_HEADS, QK_DIM), Q_LORA ** -0.5)
    inp["mla_w_uk"] = nrm((N_EVEN, KV_LORA, MLA_HEADS, QK_NOPE), KV_LORA ** -0.5)
    inp["mla_w_uv"] = nrm((N_EVEN, KV_LORA, MLA_HEADS, V_DIM), beta * KV_LORA ** -0.5)
    inp["mla_w_o"] = nrm((N_EVEN, MLA_HEADS * V_DIM, d), beta * (MLA_HEADS * V_DIM) ** -0.5)
    inp["s5_a_re"] = -0.5 + nrm((N_ODD, 2, N_GROUPS, STATE), 0.02)
    inp["s5_a_im"] = math.pi * jnp.arange(STATE, dtype=f32) + nrm((N_ODD, 2, N_GROUPS, STATE), 0.02)
    inp["s5_log_step"] = jax.random.uniform(next(ks), (N_ODD, 2, N_GROUPS), f32,
                                            math.log(STEP_MIN), math.log(STEP_MAX))
    inp["s5_b_re"] = nrm((N_ODD, 2, N_GROUPS, STATE, GROUP_SIZE), (2 * GROUP_SIZE) ** -0.5)
    inp["s5_b_im"] = nrm((N_ODD, 2, N_GROUPS, STATE, GROUP_SIZE), (2 * GROUP_SIZE) ** -0.5)
    inp["s5_c_re"] = nrm((N_ODD, 2, N_GROUPS, GROUP_SIZE, STATE), 0.5)
    inp["s5_c_im"] = nrm((N_ODD, 2, N_GROUPS, GROUP_SIZE, STATE), 0.5)
    inp["s5_d"] = nrm((N_ODD, d), 0.5)
    inp["s5_w_glu_a"] = nrm((N_ODD, d, d), beta * d ** -0.5)
    inp["s5_w_glu_b"] = nrm((N_ODD, d, d), d ** -0.5)
    inp["ffn_w1"] = nrm((N_EVEN, d, D_FF), d ** -0.5)
    inp["ffn_w3"] = nrm((N_EVEN, d, D_FF), d ** -0.5)
    inp["ffn_w2"] = nrm((N_EVEN, D_FF, d), beta * D_FF ** -0.5)
    inp["moe_w_router"] = nrm((N_ODD, d, N_EXPERTS), d ** -0.5)
    inp["moe_w1"] = nrm((N_ODD, N_EXPERTS, d, D_FF_EXPERT), d ** -0.5)
    inp["moe_w3"] = nrm((N_ODD, N_EXPERTS, d, D_FF_EXPERT), d ** -0.5)
    inp["moe_w2"] = nrm((N_ODD, N_EXPERTS, D_FF_EXPERT, d), beta * D_FF_EXPERT ** -0.5)
    return inp


def reference(x, c, ctx, c_ctx, ada_w, ada_b, ln_g, ln_b,
              mla_w_down, mla_g_q, mla_g_kv, mla_w_uq, mla_w_uk, mla_w_uv, mla_w_o,
              s5_a_re, s5_a_im, s5_log_step, s5_b_re, s5_b_im, s5_c_re, s5_c_im, s5_d,
              s5_w_glu_a, s5_w_glu_b,
              ffn_w1, ffn_w3, ffn_w2,
              moe_w_router, moe_w1, moe_w3, moe_w2):
    bsz, n_lat, _ = x.shape
    rows = n_lat // GRID_W
    cos, sin = axial_rope_tables(rows)
    sc = jax.nn.silu(c)
    scc = jax.nn.silu(c_ctx)
    h, hc = x, ctx
    for layer in range(DEPTH):
        j = layer // N_MIXERS
        last = layer == DEPTH - 1
        need_ctx = not last
        mod = (sc @ ada_w[layer] + ada_b[layer]).reshape(bsz, 1, N_MOD, D_MODEL)
        mod_c = (scc @ ada_w[layer] + ada_b[layer]).reshape(N_MOD, D_MODEL)
        u = modulate(h, mod[:, :, 0], mod[:, :, 1])
        uc = modulate(hc, mod_c[0], mod_c[1])
        if layer % N_MIXERS == 0:
            y, yc = mla_mixer(u, uc, cos, sin, mla_w_down[j], mla_g_q[j], mla_g_kv[j], mla_w_uq[j],
                              mla_w_uk[j], mla_w_uv[j], mla_w_o[j], need_ctx)
        else:
            y, yc = s5_mixer(u, uc, s5_a_re[j], s5_a_im[j], s5_log_step[j], s5_b_re[j], s5_b_im[j],
                             s5_c_re[j], s5_c_im[j], s5_d[j], s5_w_glu_a[j], s5_w_glu_b[j], need_ctx)
        h = layer_norm(DEEPNORM_ALPHA * h + mod[:, :, 2] * y, ln_g[layer, 0], ln_b[layer, 0])
        if need_ctx:
            hc = layer_norm(DEEPNORM_ALPHA * hc + mod_c[2] * yc, ln_g[layer, 0], ln_b[layer, 0])
        u = modulate(h, mod[:, :, 3], mod[:, :, 4])
        if need_ctx:
            uc = modulate(hc, mod_c[3], mod_c[4])
        if layer % 2 == 0:
            f = swiglu(u, ffn_w1[j], ffn_w3[j], ffn_w2[j])
            fc = swiglu(uc, ffn_w1[j], ffn_w3[j], ffn_w2[j]) if need_ctx else None
        else:
            f = moe_ffn(u, moe_w_router[j], moe_w1[j], moe_w3[j], moe_w2[j])
            fc = moe_ffn(uc, moe_w_router[j], moe_w1[j], moe_w3[j], moe_w2[j]) if need_ctx else None
        h = layer_norm(DEEPNORM_ALPHA * h + mod[:, :, 5] * f, ln_g[layer, 1], ln_b[layer, 1])
        if need_ctx:
            hc = layer_norm(DEEPNORM_ALPHA * hc + mod_c[5] * fc, ln_g[layer, 1], ln_b[layer, 1])
    return h
```

```python
import numpy as np
from contextlib import ExitStack
import concourse.bass as bass
import concourse.mybir as mybir
from concourse.bass_utils import run_bass_kernel_spmd

F32 = mybir.dt.float32
BF16 = mybir.dt.bfloat16
AF = mybir.ActivationFunctionType
ALU = mybir.AluOpType

D = 1024
KC = 8
NCTX = 256
NOWN = 2048
NQ = NCTX + NOWN
NALL = NQ + NOWN
ALPHA = 4.0 ** 0.25
EPS = 1e-6
EPS_LN = EPS / (ALPHA * ALPHA)
DFF = 2816
NEXP = 8
DFE = 3584
QK_SCALE = 192.0 ** -0.5


class Sched:
    ENGS = ('pe', 'act', 'dve', 'pool', 'sp')

    def __init__(self, nc, es, ndma=20):
        self.nc = nc
        self.sem = {e: es.enter_context(nc.semaphore("sem_" + e)) for e in self.ENGS}
        self.dsem = [es.enter_context(nc.semaphore("dsem%d" % i)) for i in range(ndma)]
        self.dcnt = [0] * ndma
        self.dpool = {'pool': list(range(0, ndma // 2)), 'sp': list(range(ndma // 2, ndma))}
        self.dnext = {'pool': 0, 'sp': 0}
        self.ccsem = es.enter_context(nc.semaphore("ccsem"))
        self.cccnt = 0
        self.cnt = {e: 0 for e in self.ENGS}
        self.prog = {e: [] for e in self.ENGS}
        self.lastw = {}
        self.readers = {}
        self.known = {e: {} for e in self.ENGS}
        self.snap = {}
        self.nops = 0

    def _semof(self, s):
        if isinstance(s, str):
            return self.sem[s]
        return self.ccsem if s[0] == 'c' else self.dsem[s[1]]

    def collective(self, fn, reads=(), writes=()):
        waits = self._deps('pool', reads, writes)
        self.cccnt += 1
        tok = (('c', 0), self.cccnt)
        self.prog['pool'].append((waits, fn, 'cc'))
        self.snap[tok] = {s: c for s, c in self.known['pool'].items() if isinstance(s, str)}
        self._record(tok, reads, writes)
        return tok

    def _deps(self, eng, reads, writes):
        deps = {}
        for k in reads:
            t = self.lastw.get(k)
            if t is not None and deps.get(t[0], 0) < t[1]:
                deps[t[0]] = t[1]
        for k in writes:
            t = self.lastw.get(k)
            if t is not None and deps.get(t[0], 0) < t[1]:
                deps[t[0]] = t[1]
            for s, c in self.readers.get(k, {}).items():
                if deps.get(s, 0) < c:
                    deps[s] = c
        kn = self.known[eng]
        waits = []
        for s, c in deps.items():
            if s == 'pe' and eng == 'pe':
                continue
            if kn.get(s, 0) >= c:
                continue
            waits.append((s, c))
        for s, c in waits:
            kn[s] = c
            sn = self.snap.get((s, c))
            if sn:
                for s2, c2 in sn.items():
                    if kn.get(s2, 0) < c2:
                        kn[s2] = c2
        return waits

    def _record(self, tok, reads, writes):
        for k in writes:
            self.lastw[k] = tok
            self.readers[k] = {}
        for k in reads:
            r = self.readers.setdefault(k, {})
            if r.get(tok[0], 0) < tok[1]:
                r[tok[0]] = tok[1]

    def op(self, eng, fn, reads=(), writes=()):
        waits = self._deps(eng, reads, writes)
        self.cnt[eng] += 1
        tok = (eng, self.cnt[eng])
        self.prog[eng].append((waits, fn, None))
        self.snap[tok] = {s: c for s, c in self.known[eng].items() if isinstance(s, str)}
        self._record(tok, reads, writes)
        self.nops += 1
        return tok

    def dma(self, q, out, in_, reads=(), writes=()):
        pool_ = self.dpool[q]
        i = pool_[self.dnext[q] % len(pool_)]
        self.dnext[q] += 1
        waits = self._deps(q, reads, writes)
        src = ('d', i)
        prev = 16 * self.dcnt[i]
        if prev > 0 and self.known[q].get(src, 0) < prev:
            waits.append((src, prev))
            self.known[q][src] = prev
        self.dcnt[i] += 1
        tok = (src, 16 * self.dcnt[i])
        self.prog[q].append((waits, (lambda e, o=out, a=in_: e.dma_start(out=o, in_=a)), i))
        self.snap[tok] = {s: c for s, c in self.known[q].items() if isinstance(s, str)}
        self._record(tok, reads, writes)
        self.nops += 1
        return tok

    def flush(self, final_tokens=()):
        nc = self.nc
        prog = self.prog
        self.prog = {e: [] for e in self.ENGS}

        def emit(name, e):
            for waits, fn, di in prog[name]:
                for s, c in waits:
                    e.wait_ge(self._semof(s), c)
                ins = fn(e)
                if di is None:
                    ins.then_inc(self.sem[name], 1)
                elif di == 'cc':
                    ins.then_inc(self.ccsem)
                else:
                    ins.then_inc(self.dsem[di], 16)
            if name == 'sp':
                for s, c in final_tokens:
                    e.wait_ge(self._semof(s), c)

        with nc.Block() as block:
            if prog['pe']:
                @block.tensor
                def _(e):
                    emit('pe', e)
            if prog['act']:
                @block.scalar
                def _(e):
                    emit('act', e)
            if prog['dve']:
                @block.vector
                def _(e):
                    emit('dve', e)
            if prog['pool']:
                @block.gpsimd
                def _(e):
                    emit('pool', e)
            if prog['sp'] or final_tokens:
                @block.sync
                def _(e):
                    emit('sp', e)
        for e in self.ENGS:
            for s in self.ENGS:
                self.known[e][s] = self.cnt[s]


class B:
    def __init__(self, S):
        self.S = S
        self.rr = 0

    def mm(self, items, reads, writes):
        its = list(items)

        def fn(e):
            ins = None
            for (o, l, r, st, sp) in its:
                ins = e.matmul(o, l, r, start=st, stop=sp)
            return ins
        return self.S.op('pe', fn, reads, writes)

    def act(self, out, in_, func, reads, writes, bias=None, scale=None, eng='act'):
        kw = {}
        if bias is not None:
            kw['bias'] = bias
        if scale is not None:
            kw['scale'] = scale
        return self.S.op('act', lambda e: e.activation(out=out, in_=in_, func=func, **kw), reads, writes)

    def tt(self, eng, out, in0, in1, op, reads, writes):
        return self.S.op(eng, lambda e: e.tensor_tensor(out, in0, in1, op), reads, writes)

    def ts(self, eng, out, in0, s1, s2, op0, op1, reads, writes):
        if op1 is None:
            return self.S.op(eng, lambda e: e.tensor_scalar(out, in0, s1, None, op0), reads, writes)
        return self.S.op(eng, lambda e: e.tensor_scalar(out, in0, s1, s2, op0, op1), reads, writes)

    def stt(self, eng, out, in0, scalar, in1, op0, op1, reads, writes):
        return self.S.op(eng, lambda e: e.scalar_tensor_tensor(out, in0, scalar, in1, op0, op1), reads, writes)

    def copy(self, eng, out, in_, reads, writes):
        if eng == 'act':
            return self.S.op('act', lambda e: e.activation(out=out, in_=in_, func=AF.Copy), reads, writes)
        return self.S.op(eng, lambda e: e.tensor_copy(out, in_), reads, writes)

    def recip(self, eng, out, in_, reads, writes):
        return self.S.op(eng, lambda e: e.reciprocal(out, in_), reads, writes)

    def memset(self, eng, ap, val, writes):
        return self.S.op(eng, lambda e: e.memset(ap, val), (), writes)


def _rs(ap2d, shape):
    dims = shape[1:]
    if len(dims) > 1:
        names = ["a%d" % i for i in range(len(dims))]
        kw = {names[i]: int(dims[i]) for i in range(len(dims) - 1)}
        ap2d = ap2d.rearrange("p (%s) -> p %s" % (" ".join(names), " ".join(names)), **kw)
    return ap2d[:shape[0]]


class Arena:
    def __init__(self, flat_f32):
        self.f = flat_f32
        self.off = 0
        self.W = flat_f32.shape[1]

    def f32(self, shape):
        n = int(np.prod(shape[1:]))
        v = self.f[:, self.off:self.off + n]
        self.off += n
        assert self.off <= self.W, (self.off, self.W)
        return _rs(v, shape)

    def bf16(self, shape):
        n = int(np.prod(shape[1:]))
        nw = (n + 1) // 2
        v = self.f[:, self.off:self.off + nw].bitcast(BF16)[:, :n]
        self.off += nw
        assert self.off <= self.W, (self.off, self.W)
        return _rs(v, shape)


def build_program(debug=False, stop_after=None):
    nc = bass.Bass("TRN2", target_bir_lowering=False)
    dt = nc.dram_tensor

    def din(name, shape, dtype=F32):
        return dt(name, list(shape), dtype, kind="ExternalInput").ap()

    def dout(name, shape, dtype=F32):
        return dt(name, list(shape), dtype, kind="ExternalOutput").ap()

    xall = din("xall", [128, KC, NALL])
    cosk = din("cosk", [64, NALL])
    sink = din("sink", [64, NALL])
    cvec = din("cvec", [128, KC, 2])
    ada_w = din("ada_w", [2, D, 6 * D])
    adab = din("adab", [128, 2, 48])
    lng = din("lng", [128, 4, KC])
    lnb = din("lnb", [128, 4, KC])
    wdown = din("wdown", [128, KC, 768])
    gq = din("gq", [128, 3])
    gkv = din("gkv", [128, 2])
    wuq = din("wuq", [128, 3, 8 * 256])
    wuk = din("wuk", [128, 2, 1024])
    wuv = din("wuv", [128, 2, 1024])
    wo = din("wo", [128, 8, 1024])
    ffn_w1 = din("ffn_w1", [D, DFF])
    ffn_w3 = din("ffn_w3", [D, DFF])
    ffn_w2 = din("ffn_w2", [DFF, D])
    s5a = din("s5a", [128, 2, 2, 32])
    s5ls = din("s5ls", [128, 2, 32])
    s5b = din("s5b", [128, 2, 2, 32, 16])
    s5c = din("s5c", [128, 2, 2, 32, 16])
    s5d = din("s5d", [128, KC])
    pmask = din("pmask", [128, 2])
    cc_in = nc.dram_tensor("cc_in", [128, 128], F32)
    cc_out = nc.dram_tensor("cc_out", [128, 128], F32)
    ident = din("ident", [128, 128])
    identf = din("identf", [128, 128])
    glu_a = din("glu_a", [D, D])
    glu_b = din("glu_b", [D, D])
    wrt = din("wrt", [128, KC, 8])
    sel = din("sel", [8, 8, 128])
    moe_w1 = din("moe_w1", [NEXP, D, DFE])
    moe_w3 = din("moe_w3", [NEXP, D, DFE])
    moe_w2 = din("moe_w2", [NEXP, DFE, D])
    yout = dout("yout", [128, KC, NOWN])
    dbg = dout("dbg", [128, KC, NQ]) if debug else None

    with ExitStack() as es:
        S = Sched(nc, es)
        b = B(S)
        sb = lambda name, shape, dtype=F32, stack=es: stack.enter_context(nc.sbuf_tensor(name, list(shape), dtype))
        PS = [es.enter_context(nc.psum_tensor("ps%d" % i, [128, 512], F32)) for i in range(8)]
        pk = lambda i: ('ps', i)

        hT = sb("hT", [128, KC, NQ], F32)
        ubuf = sb("ubuf", [128, KC, NQ], BF16)
        hT_flat = hT[:].rearrange("p k n -> p (k n)")
        ub_flat32 = ubuf[:].rearrange("p k n -> p (k n)").bitcast(F32)
        modT = sb("modT", [128, 2, 48, 2], F32)
        pv = sb("pv", [128, 512], F32)
        lng_s = sb("lng_s", [128, 4, KC], F32)
        lnb_s = sb("lnb_s", [128, 4, KC], F32)
        ones_d = sb("ones_d", [128, 128], F32)
        ones_q = sb("ones_q", [128, 128], F32)
        ones_kv = sb("ones_kv", [128, 128], F32)
        ones_bf = sb("ones_bf", [128, 128], BF16)

        pv_off = [0]

        def pvslot(n):
            o = pv_off[0]
            pv_off[0] += n
            assert pv_off[0] <= 512
            return o

        SL = {}
        for l in range(2):
            SL[('s1p', l)] = pvslot(16)
            SL[('gam', l)] = pvslot(16)
            SL[('s2p', l)] = pvslot(16)
            SL[('gaf', l)] = pvslot(16)
            SL[('gU', l, 0)] = pvslot(16)
            SL[('bU', l, 0)] = pvslot(16)
        SL[('gU', 0, 1)] = pvslot(16)
        SL[('bU', 0, 1)] = pvslot(16)

        def pvs(key, kc, s):
            o = SL[key] + kc * 2 + s
            return pv[:, o:o + 1]

        def modv(l, m, kc, s):
            return modT[:, l, m * 8 + kc, s:s + 1]

        def ln_norm(r_ap_fn, N, s, rkeys, g1, b1, out1_fn, out1_keys, g2=None, b2=None, out2_fn=None, out2_keys=(),
                    tmp=None, psA=6, psB=7, in2_fn=None):
            sqb, mean_sb, m2, sd, rstd, nmr = tmp
            for kc in range(KC):
                b.mm([(PS[psA][:, :N], ones_d[:], r_ap_fn(kc), kc == 0, kc == KC - 1)], [rkeys[kc], 'ones'], [pk(psA)])
            for kc in range(KC):
                q = kc % 2
                b.act(sqb[:, q, :N], r_ap_fn(kc), AF.Square, [rkeys[kc]], [('sqb', q)])
                b.mm([(PS[psB][:, :N], ones_d[:], sqb[:, q, :N], kc == 0, kc == KC - 1)], [('sqb', q), 'ones'], [pk(psB)])
            b.copy('act', mean_sb[:, :N], PS[psA][:, :N], [pk(psA)], ['mean_sb'])
            b.tt('dve', m2[:, :N], mean_sb[:, :N], mean_sb[:, :N], ALU.mult, ['mean_sb'], ['m2'])
            b.tt('dve', m2[:, :N], PS[psB][:, :N], m2[:, :N], ALU.subtract, [pk(psB), 'm2'], ['m2'])
            b.act(sd[:, :N], m2[:, :N], AF.Sqrt, ['m2'], ['sd'], bias=eps_ln[:, 0:1])
            b.recip('dve', rstd[:, :N], sd[:, :N], ['sd'], ['rstd'])
            b.stt('dve', nmr[:, :N], mean_sb[:, :N], -1.0, rstd[:, :N], ALU.mult, ALU.mult, ['mean_sb', 'rstd'], ['nmr'])
            for kc in range(KC):
                ra = r_ap_fn(kc)
                eng = 'dve' if kc % 2 == 0 else 'pool'
                b.tt(eng, ra, ra, rstd[:, :N], ALU.mult, [rkeys[kc], 'rstd'], [rkeys[kc]])
                b.tt(eng, ra, ra, nmr[:, :N], ALU.add, [rkeys[kc], 'nmr'], [rkeys[kc]])
                if out2_fn is not None:
                    ok2 = list(out2_keys) if in2_fn is not None else [out2_keys[kc]]
                    b.act(out2_fn(kc), (in2_fn(kc) if in2_fn is not None else ra), AF.Identity, [rkeys[kc]], ok2, bias=b2(kc), scale=g2(kc))
                b.act(out1_fn(kc), ra, AF.Identity, [rkeys[kc]], [out1_keys[kc]], bias=b1(kc), scale=g1(kc))

        eps_ln = sb("eps_ln", [128, 2], F32)

        def ln_tmps(stack, tag):
            return (sb("sqb" + tag, [128, 2, 512], F32, stack), sb("mean" + tag, [128, 512], F32, stack),
                    sb("m2" + tag, [128, 512], F32, stack), sb("sd" + tag, [128, 512], F32, stack),
                    sb("rstd" + tag, [128, 512], F32, stack), sb("nmr" + tag, [128, 512], F32, stack))

        with ExitStack() as ps0:
            aw = sb("aw", [128, 2, KC, 1024], F32, ps0)
            cv = sb("cv", [128, KC, 2], F32, ps0)
            scT = sb("scT", [128, KC, 2], F32, ps0)
            adab_s = sb("adab_s", [128, 2, 48], F32, ps0)
            sig = sb("sig", [128, KC, 2], F32, ps0)
            b.memset('dve', ones_d[:], 1.0 / 1024, ['ones'])
            b.memset('dve', ones_q[:], 1.0 / 384, ['ones_q'])
            b.memset('dve', ones_kv[:], 1.0 / 256, ['ones_kv'])
            b.memset('dve', ones_bf[:], 1.0, ['ones_bf'])
            b.memset('dve', eps_ln[:, 0:1], EPS_LN, ['eps_ln'])
            b.memset('dve', eps_ln[:, 1:2], EPS, ['eps_ln1'])
            S.dma('sp', cv[:], cvec[:], [], ['cv'])
            S.dma('sp', adab_s[:], adab[:], [], ['adab'])
            S.dma('sp', lng_s[:], lng[:], [], ['lng'])
            S.dma('sp', lnb_s[:], lnb[:], [], ['lnb'])
            b.act(sig[:], cv[:], AF.Sigmoid, ['cv'], ['sig'])
            b.tt('dve', scT[:], cv[:], sig[:], ALU.mult, ['cv', 'sig'], ['scT'])
            i = 0
            for l in range(2):
                for m in range(6):
                    q = i % 2
                    i += 1
                    src = ada_w[l, :, m * 1024:(m + 1) * 1024].rearrange("(kc p) n -> p kc n", p=128)
                    S.dma('sp', aw[:, q, :, :], src, [], [('aw', q)])
                    for oc in range(KC):
                        col = (m * 8 + oc) * 2
                        pb = l
                        b.mm([(PS[pb][:, col:col + 2], aw[:, q, kc, oc * 128:(oc + 1) * 128], scT[:, kc, :], kc == 0, kc == KC - 1)
                              for kc in range(KC)], [('aw', q), 'scT'], [pk(pb)])
                for s in range(2):
                    b.tt('dve', modT[:, l, :, s], PS[l][:, 0:96].rearrange("p (j s) -> p j s", s=2)[:, :, s], adab_s[:, l, :],
                         ALU.add, [pk(l), 'adab'], [('modT', l)])
            def pvw(key):
                o = SL[key]
                return pv[:, o:o + 16].rearrange("p (k s) -> p k s", s=2)

            def modw(l, m):
                return modT[:, l, m * 8:(m + 1) * 8, :]

            def bc2(ap):
                return ap.unsqueeze(2).to_broadcast([128, KC, 2])
            for l in range(2):
                rd = [('modT', l), 'lng', 'lnb']
                b.ts('dve', pvw(('s1p', l)), modw(l, 1), 1.0, None, ALU.add, None, rd, ['pv'])
                b.ts('dve', pvw(('gam', l)), modw(l, 2), 1.0 / ALPHA, None, ALU.mult, None, rd, ['pv'])
                b.ts('dve', pvw(('s2p', l)), modw(l, 4), 1.0, None, ALU.add, None, rd, ['pv'])
                b.ts('dve', pvw(('gaf', l)), modw(l, 5), 1.0 / ALPHA, None, ALU.mult, None, rd, ['pv'])
                b.tt('dve', pvw(('gU', l, 0)), pvw(('s2p', l)), bc2(lng_s[:, l * 2 + 0, :]), ALU.mult, rd + ['pv'], ['pv'])
                b.tt('dve', pvw(('bU', l, 0)), pvw(('s2p', l)), bc2(lnb_s[:, l * 2 + 0, :]), ALU.mult, rd + ['pv'], ['pv'])
                b.tt('dve', pvw(('bU', l, 0)), pvw(('bU', l, 0)), modw(l, 3), ALU.add, rd + ['pv'], ['pv'])
            rd = [('modT', 1), 'lng', 'lnb', 'pv']
            b.tt('dve', pvw(('gU', 0, 1)), pvw(('s1p', 1)), bc2(lng_s[:, 1, :]), ALU.mult, rd, ['pv'])
            b.tt('dve', pvw(('bU', 0, 1)), pvw(('s1p', 1)), bc2(lnb_s[:, 1, :]), ALU.mult, rd, ['pv'])
            b.tt('dve', pvw(('bU', 0, 1)), pvw(('bU', 0, 1)), modw(1, 0), ALU.add, rd, ['pv'])
            S.flush()

        groups = [(0, 256, 1)] + [(256 + 512 * i, 512, 0) for i in range(8)]
        qgroups = groups[:5]

        pl0 = ExitStack()
        if True:
            cqT = sb("cqT", [128, 3, NQ], BF16, pl0)
            ckvT = sb("ckvT", [128, 2, NALL], BF16, pl0)
            krT = sb("krT", [64, NALL], BF16, pl0)

            with ExitStack() as p1:
                wd = sb("wd", [128, KC, 768], BF16, p1)
                gq_s = sb("gq_s", [128, 3], F32, p1)
                gkv_s = sb("gkv_s", [128, 2], F32, p1)
                hA = Arena(hT_flat)
                uA = Arena(ub_flat32)
                xg = hA.f32([128, 2, KC, 512])
                raw = hA.f32([128, 7, 512])
                sqr = hA.f32([128, 5, 512])
                rq = hA.f32([128, 2, 512])
                rs = hA.f32([128, 2, 512])
                kt1 = hA.f32([64, 2, 512])
                ug = uA.bf16([128, 2, KC, 512])
                csk = uA.f32([64, 2, 2, 512])
                S.dma('pool', wd[:], wdown[:], [], ['wd'])
                S.dma('sp', gq_s[:], gq[:], [], ['gq'])
                S.dma('sp', gkv_s[:], gkv[:], [], ['gkv'])
                psr = 0
                for gi, (c0, N, s) in enumerate(groups):
                    q = gi % 2
                    own = c0 < NQ
                    S.dma('sp', xg[:, q, :, :N], xall[:, :, c0:c0 + N], [], [('xg', q)])
                    S.dma('sp', csk[:, q, 0, :N], cosk[:, c0:c0 + N], [], [('csk', q)])
                    S.dma('sp', csk[:, q, 1, :N], sink[:, c0:c0 + N], [], [('csk', q)])
                    for kc in range(KC):
                        if kc % 2 == 0:
                            b.ts('dve', ug[:, q, kc, :N], xg[:, q, kc, :N], pvs(('s1p', 0), kc, s), modv(0, 0, kc, s),
                                 ALU.mult, ALU.add, [('xg', q), 'pv', ('modT', 0)], [('ug', q, kc)])
                        else:
                            b.act(ug[:, q, kc, :N], xg[:, q, kc, :N], AF.Identity, [('xg', q), 'pv', ('modT', 0)], [('ug', q, kc)],
                                  bias=modv(0, 0, kc, s), scale=pvs(('s1p', 0), kc, s))
                    chunks = ([(0, 0, 128), (1, 128, 128), (2, 256, 128)] if own else []) + \
                             [(3, 384, 128), (4, 512, 128), (5, 640, 64), (6, 704, 64)]
                    for (ri, m0, M) in chunks:
                        pb = psr % 4
                        psr += 1
                        b.mm([(PS[pb][:M, :N], wd[:, kc, m0:m0 + M], ug[:, q, kc, :N], kc == 0, kc == KC - 1) for kc in range(KC)],
                             ['wd'] + [('ug', q, kc) for kc in range(KC)], [pk(pb)])
                        b.copy('act', raw[:M, ri, :N], PS[pb][:M, :N], [pk(pb)], [('raw', ri)])
                    qs = [0, 1, 2] if own else []
                    for ri in qs + [3, 4]:
                        eng = 'dve' if ri % 2 == 0 else 'pool'
                        b.tt(eng, sqr[:, ri, :N], raw[:, ri, :N], raw[:, ri, :N], ALU.mult, [('raw', ri)], [('sqr', ri)])
                    if own:
                        b.mm([(PS[4][:, :N], ones_q[:], sqr[:, ri, :N], ri == 0, ri == 2) for ri in range(3)],
                             ['ones_q'] + [('sqr', ri) for ri in range(3)], [pk(4)])
                        b.act(rq[:, 0, :N], PS[4][:, :N], AF.Sqrt, [pk(4), 'eps_ln1'], [('rq', 0)], bias=eps_ln[:, 1:2])
                        b.recip('dve', rs[:, 0, :N], rq[:, 0, :N], [('rq', 0)], [('rs', 0)])
                        for ri in range(3):
                            b.stt('dve', cqT[:, ri, c0:c0 + N], raw[:, ri, :N], gq_s[:, ri:ri + 1], rs[:, 0, :N], ALU.mult, ALU.mult,
                                  [('raw', ri), 'gq', ('rs', 0)], [('cqT', gi)])
                    b.mm([(PS[5][:, :N], ones_kv[:], sqr[:, ri, :N], ri == 3, ri == 4) for ri in (3, 4)],
                         ['ones_kv', ('sqr', 3), ('sqr', 4)], [pk(5)])
                    b.act(rq[:, 1, :N], PS[5][:, :N], AF.Sqrt, [pk(5), 'eps_ln1'], [('rq', 1)], bias=eps_ln[:, 1:2])
                    b.recip('dve', rs[:, 1, :N], rq[:, 1, :N], [('rq', 1)], [('rs', 1)])
                    for ri in (3, 4):
                        b.stt('dve', ckvT[:, ri - 3, c0:c0 + N], raw[:, ri, :N], gkv_s[:, ri - 3:ri - 2], rs[:, 1, :N], ALU.mult, ALU.mult,
                              [('raw', ri), 'gkv', ('rs', 1)], [('ckvT', gi)])
                    b.tt('pool', kt1[:, 0, :N], raw[:64, 5, :N], csk[:, q, 0, :N], ALU.mult, [('raw', 5), ('csk', q)], [('kt1', 0)])
                    b.tt('pool', kt1[:, 1, :N], raw[:64, 6, :N], csk[:, q, 1, :N], ALU.mult, [('raw', 6), ('csk', q)], [('kt1', 1)])
                    b.tt('pool', krT[:, c0:c0 + N], kt1[:, 0, :N], kt1[:, 1, :N], ALU.add, [('kt1', 0), ('kt1', 1)], [('krT', gi)])
                S.flush()

            if stop_after == 'p1':
                pass

            if True:
                aoT = ubuf
                with ExitStack() as p2:
                    hA = Arena(hT_flat)
                    KnT = hA.bf16([128, 2, NALL])
                    Vt = hA.bf16([128, 2, 34, 128])
                    QnT = hA.bf16([128, 2, NQ])
                    QrT = hA.bf16([64, 2, NQ])
                    qt1 = hA.f32([64, 2, 512])
                    rD = hA.f32([128, 2, 512])
                    csq = hA.f32([64, 2, 2, 512])
                    wq_h = sb("wq_h", [128, 2, 3, 256], BF16, p2)
                    wk_h = sb("wk_h", [128, 2, 2, 128], BF16, p2)
                    wv_h = sb("wv_h", [128, 2, 2, 128], BF16, p2)
                    Pt = sb("Pt", [128, 3, 512], BF16, p2)
                    allck = [('ckvT', gi) for gi in range(9)]
                    allcq = [('cqT', gi) for gi in range(5)]
                    allkr = [('krT', gi) for gi in range(9)]
                    cp = 0
                    pcount = 0
                    csn = 0
                    for h in range(8):
                        hq = h % 2
                        S.dma('pool', wq_h[:, hq, :, :], wuq[:, :, h * 256:(h + 1) * 256], [], [('wq_h', hq)])
                        S.dma('pool', wk_h[:, hq, :, :], wuk[:, :, h * 128:(h + 1) * 128], [], [('wk_h', hq)])
                        S.dma('pool', wv_h[:, hq, :, :], wuv[:, :, h * 128:(h + 1) * 128], [], [('wv_h', hq)])
                        for gi, (c0, N, s) in enumerate(groups):
                            pb = 6 + (cp % 2)
                            cp += 1
                            b.mm([(PS[pb][:, :N], wk_h[:, hq, kc, :], ckvT[:, kc, c0:c0 + N], kc == 0, kc == 1) for kc in range(2)],
                                 [('wk_h', hq), ('ckvT', gi)], [pk(pb)])
                            b.copy('act' if gi % 2 == 0 else 'dve', KnT[:, hq, c0:c0 + N], PS[pb][:, :N], [pk(pb)], [('KnT', hq, gi)])
                        for t0 in range(0, 34, 4):
                            nt = min(4, 34 - t0)
                            pb = 6 + (cp % 2)
                            cp += 1
                            items = []
                            for j in range(nt):
                                kt = t0 + j
                                for kc in range(2):
                                    items.append((PS[pb][:, j * 128:(j + 1) * 128], ckvT[:, kc, kt * 128:(kt + 1) * 128],
                                                  wv_h[:, hq, kc, :], kc == 0, kc == 1))
                            b.mm(items, [('wv_h', hq)] + allck, [pk(pb)])
                            b.copy('dve' if (t0 // 4) % 2 == 0 else 'act', Vt[:, hq, t0:t0 + nt, :],
                                   PS[pb][:, :nt * 128].rearrange("p (j d) -> p j d", d=128), [pk(pb)], [('Vt', hq, t0)])
                        for gi, (c0, N, s) in enumerate(qgroups):
                            cq_ = csn % 2
                            csn += 1
                            S.dma('sp', csq[:, cq_, 0, :N], cosk[:, c0:c0 + N], [], [('csq', cq_)])
                            S.dma('sp', csq[:, cq_, 1, :N], sink[:, c0:c0 + N], [], [('csq', cq_)])
                            pb = 6 + (cp % 2)
                            cp += 1
                            b.mm([(PS[pb][:, :N], wq_h[:, hq, kc, 0:128], cqT[:, kc, c0:c0 + N], kc == 0, kc == 2) for kc in range(3)],
                                 [('wq_h', hq), ('cqT', gi)], [pk(pb)])
                            b.copy('act', QnT[:, hq, c0:c0 + N], PS[pb][:, :N], [pk(pb)], [('QnT', hq, gi)])
                            pb = 6 + (cp % 2)
                            cp += 1
                            b.mm([(PS[pb][:64, :N], wq_h[:, hq, kc, 128:192], cqT[:, kc, c0:c0 + N], kc == 0, kc == 2) for kc in range(3)],
                                 [('wq_h', hq), ('cqT', gi)], [pk(pb)])
                            b.tt('dve', qt1[:, 0, :N], PS[pb][:64, :N], csq[:, cq_, 0, :N], ALU.mult, [pk(pb), ('csq', cq_)], [('qt1', 0)])
                            pb = 6 + (cp % 2)
                            cp += 1
                            b.mm([(PS[pb][:64, :N], wq_h[:, hq, kc, 192:256], cqT[:, kc, c0:c0 + N], kc == 0, kc == 2) for kc in range(3)],
                                 [('wq_h', hq), ('cqT', gi)], [pk(pb)])
                            b.tt('dve', qt1[:, 1, :N], PS[pb][:64, :N], csq[:, cq_, 1, :N], ALU.mult, [pk(pb), ('csq', cq_)], [('qt1', 1)])
                            b.tt('pool', QrT[:, hq, c0:c0 + N], qt1[:, 0, :N], qt1[:, 1, :N], ALU.add, [('qt1', 0), ('qt1', 1)], [('QrT', hq, gi)])
                        kn_keys = [('KnT', hq, gi) for gi in range(9)]
                        vt_keys = [('Vt', hq, t0) for t0 in range(0, 34, 4)]
                        for gi, (c0, N, s) in enumerate(qgroups):
                            kts = [0, 1] if s == 1 else list(range(34))
                            ob = 2 + (pcount % 2) * 2
                            pcount += 1

                            def issue_S(j, c0=c0, N=N, gi=gi):
                                kt = kts[j]
                                sbk = j % 2
                                b.mm([(PS[sbk][:, :N], KnT[:, hq, kt * 128:(kt + 1) * 128], QnT[:, hq, c0:c0 + N], True, False),
                                      (PS[sbk][:, :N], krT[:, kt * 128:(kt + 1) * 128], QrT[:, hq, c0:c0 + N], False, True)],
                                     kn_keys + allkr + [('QnT', hq, gi), ('QrT', hq, gi)], [pk(sbk)])
                            issue_S(0)
                            for j in range(len(kts)):
                                kt = kts[j]
                                if j + 1 < len(kts):
                                    issue_S(j + 1)
                                pq = j % 3
                                b.act(Pt[:, pq, :N], PS[j % 2][:, :N], AF.Exp, [pk(j % 2)], [('Pt', pq)], scale=QK_SCALE)
                                b.mm([(PS[ob][:, :N], Vt[:, hq, kt, :], Pt[:, pq, :N], j == 0, j == len(kts) - 1),
                                      (PS[ob + 1][:, :N], ones_bf[:], Pt[:, pq, :N], j == 0, j == len(kts) - 1)],
                                     vt_keys + [('Pt', pq), 'ones_bf'], [pk(ob), pk(ob + 1)])
                            rq_ = (pcount % 2)
                            b.recip('dve', rD[:, rq_, :N], PS[ob + 1][:, :N], [pk(ob + 1)], [('rD', rq_)])
                            b.tt('dve', aoT[:, h, c0:c0 + N], PS[ob][:, :N], rD[:, rq_, :N], ALU.mult, [pk(ob), ('rD', rq_)], [('aoT', h, gi)])
                    S.flush()
                pl0.close()

                with ExitStack() as p3:
                    wo_s = sb("wo_s", [128, 8, 1024], BF16, p3)
                    lntmp = ln_tmps(p3, "b3")
                    S.dma('pool', wo_s[:], wo[:], [], ['wo'])
                    def oproj3(gi):
                        (c0, N, s) = qgroups[gi]
                        S.dma('sp', hT[:, :, c0:c0 + N], xall[:, :, c0:c0 + N], [], [('hT', gi, kc) for kc in range(KC)])
                        for oc in range(KC):
                            pb = oc % 4
                            b.mm([(PS[pb][:, :N], wo_s[:, h, oc * 128:(oc + 1) * 128], aoT[:, h, c0:c0 + N], h == 0, h == 7) for h in range(8)],
                                 ['wo'] + [('aoT', h, gi) for h in range(8)], [pk(pb)])
                            b.stt('dve', hT[:, oc, c0:c0 + N], PS[pb][:, :N], pvs(('gam', 0), oc, s), hT[:, oc, c0:c0 + N], ALU.mult, ALU.add,
                                  [pk(pb), 'pv', ('hT', gi, oc)], [('hT', gi, oc)])
                    oproj3(0)
                    for gi, (c0, N, s) in enumerate(qgroups):
                        if gi + 1 < len(qgroups):
                            oproj3(gi + 1)
                        ln_norm(lambda kc, c0=c0, N=N: hT[:, kc, c0:c0 + N], N, s, [('hT', gi, kc) for kc in range(KC)],
                                g1=lambda kc: lng_s[:, 0, kc:kc + 1], b1=lambda kc: lnb_s[:, 0, kc:kc + 1],
                                out1_fn=lambda kc, c0=c0, N=N: hT[:, kc, c0:c0 + N], out1_keys=[('hT', gi, kc) for kc in range(KC)],
                                g2=lambda kc, s=s: pvs(('gU', 0, 0), kc, s), b2=lambda kc, s=s: pvs(('bU', 0, 0), kc, s),
                                out2_fn=lambda kc, c0=c0, N=N: ubuf[:, kc, c0:c0 + N], out2_keys=[('ubuf', gi, kc) for kc in range(KC)],
                                tmp=lntmp)
                    S.flush()

            with ExitStack() as p4:
                w1s = sb("w1s", [128, 2, KC, 512], BF16, p4)
                w3s = sb("w3s", [128, 2, KC, 512], BF16, p4)
                w2s = sb("w2s", [128, 2, 4, 1024], BF16, p4)
                s1b = sb("s1b", [128, 2, 512], F32, p4)
                gT = sb("gT", [128, 2, 4, 512], BF16, p4)
                swiglu_stages(nc, S, b, PS, pk, qgroups, list(range(5)), ubuf, hT, [(ffn_w1, ffn_w3, ffn_w2, DFF, None)],
                              (w1s, w3s, w2s, s1b, gT, None), lambda oc, s: pvs(('gaf', 0), oc, s), NF=4, NWB=2)
                S.flush()
            with ExitStack() as p4b:
                lntmp = ln_tmps(p4b, "b4")
                for gi, (c0, N, s) in enumerate(qgroups):
                    ln_norm(lambda kc, c0=c0, N=N: hT[:, kc, c0:c0 + N], N, s, [('hT', gi, kc) for kc in range(KC)],
                            g1=lambda kc: lng_s[:, 1, kc:kc + 1], b1=lambda kc: lnb_s[:, 1, kc:kc + 1],
                            out1_fn=lambda kc, c0=c0, N=N: hT[:, kc, c0:c0 + N], out1_keys=[('hT', gi, kc) for kc in range(KC)],
                            g2=lambda kc, s=s: pvs(('gU', 0, 1), kc, s), b2=lambda kc, s=s: pvs(('bU', 0, 1), kc, s),
                            out2_fn=lambda kc, c0=c0, N=N: ubuf[:, kc, :].rearrange("p (j c) -> p c j", j=8)[:, c0 // 8:(c0 + N) // 8, :],
                            out2_keys=[('ubuf', g_, kc) for kc in range(KC) for g_ in range(5)],
                            in2_fn=lambda kc, c0=c0, N=N: hT[:, kc, c0:c0 + N].rearrange("p (c j) -> p c j", j=8),
                            tmp=lntmp)
                S.flush()

        lvl = {'l0': 0, 'p5': 1, 'p6': 1, 'p7a': 2, 'p7b': 2, None: 2}[stop_after]
        if lvl >= 1:
            own_groups = [(256 + 512 * i, 512, 0) for i in range(4)]
            own_gidx = [1, 2, 3, 4]
            allh = [('hT', gi, kc) for gi in range(5) for kc in range(KC)]
            with ExitStack() as p5:
                s5a_s = sb("s5a_s", [128, 2, 2, 32], F32, p5)
                s5ls_s = sb("s5ls_s", [128, 2, 32], F32, p5)
                s5b_k = sb("s5b_k", [128, 2, 2, 2, 4, 16], F32, p5)
                s5c_k = sb("s5c_k", [128, 2, 2, 2, 4, 16], F32, p5)
                s5d_s = sb("s5d_s", [128, KC], F32, p5)
                dAB = sb("dAB", [128, 2, KC], F32, p5)
                rin_s = sb("rin_s", [128, 32, 2], F32, p5)
                fx_s = sb("fx_s", [128, 32, 2], F32, p5)
                idb = sb("idb", [128, 128], BF16, p5)
                Pw = sb("Pw", [128, 2, 9, 2, 32], F32, p5)
                cS = sb("cS", [128, 2, 2, 8, 32], F32, p5)
                rho = sb("rho", [128, 2, 32], F32, p5)
                Arot = sb("Arot", [128, 2, 2, 32, 18], F32, p5)
                Brot = sb("Brot", [128, 2, 2, 32, 16], F32, p5)
                tmp = sb("s5tmp", [128, 16, 32], F32, p5)
                wpw = sb("wpw", [128, 9, 2, 32], F32, p5)
                Vc = sb("Vc", [128, 2, 4, 8, 16], F32, p5)
                Vt1 = sb("Vt1", [128, 2, 4, 8, 16], F32, p5)
                tmp8 = Vt1[:].rearrange("p a b c d -> p a (b c d)")[:, :, 0:256]
                Vbd = sb("Vbd", [128, 2, 4, 8, 2, 32], BF16, p5)
                Ebd = sb("Ebd", [128, 2, 4, 8, 2, 32], BF16, p5)
                Cbd = sb("Cbd", [128, 2, 4, 2, 32], BF16, p5)
                Wst = sb("Wst", [128, 2, 8, 2, 128], BF16, p5)
                rot = sb("rot", [128, 2, 2, 288], F32, p5)
                trti = sb("trti", [128, 1, 2, 288], F32, p5)
                rt2 = sb("rt2", [128, 2, 2, 288], F32, p5)
                ccb2 = sb("ccb2", [128, 2, 64], F32, p5)
                gg = sb("gg", [128, 2, 2, 288], F32, p5)
                Hp = sb("Hp", [128, 2, 2, 2, 288], BF16, p5)
                t32z = sb("t32z", [128, 1, 2, 256], F32, p5)
                S.dma('sp', s5a_s[:], s5a[:], [], ['s5a'])
                S.dma('sp', s5ls_s[:], s5ls[:], [], ['s5ls'])
                S.dma('sp', s5d_s[:], s5d[:], [], ['s5d'])
                pm_s = sb("pm_s", [128, 2], F32, p5)
                ccb = sb("ccb", [128, 2, 64], F32, p5)
                S.dma('sp', pm_s[:], pmask[:], [], ['pmask'])
                S.dma('pool', idb[:], ident[:], [], ['idb'])
                b.memset('pool', Vbd[:], 0.0, [('Vbd', 0), ('Vbd', 1)])
                b.memset('pool', Ebd[:], 0.0, [('Ebd', 0), ('Ebd', 1)])
                b.memset('pool', Cbd[:], 0.0, [('Cbd', 0), ('Cbd', 1)])
                b.memset('pool', Hp[:], 0.0, [('Hp', d_, h_) for d_ in range(2) for h_ in range(2)])
                for kc in range(KC):
                    b.tt('dve', dAB[:, 0, kc:kc + 1], s5d_s[:, kc:kc + 1], pvs(('s1p', 1), kc, 0), ALU.mult, ['s5d', 'pv'], ['dAB'])
                    b.tt('dve', dAB[:, 1, kc:kc + 1], s5d_s[:, kc:kc + 1], modv(1, 0, kc, 0), ALU.mult, ['s5d', ('modT', 1)], ['dAB'])

                def T(i):
                    return tmp[:, i, :]

                def tk(i):
                    return ('s5t', i)

                def cmul(eng, o_r, o_i, a_r, a_i, b_r, b_i, t1, t2, k1, k2, rk, wk, conj_b=False):
                    b.tt(eng, t1, a_r, b_r, ALU.mult, rk, [k1])
                    b.tt(eng, t2, a_i, b_i, ALU.mult, rk, [k2])
                    b.tt(eng, o_r, t1, t2, ALU.add if conj_b else ALU.subtract, [k1, k2], wk)
                    b.tt(eng, t1, a_i, b_r, ALU.mult, rk, [k1])
                    b.tt(eng, t2, a_r, b_i, ALU.mult, rk, [k2])
                    b.tt(eng, o_i, t1, t2, ALU.subtract if conj_b else ALU.add, [k1, k2], wk)

                def discretize(d):
                    ar = s5a_s[:, d, 0, :]
                    ai = s5a_s[:, d, 1, :]
                    kin = ['s5a', 's5ls']
                    b.act(T(0), s5ls_s[:, d, :], AF.Exp, kin, [tk(0)])
                    b.tt('dve', T(1), T(0), ar, ALU.mult, [tk(0)] + kin, [tk(1)])
                    b.tt('dve', T(2), T(0), ai, ALU.mult, [tk(0)] + kin, [tk(2)])
                    b.ts('dve', T(3), T(1), 1.0 / 6, 1.0, ALU.mult, ALU.add, [tk(1)], [tk(3)])
                    for k in (5, 4, 3, 2, 1):
                        b.tt('dve', T(3), T(3), T(1), ALU.mult, [tk(3), tk(1)], [tk(3)])
                        b.ts('dve', T(3), T(3), 1.0 / k, 1.0, ALU.mult, ALU.add, [tk(3)], [tk(3)])
                    b.act(T(4), T(2), AF.Sin, [tk(2)], [tk(4)], scale=1.0 / 16)
                    b.act(T(5), T(2), AF.Sin, [tk(2)], [tk(5)], scale=1.0 / 32)
                    b.tt('dve', T(5), T(5), T(5), ALU.mult, [tk(5)], [tk(5)])
                    b.ts('dve', T(5), T(5), -2.0, 1.0, ALU.mult, ALU.add, [tk(5)], [tk(5)])
                    for _ in range(4):
                        b.tt('dve', T(6), T(5), T(5), ALU.mult, [tk(5)], [tk(6)])
                        b.tt('dve', T(7), T(4), T(4), ALU.mult, [tk(4)], [tk(7)])
                        b.stt('dve', T(8), T(5), 2.0, T(4), ALU.mult, ALU.mult, [tk(5), tk(4)], [tk(8)])
                        b.tt('dve', T(5), T(6), T(7), ALU.subtract, [tk(6), tk(7)], [tk(5)])
                        b.copy('dve', T(4), T(8), [tk(8)], [tk(4)])
                    pk_ = ('Pw', d)

                    def P(m, ri):
                        return Pw[:, d, m, ri, :]
                    b.memset('dve', P(0, 0), 1.0, [pk_])
                    b.memset('dve', P(0, 1), 0.0, [pk_])
                    b.tt('dve', P(1, 0), T(3), T(5), ALU.mult, [tk(3), tk(5)], [pk_])
                    b.tt('dve', P(1, 1), T(3), T(4), ALU.mult, [tk(3), tk(4)], [pk_])
                    for m in range(2, 9):
                        cmul('dve', P(m, 0), P(m, 1), P(m - 1, 0), P(m - 1, 1), P(1, 0), P(1, 1), T(6), T(7), tk(6), tk(7), [pk_], [pk_])
                    b.ts('dve', T(6), P(1, 0), -1.0, None, ALU.add, None, [pk_], [tk(6)])
                    b.tt('dve', T(7), ar, ar, ALU.mult, kin, [tk(7)])
                    b.tt('dve', T(8), ai, ai, ALU.mult, kin, [tk(8)])
                    b.tt('dve', T(7), T(7), T(8), ALU.add, [tk(7), tk(8)], [tk(7)])
                    b.recip('dve', T(8), T(7), [tk(7)], [tk(8)])
                    b.tt('dve', T(9), T(6), ar, ALU.mult, [tk(6)] + kin, [tk(9)])
                    b.tt('dve', T(10), P(1, 1), ai, ALU.mult, [pk_] + kin, [tk(10)])
                    b.tt('dve', T(9), T(9), T(10), ALU.add, [tk(9), tk(10)], [tk(9)])
                    b.tt('dve', T(11), T(9), T(8), ALU.mult, [tk(9), tk(8)], [tk(11)])
                    b.tt('dve', T(9), P(1, 1), ar, ALU.mult, [pk_] + kin, [tk(9)])
                    b.tt('dve', T(10), T(6), ai, ALU.mult, [tk(6)] + kin, [tk(10)])
                    b.tt('dve', T(9), T(9), T(10), ALU.subtract, [tk(9), tk(10)], [tk(9)])
                    b.tt('dve', T(12), T(9), T(8), ALU.mult, [tk(9), tk(8)], [tk(12)])
                    t8a = tmp8[:, 0, :].rearrange("p (a b) -> p a b", a=8)
                    t8b = tmp8[:, 1, :].rearrange("p (a b) -> p a b", a=8)
                    cmul('dve', cS[:, d, 0, :, :], cS[:, d, 1, :, :], Pw[:, d, 0:8, 0, :], Pw[:, d, 0:8, 1, :],
                         T(11).unsqueeze(1).to_broadcast([128, 8, 32]), T(12).unsqueeze(1).to_broadcast([128, 8, 32]),
                         t8a, t8b, 'Vt1a', 'Vt1b', [pk_, tk(11), tk(12)], [('cS', d)])
                    b.tt('dve', T(6), T(3), T(3), ALU.mult, [tk(3)], [tk(6)])
                    b.tt('dve', T(6), T(6), T(6), ALU.mult, [tk(6)], [tk(6)])
                    b.tt('dve', rho[:, d, :], T(6), T(6), ALU.mult, [tk(6)], [('rho', d)])
                    b.recip('dve', T(7), rho[:, d, :], [('rho', d)], [tk(7)])

                    def W(k, ri):
                        return wpw[:, k, ri, :]
                    b.tt('dve', W(0, 0), P(8, 0), T(7), ALU.mult, [pk_, tk(7)], ['wpw'])
                    b.stt('dve', W(0, 1), P(8, 1), -1.0, T(7), ALU.mult, ALU.mult, [pk_, tk(7)], ['wpw'])
                    for k in range(8):
                        b.tt('dve', T(8), W(k, 0), W(k, 0), ALU.mult, ['wpw'], [tk(8)])
                        b.tt('dve', T(9), W(k, 1), W(k, 1), ALU.mult, ['wpw'], [tk(9)])
                        b.tt('dve', W(k + 1, 0), T(8), T(9), ALU.subtract, [tk(8), tk(9)], ['wpw'])
                        b.stt('dve', W(k + 1, 1), W(k, 0), 2.0, W(k, 1), ALU.mult, ALU.mult, ['wpw'], ['wpw'])
                    bk = ('Brot', d)
                    b.copy('dve', Brot[:, d, 0, :, 0:1], W(0, 0).unsqueeze(2), ['wpw'], [bk])
                    b.copy('dve', Brot[:, d, 1, :, 0:1], W(0, 1).unsqueeze(2), ['wpw'], [bk])
                    for kk, k in enumerate((1, 2, 4, 8)):
                        ta = tmp8[:, 0, :32 * k].rearrange("p (a b) -> p a b", a=32)
                        tb = tmp8[:, 1, :32 * k].rearrange("p (a b) -> p a b", a=32)
                        cmul('dve', Brot[:, d, 0, :, k:2 * k], Brot[:, d, 1, :, k:2 * k], Brot[:, d, 0, :, 0:k], Brot[:, d, 1, :, 0:k],
                             W(kk, 0).unsqueeze(2).to_broadcast([128, 32, k]), W(kk, 1).unsqueeze(2).to_broadcast([128, 32, k]),
                             ta, tb, 'Vt1a', 'Vt1b', [bk, 'wpw'], [bk])
                    ak = ('Arot', d)
                    b.memset('dve', Arot[:, d, 0, :, 0:1], 1.0, [ak])
                    b.memset('dve', Arot[:, d, 1, :, 0:1], 0.0, [ak])
                    for kk, (k, n) in enumerate(((1, 1), (2, 2), (4, 4), (8, 8), (16, 2))):
                        ta = tmp8[:, 0, :32 * n].rearrange("p (a b) -> p a b", a=32)
                        tb = tmp8[:, 1, :32 * n].rearrange("p (a b) -> p a b", a=32)
                        cmul('dve', Arot[:, d, 0, :, k:k + n], Arot[:, d, 1, :, k:k + n], Arot[:, d, 0, :, 0:n], Arot[:, d, 1, :, 0:n],
                             W(4 + kk, 0).unsqueeze(2).to_broadcast([128, 32, n]), W(4 + kk, 1).unsqueeze(2).to_broadcast([128, 32, n]),
                             ta, tb, 'Vt1a', 'Vt1b', [ak, 'wpw'], [ak])

                discretize(0)
                bg_ops = []
                real_op = S.op
                S.op = lambda eng, fn, reads=(), writes=(): bg_ops.append((eng, fn, reads, writes))
                discretize(1)
                S.op = real_op

                def bg_emit(n):
                    for _ in range(n):
                        if bg_ops:
                            real_op(*bg_ops.pop(0))

                def bd_scatter(dst_fn, src, negate, rk, wk):
                    for half in range(2):
                        ps_ = slice(half * 64, half * 64 + 64)
                        o = dst_fn(ps_, slice(half * 16, half * 16 + 16))
                        if negate:
                            b.act(o, src[ps_], AF.Copy, rk, wk, scale=-1.0)
                        else:
                            b.act(o, src[ps_], AF.Copy, rk, wk)

                def load_bc(kc):
                    kb = kc % 2
                    S.dma('sp', s5b_k[:, kb], s5b[:, :, :, 4 * kc:4 * kc + 4, :], [], [('s5b', kb)])
                    S.dma('sp', s5c_k[:, kb], s5c[:, :, :, 4 * kc:4 * kc + 4, :], [], [('s5c', kb)])

                def gen_tables(d, kc, only_v=False):
                    kb = kc % 2
                    pr = slice(4 * kc, 4 * kc + 4)
                    vr = Vc[:, 0]
                    vi = Vc[:, 1]
                    t1 = Vt1[:, 0]
                    t2 = Vt1[:, 1]
                    crb = cS[:, d, 0, :, pr].rearrange("p d q -> p q d").unsqueeze(3).to_broadcast([128, 4, 8, 16])
                    cib = cS[:, d, 1, :, pr].rearrange("p d q -> p q d").unsqueeze(3).to_broadcast([128, 4, 8, 16])
                    Brb = s5b_k[:, kb, d, 0, :, :].unsqueeze(2).to_broadcast([128, 4, 8, 16])
                    Bib = s5b_k[:, kb, d, 1, :, :].unsqueeze(2).to_broadcast([128, 4, 8, 16])
                    cmul('dve', vr, vi, crb, cib, Brb, Bib, t1, t2, 'Vt1a', 'Vt1b', [('cS', d), ('s5b', kb)], ['Vc'])
                    bd_scatter(lambda p_, c_: Vbd[p_, d, :, :, 0, c_], vr, False, ['Vc'], [('Vbd', d)])
                    bd_scatter(lambda p_, c_: Vbd[p_, d, :, :, 1, c_], vi, False, ['Vc'], [('Vbd', d)])
                    if only_v:
                        return
                    bd_scatter(lambda p_, c_: Cbd[p_, d, :, 0, c_], s5c_k[:, kb, d, 0, :, :], False, [('s5c', kb)], [('Cbd', d)])
                    bd_scatter(lambda p_, c_: Cbd[p_, d, :, 1, c_], s5c_k[:, kb, d, 1, :, :], True, [('s5c', kb)], [('Cbd', d)])

                def gen_E(d, kc):
                    kb = kc % 2
                    pr = slice(4 * kc, 4 * kc + 4)
                    vr = Vc[:, 0]
                    vi = Vc[:, 1]
                    t1 = Vt1[:, 0]
                    t2 = Vt1[:, 1]
                    prb = Pw[:, d, 1:9, 0, pr].rearrange("p m q -> p q m").unsqueeze(3).to_broadcast([128, 4, 8, 16])
                    pib = Pw[:, d, 1:9, 1, pr].rearrange("p m q -> p q m").unsqueeze(3).to_broadcast([128, 4, 8, 16])
                    Crb = s5c_k[:, kb, d, 0, :, :].unsqueeze(2).to_broadcast([128, 4, 8, 16])
                    Cib = s5c_k[:, kb, d, 1, :, :].unsqueeze(2).to_broadcast([128, 4, 8, 16])
                    cmul('dve', vr, vi, prb, pib, Crb, Cib, t1, t2, 'Vt1a', 'Vt1b', [('Pw', d), ('s5c', kb)], ['Vc'])
                    bd_scatter(lambda p_, c_: Ebd[p_, d, :, :, 0, c_], vr, False, ['Vc'], [('Ebd', d)])
                    bd_scatter(lambda p_, c_: Ebd[p_, d, :, :, 1, c_], vi, True, ['Vc'], [('Ebd', d)])

                PTb = PS[6][:].bitcast(BF16)
                pcnt = [0]

                kin4_p = [hT[:, p_, 0:256].bitcast(BF16).rearrange("p (a n) -> p a n", a=4) for p_ in range(4)]
                for p_ in range(4):
                    b.memset('pool', hT[:, p_, 0:256], 0.0, [('hT', 0, p_), ('Kin4', p_ // 2)])

                def kin4(d, dl):
                    return kin4_p[d * 2 + dl // 4][:, dl % 4, :]

                def kin_prologue(kc):
                    for d in range(2):
                        for q in range(4):
                            rows = slice(32 * q, 32 * q + 32)

                            def fnk(e, d=d, q=q, rows=rows):
                                ins = None
                                for dl in range(8):
                                    o = PS[7][rows, dl * 32:(dl + 1) * 32]
                                    e.matmul(o, Vbd[:, d, q, dl, 0, :], Cbd[:, d, q, 0, :], start=True, stop=False, tile_position=(0, 32 * q))
                                    ins = e.matmul(o, Vbd[:, d, q, dl, 1, :], Cbd[:, d, q, 1, :], start=False, stop=True, tile_position=(0, 32 * q))
                                return ins
                            S.op('pe', fnk, [('Vbd', d), ('Cbd', d)], [pk(7)])
                        for q in range(4):
                            rows = slice(32 * q, 32 * q + 32)
                            for hf in range(2):
                                b.copy('act', kin4_p[d * 2 + hf][rows, :, 32 * q:32 * q + 32],
                                       PS[7][rows, hf * 128:hf * 128 + 128].rearrange("p (a n) -> p a n", a=4), [pk(7)], [('Kin4', d)])

                def intra_prologue(kc):
                    ukeys = [('ubuf', gi, kc) for gi in range(5)]

                    def fni(e):
                        ins = None
                        for d in range(2):
                            for j in range(8):
                                o = PS[j // 2][:, (j % 2) * 256:(j % 2) * 256 + 256]
                                js = list(range(0, j + 1)) if d == 0 else list(range(j, 8))
                                for n_, jp in enumerate(js):
                                    ins = e.matmul(o, kin4(d, abs(j - jp)), ubuf[:, kc, jp * 288 + 32:jp * 288 + 288],
                                                   start=(d == 0 and j % 2 == 0 and n_ == 0), stop=False)
                        return ins
                    S.op('pe', fni, [('Kin4', 0), ('Kin4', 1)] + ukeys, [pk(0), pk(1), pk(2), pk(3)])

                def u_geom(d, kc, q):
                    return 4 * kc + q, slice(32 * q, 32 * q + 32), (288 if d == 0 else 256), (0 if d == 0 else NCTX)

                def u_front(d, kc, q, iu, need_out):
                    pair, rows, C, col0 = u_geom(d, kc, q)
                    wb = iu % 2
                    for half in range(2):
                        def fn(e, half=half):
                            ins = None
                            for dl in range(4):
                                for ri in range(2):
                                    idx = dl * 2 + ri
                                    ins = e.transpose(PTb[rows, idx * 128:(idx + 1) * 128], Vbd[:, d, q, half * 4 + dl, ri, :], idb[:],
                                                      tile_position=(0, 32 * q))
                            return ins
                        S.op('pe', fn, [('Vbd', d), 'idb'], [pk(6)])
                        b.copy('act', Wst[rows, wb, half * 4:half * 4 + 4, :, :],
                               PTb[rows, :].rearrange("p (a r n) -> p a r n", a=4, r=2), [pk(6)], [('Wst', wb)])

                def u_rot(d, kc, q, iu):
                    pair, rows, C, col0 = u_geom(d, kc, q)
                    rb = iu % 2
                    na = C // 16
                    rv = lambda ri: rot[:, rb, ri, :C].rearrange("p (a b) -> p a b", b=16)
                    cmul('pool', rv(0), rv(1),
                         Arot[:, d, 0, pair, 0:na].unsqueeze(2).to_broadcast([128, na, 16]), Arot[:, d, 1, pair, 0:na].unsqueeze(2).to_broadcast([128, na, 16]),
                         Brot[:, d, 0, pair, :].unsqueeze(1).to_broadcast([128, na, 16]), Brot[:, d, 1, pair, :].unsqueeze(1).to_broadcast([128, na, 16]),
                         rt2[:, 1, 0, :C].rearrange("p (a b) -> p a b", b=16), rt2[:, 1, 1, :C].rearrange("p (a b) -> p a b", b=16),
                         ('rt2a', 'pool'), ('rt2b', 'pool'), [('Arot', d), ('Brot', d)], [('rot', rb)])

                def u_states(d, kc, q, iu):
                    pair, rows, C, col0 = u_geom(d, kc, q)
                    wb = iu % 2
                    ukeys = [('ubuf', gi, kc) for gi in range(5)]
                    for ri in range(2):
                        def fns(e, ri=ri):
                            ins = None
                            for j in range(8):
                                dl = (7 - j) if d == 0 else j
                                ins = e.matmul(PS[4 + ri][:, :C], Wst[rows, wb, dl, ri, :], ubuf[rows, kc, j * 288 + col0 // 8:j * 288 + col0 // 8 + C],
                                               start=(j == 0), stop=(j == 7), tile_position=(32 * q, 0))
                            return ins
                        S.op('pe', fns, [('Wst', wb)] + ukeys, [pk(4 + ri)])

                def u_scan(d, kc, q, iu):
                    pair, rows, C, col0 = u_geom(d, kc, q)
                    wb = iu % 2
                    if d == 0:
                        Sr, Si = PS[4][:, :C], PS[5][:, :C]
                    else:
                        Sr, Si = PS[4][:, C - 1::-1], PS[5][:, C - 1::-1]
                    R0, R1 = rot[:, wb, 0, :C], rot[:, wb, 1, :C]
                    tr, ti = trti[:, 0, 0, :C], trti[:, 0, 1, :C]
                    ta, tb = gg[:, wb, 0, :C], gg[:, wb, 1, :C]
                    kr_ = [pk(4), pk(5), ('rot', wb)]
                    b.tt('dve', ta, Sr, R0, ALU.mult, kr_, [('gg', wb, 0)])
                    b.tt('dve', tb, Si, R1, ALU.mult, kr_, [('gg', wb, 1)])
                    b.tt('dve', tr, ta, tb, ALU.subtract, [('gg', wb, 0), ('gg', wb, 1)], [('trti', 0)])
                    b.tt('dve', ta, Si, R0, ALU.mult, kr_, [('gg', wb, 0)])
                    b.tt('dve', tb, Sr, R1, ALU.mult, kr_, [('gg', wb, 1)])
                    b.tt('dve', ti, ta, tb, ALU.add, [('gg', wb, 0), ('gg', wb, 1)], [('trti', 1)])
                    rho_b = rho[:, d, pair:pair + 1].to_broadcast([128, C])
                    if d == 0:
                        i_r, i_i, ik = 0.0, 0.0, []
                    else:
                        i_r, i_i, ik = rin_s[:, pair, 0:1], rin_s[:, pair, 1:2], ['rin']
                    gr, gi_ = gg[:, wb, 0, :C], gg[:, wb, 1, :C]
                    S.op('dve', lambda e: e.tensor_tensor_scan(gr, rho_b, tr, i_r, ALU.mult, ALU.add),
                         [('rho', d), ('trti', 0)] + ik, [('gg', wb, 0)])
                    S.op('dve', lambda e: e.tensor_tensor_scan(gi_, rho_b, ti, i_i, ALU.mult, ALU.add),
                         [('rho', d), ('trti', 1)] + ik, [('gg', wb, 1)])

                def u_final(d, kc, q, iu):
                    pair, rows, C, col0 = u_geom(d, kc, q)
                    wb = iu % 2
                    R0, R1 = rot[:, wb, 0, :C], rot[:, wb, 1, :C]
                    gr, gi_ = gg[:, wb, 0, :C], gg[:, wb, 1, :C]
                    L = slice(C - 1, C)
                    b.tt('dve', T(13)[:, 0:1], R0[:, L], gr[:, L], ALU.mult, [('rot', wb), ('gg', wb, 0)], [tk(13)])
                    b.tt('dve', T(13)[:, 1:2], R1[:, L], gi_[:, L], ALU.mult, [('rot', wb), ('gg', wb, 1)], [tk(13)])
                    b.tt('dve', fx_s[:, pair, 0:1], T(13)[:, 0:1], T(13)[:, 1:2], ALU.add, [tk(13)], ['fx'])
                    b.tt('dve', T(13)[:, 2:3], R0[:, L], gi_[:, L], ALU.mult, [('rot', wb), ('gg', wb, 1)], [tk(13)])
                    b.tt('dve', T(13)[:, 3:4], R1[:, L], gr[:, L], ALU.mult, [('rot', wb), ('gg', wb, 0)], [tk(13)])
                    b.tt('dve', fx_s[:, pair, 1:2], T(13)[:, 2:3], T(13)[:, 3:4], ALU.subtract, [tk(13)], ['fx'])

                def u_unrot(d, kc, q, iu):
                    pair, rows, C, col0 = u_geom(d, kc, q)
                    wb = iu % 2
                    hb = (iu // 2) % 2
                    R0, R1 = rot[:, wb, 0, :C], rot[:, wb, 1, :C]
                    gr, gi_ = gg[:, wb, 0, :C], gg[:, wb, 1, :C]
                    if d == 0:
                        o_r, o_i = Hp[:, d, hb, 0, 1:C], Hp[:, d, hb, 1, 1:C]
                    else:
                        o_r, o_i = Hp[:, d, hb, 0, C - 2::-1], Hp[:, d, hb, 1, C - 2::-1]
                    n1 = C - 1
                    hk = ('Hp', d, hb)
                    e2 = 'dve' if d == 0 else 'pool'
                    ka, kb_ = ('rt2a', e2), ('rt2b', e2)
                    r2 = rt2[:, 0 if d == 0 else 1]
                    b.tt(e2, r2[:, 0, :n1], R0[:, :n1], gr[:, :n1], ALU.mult, [('rot', wb), ('gg', wb, 0)], [ka])
                    b.tt(e2, r2[:, 1, :n1], R1[:, :n1], gi_[:, :n1], ALU.mult, [('rot', wb), ('gg', wb, 1)], [kb_])
                    b.tt(e2, o_r, r2[:, 0, :n1], r2[:, 1, :n1], ALU.add, [ka, kb_], [hk])
                    b.tt(e2, r2[:, 0, :n1], R0[:, :n1], gi_[:, :n1], ALU.mult, [('rot', wb), ('gg', wb, 1)], [ka])
                    b.tt(e2, r2[:, 1, :n1], R1[:, :n1], gr[:, :n1], ALU.mult, [('rot', wb), ('gg', wb, 0)], [kb_])
                    b.tt(e2, o_i, r2[:, 0, :n1], r2[:, 1, :n1], ALU.subtract, [ka, kb_], [hk])
                    if d == 1:
                        b.copy('pool', Hp[:, d, hb, 0, C - 1:C], rin_s[:, pair, 0:1], ['rin'], [hk])
                        b.copy('pool', Hp[:, d, hb, 1, C - 1:C], rin_s[:, pair, 1:2], ['rin'], [hk])

                def u_out(d, kc, q, iu, first_dir, last_dir):
                    pair, rows, C, col0 = u_geom(d, kc, q)
                    wb = iu % 2
                    hb = (iu // 2) % 2
                    hk = ('Hp', d, hb)
                    hoff = 32 if d == 0 else 0
                    ukeys = [('ubuf', gi, kc) for gi in range(5)]

                    def fno(e):
                        ins = None
                        for j in range(8):
                            o = PS[j // 2][rows, (j % 2) * 256:(j % 2) * 256 + 256]
                            mi = j if d == 0 else 7 - j
                            e.matmul(o, Ebd[:, d, q, mi, 0, :], Hp[:, d, hb, 0, hoff:hoff + 256], start=False, stop=False, tile_position=(0, 32 * q))
                            ins = e.matmul(o, Ebd[:, d, q, mi, 1, :], Hp[:, d, hb, 1, hoff:hoff + 256], start=False,
                                           stop=(last_dir and q == 3), tile_position=(0, 32 * q))
                        return ins
                    S.op('pe', fno, [('Ebd', d), hk] + ukeys, [pk(0), pk(1), pk(2), pk(3)])

                unitsA = [(0, kc, q) for kc in range(KC) for q in range(4)]
                load_bc(0)
                gen_tables(0, 0, only_v=True)
                u_front(*unitsA[0], 0, False)
                u_rot(*unitsA[0], 0)
                u_states(*unitsA[0], 0)
                for i, u in enumerate(unitsA):
                    if i + 1 < len(unitsA):
                        un = unitsA[i + 1]
                        if un[1] != u[1]:
                            load_bc(un[1])
                            gen_tables(0, un[1], only_v=True)
                        u_front(*un, i + 1, False)
                        u_rot(*un, i + 1)
                    u_scan(*u, i)
                    bg_emit(4)
                    if i + 1 < len(unitsA):
                        u_states(*unitsA[i + 1], i + 1)
                    u_final(*u, i)
                    bg_emit(4)
                bg_emit(len(bg_ops))
                fxf = fx_s[:].rearrange("p a b -> p (a b)")
                for sl in range(2):
                    b.ts('dve', ccb[:, sl, :], fxf, pm_s[:, sl:sl + 1], None, ALU.mult, None, ['fx', 'pmask'], ['ccb'])
                S.dma('pool', cc_in.ap().opt() if False else cc_in[:, :], ccb[:].rearrange("p a b -> p (a b)"), ['ccb'], ['cc_in'])
                S.collective(lambda e: e.collective_compute("AllReduce", ALU.add, replica_groups=[[0, 1], [2, 3], [4, 5], [6, 7]],
                                                            ins=[cc_in.ap().opt()], outs=[cc_out.ap().opt()]), ['cc_in'], ['cc_out'])
                S.dma('sp', ccb2[:].rearrange("p a b -> p (a b)"), cc_out[:, :], ['cc_out'], ['ccb2'])

                def recv_rin():
                    rinf = rin_s[:].rearrange("p a b -> p (a b)")
                    b.ts('dve', rinf, ccb2[:, 0, :], pm_s[:, 1:2], None, ALU.mult, None, ['ccb2', 'pmask'], ['rin'])
                    b.stt('dve', rinf, ccb2[:, 1, :], pm_s[:, 0:1], rinf, ALU.mult, ALU.add, ['ccb2', 'pmask', 'rin'], ['rin'])

                unitsB = [(d, kc, q) for kc in range(KC) for q in range(4) for d in range(2)]

                def z_step(kc):
                    hv = hT[:, kc, NCTX:NQ].rearrange("p (c j) -> p j c", j=8)
                    zv = ubuf[:, kc, NCTX:NQ].rearrange("p (c j) -> p j c", j=8)
                    for bk_ in range(4):
                        zb = 0
                        b.stt('dve', t32z[:, zb, :, :], hv[:, 2 * bk_:2 * bk_ + 2, :], dAB[:, 0, kc:kc + 1],
                              PS[bk_][:, :].rearrange("p (j c) -> p j c", j=2), ALU.mult, ALU.add,
                              [pk(bk_), 'dAB'] + [('hT', gi, kc) for gi in range(1, 5)], [('t32z', zb)])
                        if stop_after == 'p5':
                            b.act(hv[:, 2 * bk_:2 * bk_ + 2, :], t32z[:, zb, :, :], AF.Identity, [('t32z', zb), 'dAB'], [('hT', gi, kc) for gi in range(1, 5)],
                                  bias=dAB[:, 1, kc:kc + 1])
                        else:
                            b.act(zv[:, 2 * bk_:2 * bk_ + 2, :], t32z[:, zb, :, :], AF.Gelu_apprx_tanh, [('t32z', zb), 'dAB'], [('ubuf', gi, kc) for gi in range(1, 5)],
                                  bias=dAB[:, 1, kc:kc + 1])

                load_bc(0)
                for d_ in range(2):
                    gen_tables(d_, 0)
                    gen_E(d_, 0)
                kin_prologue(0)
                intra_prologue(0)
                u_front(*unitsB[0], 0, True)
                u_rot(*unitsB[0], 0)
                u_states(*unitsB[0], 0)
                for i, u in enumerate(unitsB):
                    nxt_kc = None
                    if i + 1 < len(unitsB):
                        un = unitsB[i + 1]
                        if un[1] != u[1]:
                            nxt_kc = un[1]
                            load_bc(nxt_kc)
                            gen_tables(0, nxt_kc)
                            gen_tables(1, nxt_kc)
                            kin_prologue(nxt_kc)
                        u_front(*un, i + 1, True)
                        u_rot(*un, i + 1)
                    if i == 1:
                        recv_rin()
                    u_scan(*u, i)
                    if i + 1 < len(unitsB):
                        u_states(*unitsB[i + 1], i + 1)
                    u_unrot(*u, i)
                    u_out(*u, i, u[0] == 0, u[0] == 1)
                    if u[0] == 1 and u[2] == 3:
                        z_step(u[1])
                    if nxt_kc is not None:
                        intra_prologue(nxt_kc)
                        gen_E(0, nxt_kc)
                        gen_E(1, nxt_kc)
                S.flush()

            p6 = ExitStack()
            if stop_after != 'p5':
                wga = sb("wga", [128, KC, 1024], BF16, p6)
                wgb = sb("wgb", [128, KC, 1024], BF16, p6)
                sgb = sb("sgb", [128, 2, 512], F32, p6)
                ogb = sb("ogb", [128, 2, 512], F32, p6)
                lntmp = ln_tmps(p6, "b6")
                S.dma('pool', wga[:], glu_a.rearrange("(kc p) n -> p kc n", p=128), [], ['wga'])
                S.dma('pool', wgb[:], glu_b.rearrange("(kc p) n -> p kc n", p=128), [], ['wgb'])
                cnt6 = [0]

                def glu6(ti):
                    (c0, N, s) = own_groups[ti]
                    gi = own_gidx[ti]
                    zkeys = [('ubuf', gi, kc) for kc in range(KC)]
                    for oc in range(KC):
                        pa = (cnt6[0] % 2) * 2
                        q6 = cnt6[0] % 2
                        cnt6[0] += 1
                        b.mm([(PS[pa][:, :N], wga[:, kc, oc * 128:(oc + 1) * 128], ubuf[:, kc, c0:c0 + N], kc == 0, kc == KC - 1) for kc in range(KC)],
                             ['wga'] + zkeys, [pk(pa)])
                        b.mm([(PS[pa + 1][:, :N], wgb[:, kc, oc * 128:(oc + 1) * 128], ubuf[:, kc, c0:c0 + N], kc == 0, kc == KC - 1) for kc in range(KC)],
                             ['wgb'] + zkeys, [pk(pa + 1)])
                        b.act(sgb[:, q6, :N], PS[pa + 1][:, :N], AF.Sigmoid, [pk(pa + 1)], [('sgb', q6)])
                        b.stt('dve', ogb[:, q6, :N], PS[pa][:, :N], pvs(('gam', 1), oc, 0), sgb[:, q6, :N], ALU.mult, ALU.mult,
                              [pk(pa), 'pv', ('sgb', q6)], [('ogb', q6)])
                        b.tt('pool', hT[:, oc, c0:c0 + N], ogb[:, q6, :N], hT[:, oc, c0:c0 + N], ALU.add, [('ogb', q6), ('hT', gi, oc)], [('hT', gi, oc)])
                glu6(0)
                for ti, (c0, N, s) in enumerate(own_groups):
                    gi = own_gidx[ti]
                    if ti + 1 < len(own_groups):
                        glu6(ti + 1)
                    ln_norm(lambda kc, c0=c0, N=N: hT[:, kc, c0:c0 + N], N, 0, [('hT', gi, kc) for kc in range(KC)],
                            g1=lambda kc: lng_s[:, 2, kc:kc + 1], b1=lambda kc: lnb_s[:, 2, kc:kc + 1],
                            out1_fn=lambda kc, c0=c0, N=N: hT[:, kc, c0:c0 + N], out1_keys=[('hT', gi, kc) for kc in range(KC)],
                            g2=lambda kc: pvs(('gU', 1, 0), kc, 0), b2=lambda kc: pvs(('bU', 1, 0), kc, 0),
                            out2_fn=lambda kc, c0=c0, N=N: ubuf[:, kc, c0:c0 + N], out2_keys=[('ubuf', gi, kc) for kc in range(KC)],
                            tmp=lntmp, psA=4, psB=5)
                S.flush()
            p6.close()

        if lvl >= 2:
            with ExitStack() as p7:
                combT = sb("combT", [8, NOWN], F32, p7)
                cbt = sb("cbt", [128, NOWN], F32, p7)
                sel_s = sb("sel_s", [8, 8, 128], F32, p7)
                S.dma('sp', sel_s[:], sel[:], [], ['sel'])
                with ExitStack() as p7a:
                    wr_s = sb("wr_s", [128, KC, 8], F32, p7a)
                    idf = sb("idf", [128, 128], F32, p7a)
                    u32 = sb("u32", [128, 2, KC, 128], F32, p7a)
                    lg = sb("lg", [128, 2, 8], F32, p7a)
                    mx = sb("mx", [128, 2, 8], F32, p7a)
                    msk = sb("msk", [128, 2, 8], F32, p7a)
                    ex = sb("ex", [128, 2, 8], F32, p7a)
                    den = sb("den", [128, 2, 2], F32, p7a)
                    cmb = sb("cmb", [128, 2, 8], F32, p7a)
                    S.dma('sp', wr_s[:], wrt[:], [], ['wr'])
                    S.dma('sp', idf[:], identf[:], [], ['idf'])
                    for tp in range(8):
                        tiles = [(2 * tp + tb, tb) for tb in range(2)]
                        for (t, tb) in tiles:
                            c0 = NCTX + 128 * t
                            gi = 1 + t // 4
                            for kc in range(KC):
                                eng = 'dve' if kc % 2 == 0 else 'pool'
                                b.ts(eng, u32[:, tb, kc, :], hT[:, kc, c0:c0 + 128], pvs(('s2p', 1), kc, 0), modv(1, 3, kc, 0), ALU.mult, ALU.add,
                                     [('hT', gi, kc), 'pv', ('modT', 1)], [('u32', tb, kc)])
                        for (t, tb) in tiles:
                            b.mm([(PS[tb][:, 0:8], u32[:, tb, kc, :], wr_s[:, kc, :], kc == 0, kc == KC - 1) for kc in range(KC)],
                                 ['wr'] + [('u32', tb, kc) for kc in range(KC)], [pk(tb)])
                        for (t, tb) in tiles:
                            b.copy('act', lg[:, tb, :], PS[tb][:, 0:8], [pk(tb)], [('lg', tb)])
                        for (t, tb) in tiles:
                            S.op('dve', lambda e, tb=tb: e.max(mx[:, tb, :], lg[:, tb, :]), [('lg', tb)], [('mx', tb)])
                        for (t, tb) in tiles:
                            b.ts('dve', msk[:, tb, :], lg[:, tb, :], mx[:, tb, 1:2], None, ALU.is_ge, None, [('lg', tb), ('mx', tb)], [('msk', tb)])
                        for (t, tb) in tiles:
                            b.ts('dve', ex[:, tb, :], lg[:, tb, :], mx[:, tb, 0:1], None, ALU.subtract, None, [('lg', tb), ('mx', tb)], [('ex', tb)])
                        for (t, tb) in tiles:
                            b.act(ex[:, tb, :], ex[:, tb, :], AF.Exp, [('ex', tb)], [('ex', tb)])
                        for (t, tb) in tiles:
                            b.tt('dve', ex[:, tb, :], ex[:, tb, :], msk[:, tb, :], ALU.mult, [('ex', tb), ('msk', tb)], [('ex', tb)])
                        for (t, tb) in tiles:
                            S.op('dve', lambda e, tb=tb: e.reduce_sum(den[:, tb, 0:1], ex[:, tb, :], axis=mybir.AxisListType.X), [('ex', tb)], [('den', tb)])
                        for (t, tb) in tiles:
                            b.recip('dve', den[:, tb, 1:2], den[:, tb, 0:1], [('den', tb)], [('den2', tb)])
                        for (t, tb) in tiles:
                            b.ts('dve', cmb[:, tb, :], ex[:, tb, :], den[:, tb, 1:2], None, ALU.mult, None, [('ex', tb), ('den2', tb)], [('cmb', tb)])
                        for (t, tb) in tiles:
                            S.op('pe', lambda e, tb=tb: e.transpose(PS[2 + tb][0:8, 0:128], cmb[:, tb, :], idf[:]), [('cmb', tb), 'idf'], [pk(2 + tb)])
                        for (t, tb) in tiles:
                            b.copy('act', combT[:, t * 128:(t + 1) * 128], PS[2 + tb][0:8, 0:128], [pk(2 + tb)], ['combT'])
                    S.flush()
                p7b = ExitStack()
                if stop_after != 'p7a':
                    w1s = sb("w1m", [128, 3, KC, 256], BF16, p7b)
                    w3s = sb("w3m", [128, 3, KC, 256], BF16, p7b)
                    w2s = sb("w2m", [128, 3, 2, 1024], BF16, p7b)
                    s1b = sb("s1m", [128, 2, 512], F32, p7b)
                    t32 = sb("t32m", [128, 2, 512], F32, p7b)
                    gT = sb("gTm", [128, 2, 2, 512], BF16, p7b)

                    def cb_prepare(e):
                        for tg in range(4):
                            pb = 4 + tg
                            b.mm([(PS[pb][:, :512], sel_s[:, e, :], combT[:, tg * 512:(tg + 1) * 512], True, True)], ['sel', 'combT'], [pk(pb)])
                            b.copy('act', cbt[:, tg * 512:(tg + 1) * 512], PS[pb][:, :512], [pk(pb)], ['cbt'])
                    experts = [(moe_w1[e], moe_w3[e], moe_w2[e], DFE, e) for e in range(NEXP)]
                    swiglu_stages(nc, S, b, PS, pk, own_groups, own_gidx, ubuf, hT, experts, (w1s, w3s, w2s, s1b, gT, t32),
                                  lambda oc, s: pvs(('gaf', 1), oc, 0), NF=2, cb=(cb_prepare, cbt), NWB=3)
                    S.flush()
                p7b.close()
                p7c = ExitStack()
                if stop_after not in ('p7a', 'p7b'):
                    lntmp = ln_tmps(p7c, "b7")
                    for ti, (c0, N, s) in enumerate(own_groups):
                        gi = own_gidx[ti]
                        ln_norm(lambda kc, c0=c0, N=N: hT[:, kc, c0:c0 + N], N, 0, [('hT', gi, kc) for kc in range(KC)],
                                g1=lambda kc: lng_s[:, 3, kc:kc + 1], b1=lambda kc: lnb_s[:, 3, kc:kc + 1],
                                out1_fn=lambda kc, c0=c0, N=N: hT[:, kc, c0:c0 + N], out1_keys=[('hT', gi, kc) for kc in range(KC)],
                                tmp=lntmp)
                    S.flush()
                p7c.close()


        toks = []
        if debug:
            toks.append(S.dma('sp', dbg[:], hT[:], [('hT', gi, kc) for gi in range(5) for kc in range(KC)], []))
        toks.append(S.dma('sp', yout[:], hT[:, :, NCTX:NQ], [('hT', gi, kc) for gi in range(5) for kc in range(KC)], []))
        S.flush(final_tokens=toks)
    return nc


def swiglu_stages(nc, S, b, PS, pk, tgroups, gidx, ubuf, hT, experts, bufs, gate_fn, NF=4, cb=None, NWB=2):
    w1s, w3s, w2s, s1b, gT, t32 = bufs
    stages = []
    for (w1, w3, w2, F, e) in experts:
        nfc = F // 128
        f0 = 0
        first = True
        while f0 < nfc:
            nf = min(NF, nfc - f0)
            stages.append((w1, w3, w2, f0, nf, e, first))
            first = False
            f0 += nf

    def load(si):
        (w1, w3, w2, f0, nf, e, first) = stages[si]
        q = si % NWB
        c0f = f0 * 128
        S.dma('pool', w1s[:, q, :, :nf * 128], w1[:, c0f:c0f + nf * 128].rearrange("(kc p) n -> p kc n", p=128), [], [('w1s', q)])
        S.dma('pool', w3s[:, q, :, :nf * 128], w3[:, c0f:c0f + nf * 128].rearrange("(kc p) n -> p kc n", p=128), [], [('w3s', q)])
        S.dma('pool', w2s[:, q, :nf, :], w2[c0f:c0f + nf * 128, :].rearrange("(fc p) n -> p fc n", p=128), [], [('w2s', q)])

    items = [(si, ti) for si in range(len(stages)) for ti in range(len(tgroups))]
    hc = [0]

    def H(i):
        si, ti = items[i]
        (w1, w3, w2, f0, nf, e, first) = stages[si]
        q = si % NWB
        if ti == 0:
            if si == 0:
                for k in range(min(NWB - 1, len(stages))):
                    load(k)
            if cb is not None and first:
                cb[0](e)
        (c0, N, s) = tgroups[ti]
        gi = gidx[ti]
        gq_ = i % 2
        ukeys = [('ubuf', gi, kc) for kc in range(KC)]
        for fc in range(nf):
            p1 = (hc[0] % 2) * 2
            sq_ = hc[0] % 2
            hc[0] += 1
            b.mm([(PS[p1][:, :N], w1s[:, q, kc, fc * 128:(fc + 1) * 128], ubuf[:, kc, c0:c0 + N], kc == 0, kc == KC - 1) for kc in range(KC)],
                 [('w1s', q)] + ukeys, [pk(p1)])
            b.mm([(PS[p1 + 1][:, :N], w3s[:, q, kc, fc * 128:(fc + 1) * 128], ubuf[:, kc, c0:c0 + N], kc == 0, kc == KC - 1) for kc in range(KC)],
                 [('w3s', q)] + ukeys, [pk(p1 + 1)])
            b.act(s1b[:, sq_, :N], PS[p1][:, :N], AF.Silu, [pk(p1)], [('s1b', sq_)])
            if cb is None:
                b.tt('dve', gT[:, gq_, fc, :N], PS[p1 + 1][:, :N], s1b[:, sq_, :N], ALU.mult, [pk(p1 + 1), ('s1b', sq_)], [('gT', gq_, fc)])
            else:
                b.tt('dve', t32[:, sq_, :N], PS[p1 + 1][:, :N], s1b[:, sq_, :N], ALU.mult, [pk(p1 + 1), ('s1b', sq_)], [('t32', sq_)])
                b.tt('pool', gT[:, gq_, fc, :N], t32[:, sq_, :N], cb[1][:, ti * 512:ti * 512 + N], ALU.mult,
                     [('t32', sq_), 'cbt'], [('gT', gq_, fc)])

    def W2(i):
        si, ti = items[i]
        (w1, w3, w2, f0, nf, e, first) = stages[si]
        q = si % NWB
        (c0, N, s) = tgroups[ti]
        gi = gidx[ti]
        gq_ = i % 2
        if ti == 0 and si + NWB - 1 < len(stages):
            load(si + NWB - 1)
        for oc in range(KC):
            pb = 4 + (oc % 4)
            b.mm([(PS[pb][:, :N], w2s[:, q, fc, oc * 128:(oc + 1) * 128], gT[:, gq_, fc, :N], fc == 0, fc == nf - 1) for fc in range(nf)],
                 [('w2s', q)] + [('gT', gq_, fc) for fc in range(nf)], [pk(pb)])
            b.stt('dve', hT[:, oc, c0:c0 + N], PS[pb][:, :N], gate_fn(oc, s), hT[:, oc, c0:c0 + N], ALU.mult, ALU.add,
                  [pk(pb), 'pv', ('hT', gi, oc)], [('hT', gi, oc)])

    H(0)
    for i in range(len(items)):
        if i + 1 < len(items):
            H(i + 1)
        W2(i)


def _fm(a):
    n, d = a.shape
    return np.ascontiguousarray(a.reshape(n, d // 128, 128).transpose(2, 1, 0))


def _vec(a):
    return np.ascontiguousarray(a.reshape(-1, 128).T)


def _rope_tables(tok_idx):
    pairs = 16
    freqs = (10000.0 ** (-np.arange(pairs, dtype=np.float32) / pairs)).astype(np.float32)
    row = (tok_idx // 64).astype(np.float32)
    col = (tok_idx % 64).astype(np.float32)
    ang = np.stack([row[:, None] * freqs, col[:, None] * freqs], axis=1).astype(np.float32)
    c = np.cos(ang).astype(np.float32)
    s = np.sin(ang).astype(np.float32)
    n = len(tok_idx)
    cos_t = np.zeros((64, n), np.float32)
    sin_t = np.zeros((64, n), np.float32)
    for ax in range(2):
        for half in range(2):
            d0 = ax * 32 + half * 16
            cos_t[d0:d0 + 16] = c[:, ax, :].T
            sin_t[d0:d0 + 16] = (-s[:, ax, :].T) if half == 0 else s[:, ax, :].T
    return cos_t, sin_t


_SWAP = np.array([ax * 32 + (1 - half) * 16 + p for ax in range(2) for half in range(2) for p in range(16)])


def make_in_maps(inp):
    f = lambda k: np.asarray(inp[k], dtype=np.float32)
    x, c, ctx, c_ctx = f("x"), f("c"), f("ctx"), f("c_ctx")
    shared = {}
    shared["ada_w"] = np.ascontiguousarray(f("ada_w"))
    ab = f("ada_b")
    shared["adab"] = np.ascontiguousarray(ab.reshape(2, 48, 128).transpose(2, 0, 1))
    shared["lng"] = np.ascontiguousarray(f("ln_g").reshape(4, KC, 128).transpose(2, 0, 1))
    shared["lnb"] = np.ascontiguousarray(f("ln_b").reshape(4, KC, 128).transpose(2, 0, 1))
    wd = f("mla_w_down")[0]
    wd_ext = np.concatenate([wd, wd[:, 640 + _SWAP]], axis=1)
    shared["wdown"] = np.ascontiguousarray(wd_ext.reshape(KC, 128, 768).transpose(1, 0, 2))
    shared["gq"] = _vec(f("mla_g_q")[0])
    shared["gkv"] = _vec(f("mla_g_kv")[0])
    wq = f("mla_w_uq")[0]
    wq_ext = np.concatenate([wq, wq[:, :, 128 + _SWAP]], axis=2)
    shared["wuq"] = np.ascontiguousarray(wq_ext.reshape(3, 128, 8 * 256).transpose(1, 0, 2))
    shared["wuk"] = np.ascontiguousarray(f("mla_w_uk")[0].reshape(2, 128, 1024).transpose(1, 0, 2))
    shared["wuv"] = np.ascontiguousarray(f("mla_w_uv")[0].reshape(2, 128, 1024).transpose(1, 0, 2))
    shared["wo"] = np.ascontiguousarray(f("mla_w_o")[0].reshape(8, 128, 1024).transpose(1, 0, 2))
    shared["ffn_w1"] = np.ascontiguousarray(f("ffn_w1")[0])
    shared["ffn_w3"] = np.ascontiguousarray(f("ffn_w3")[0])
    shared["ffn_w2"] = np.ascontiguousarray(f("ffn_w2")[0])
    shared["glu_a"] = np.ascontiguousarray(f("s5_w_glu_a")[0])
    shared["glu_b"] = np.ascontiguousarray(f("s5_w_glu_b")[0])
    shared["s5d"] = _vec(f("s5_d")[0])
    shared["ident"] = np.eye(128, dtype=np.float32)
    shared["identf"] = np.eye(128, dtype=np.float32)
    shared["wrt"] = np.ascontiguousarray(f("moe_w_router")[0].reshape(KC, 128, NEXP).transpose(1, 0, 2))
    selm = np.zeros((8, 8, 128), np.float32)
    for e in range(8):
        selm[e, e, :] = 1.0
    shared["sel"] = selm
    shared["moe_w1"] = np.ascontiguousarray(f("moe_w1")[0])
    shared["moe_w3"] = np.ascontiguousarray(f("moe_w3")[0])
    shared["moe_w2"] = np.ascontiguousarray(f("moe_w2")[0])
    a_re, a_im, lstep = f("s5_a_re")[0], f("s5_a_im")[0], f("s5_log_step")[0]
    b_re, b_im, c_re, c_im = f("s5_b_re")[0], f("s5_b_im")[0], f("s5_c_re")[0], f("s5_c_im")[0]

    def gp(a2):
        return a2.reshape(32, 2, 64).transpose(1, 2, 0).reshape(128, 32)

    def gpb(b4):
        return b4.reshape(32, 2, 64, 16).transpose(1, 2, 0, 3).reshape(128, 32, 16)

    def gpc(c4):
        return c4.reshape(32, 2, 16, 64).transpose(1, 3, 0, 2).reshape(128, 32, 16)
    s5_dir = []
    for d in range(2):
        ls = np.broadcast_to(lstep[d].reshape(32, 2).T[:, None, :], (2, 64, 32)).reshape(128, 32)
        s5_dir.append(dict(a=np.stack([gp(a_re[d]), gp(a_im[d])], 0), ls=ls,
                           b=np.stack([gpb(b_re[d]), gpb(b_im[d])], 0), c=np.stack([gpc(c_re[d]), gpc(c_im[d])], 0)))
    in_maps = []
    orders = []
    for k in range(8):
        bi, half = k // 2, k % 2
        if half == 0:
            own = np.arange(0, 2048)
            partner = np.arange(2048, 4096)
            cidx = np.arange(0, 256)
        else:
            own = np.arange(4095, 2047, -1)
            partner = np.arange(2047, -1, -1)
            cidx = np.arange(255, -1, -1)
        X = np.concatenate([ctx[bi][cidx], x[bi][own], x[bi][partner]], axis=0)
        m = dict(shared)
        m["xall"] = _fm(X)
        ck, sk = _rope_tables(np.concatenate([own, partner]))
        cos_t = np.concatenate([np.ones((64, 256), np.float32), ck], axis=1)
        sin_t = np.concatenate([np.zeros((64, 256), np.float32), sk], axis=1)
        m["cosk"] = np.ascontiguousarray(cos_t)
        m["sink"] = np.ascontiguousarray(sin_t)
        m["cvec"] = np.ascontiguousarray(np.stack([_vec(c[bi]), _vec(c_ctx)], axis=2))
        dd = [s5_dir[half], s5_dir[1 - half]]
        m["s5a"] = np.ascontiguousarray(np.stack([dd[0]["a"], dd[1]["a"]], 0).transpose(2, 0, 1, 3))
        m["s5ls"] = np.ascontiguousarray(np.stack([dd[0]["ls"], dd[1]["ls"]], 0).transpose(1, 0, 2))
        m["s5b"] = np.ascontiguousarray(np.stack([dd[0]["b"], dd[1]["b"]], 0).transpose(2, 0, 1, 3, 4))
        m["s5c"] = np.ascontiguousarray(np.stack([dd[0]["c"], dd[1]["c"]], 0).transpose(2, 0, 1, 3, 4))
        pm = np.zeros((128, 2), np.float32)
        pm[:, half] = 1.0
        m["pmask"] = pm
        in_maps.append(m)
        orders.append(own)
    return in_maps, orders


_NC_CACHE = {}


def kernel(**inputs):
    in_maps, orders = make_in_maps(inputs)
    if "nc" not in _NC_CACHE:
        _NC_CACHE["nc"] = build_program()
    nc = _NC_CACHE["nc"]
    res = run_bass_kernel_spmd(nc, in_maps, core_ids=list(range(8)))
    out = np.zeros((4, 4096, D), np.float32)
    for k in range(8):
        y = np.asarray(res.results[k]["yout"])
        out[k // 2, orders[k], :] = y.transpose(2, 1, 0).reshape(NOWN, D)
    return out
```

```python
import numpy as np
from contextlib import ExitStack
import concourse.bass as bass
import concourse.mybir as mybir
from concourse.bass_utils import run_bass_kernel_spmd

F32 = mybir.dt.float32
BF16 = mybir.dt.bfloat16
AF = mybir.ActivationFunctionType
ALU = mybir.AluOpType

D = 1024
KC = 8
NCTX = 256
NOWN = 2048
NQ = NCTX + NOWN
NALL = NQ + NOWN
ALPHA = 4.0 ** 0.25
EPS = 1e-6
EPS_LN = EPS / (ALPHA * ALPHA)
DFF = 2816
NEXP = 8
DFE = 3584
QK_SCALE = 192.0 ** -0.5


class Sched:
    ENGS = ('pe', 'act', 'dve', 'pool', 'sp')

    def __init__(self, nc, es, ndma=20):
        self.nc = nc
        self.sem = {e: es.enter_context(nc.semaphore("sem_" + e)) for e in self.ENGS}
        self.dsem = [es.enter_context(nc.semaphore("dsem%d" % i)) for i in range(ndma)]
        self.dcnt = [0] * ndma
        self.dpool = {'pool': list(range(0, ndma // 2)), 'sp': list(range(ndma // 2, ndma))}
        self.dnext = {'pool': 0, 'sp': 0}
        self.ccsem = es.enter_context(nc.semaphore("ccsem"))
        self.cccnt = 0
        self.cnt = {e: 0 for e in self.ENGS}
        self.prog = {e: [] for e in self.ENGS}
        self.lastw = {}
        self.readers = {}
        self.known = {e: {} for e in self.ENGS}
        self.snap = {}
        self.nops = 0

    def _semof(self, s):
        if isinstance(s, str):
            return self.sem[s]
        return self.ccsem if s[0] == 'c' else self.dsem[s[1]]

    def collective(self, fn, reads=(), writes=()):
        waits = self._deps('pool', reads, writes)
        self.cccnt += 1
        tok = (('c', 0), self.cccnt)
        self.prog['pool'].append((waits, fn, 'cc'))
        self.snap[tok] = {s: c for s, c in self.known['pool'].items() if isinstance(s, str)}
        self._record(tok, reads, writes)
        return tok

    def _deps(self, eng, reads, writes):
        deps = {}
        for k in reads:
            t = self.lastw.get(k)
            if t is not None and deps.get(t[0], 0) < t[1]:
                deps[t[0]] = t[1]
        for k in writes:
            t = self.lastw.get(k)
            if t is not None and deps.get(t[0], 0) < t[1]:
                deps[t[0]] = t[1]
            for s, c in self.readers.get(k, {}).items():
                if deps.get(s, 0) < c:
                    deps[s] = c
        kn = self.known[eng]
        waits = []
        for s, c in deps.items():
            if s == 'pe' and eng == 'pe':
                continue
            if kn.get(s, 0) >= c:
                continue
            waits.append((s, c))
        for s, c in waits:
            kn[s] = c
            sn = self.snap.get((s, c))
            if sn:
                for s2, c2 in sn.items():
                    if kn.get(s2, 0) < c2:
                        kn[s2] = c2
        return waits

    def _record(self, tok, reads, writes):
        for k in writes:
            self.lastw[k] = tok
            self.readers[k] = {}
        for k in reads:
            r = self.readers.setdefault(k, {})
            if r.get(tok[0], 0) < tok[1]:
                r[tok[0]] = tok[1]

    def op(self, eng, fn, reads=(), writes=()):
        waits = self._deps(eng, reads, writes)
        self.cnt[eng] += 1
        tok = (eng, self.cnt[eng])
        self.prog[eng].append((waits, fn, None))
        self.snap[tok] = {s: c for s, c in self.known[eng].items() if isinstance(s, str)}
        self._record(tok, reads, writes)
        self.nops += 1
        return tok

    def dma(self, q, out, in_, reads=(), writes=()):
        pool_ = self.dpool[q]
        i = pool_[self.dnext[q] % len(pool_)]
        self.dnext[q] += 1
        waits = self._deps(q, reads, writes)
        src = ('d', i)
        prev = 16 * self.dcnt[i]
        if prev > 0 and self.known[q].get(src, 0) < prev:
            waits.append((src, prev))
            self.known[q][src] = prev
        self.dcnt[i] += 1
        tok = (src, 16 * self.dcnt[i])
        self.prog[q].append((waits, (lambda e, o=out, a=in_: e.dma_start(out=o, in_=a)), i))
        self.snap[tok] = {s: c for s, c in self.known[q].items() if isinstance(s, str)}
        self._record(tok, reads, writes)
        self.nops += 1
        return tok

    def flush(self, final_tokens=()):
        nc = self.nc
        prog = self.prog
        self.prog = {e: [] for e in self.ENGS}

        def emit(name, e):
            for waits, fn, di in prog[name]:
                for s, c in waits:
                    e.wait_ge(self._semof(s), c)
                ins = fn(e)
                if di is None:
                    ins.then_inc(self.sem[name], 1)
                elif di == 'cc':
                    ins.then_inc(self.ccsem)
                else:
                    ins.then_inc(self.dsem[di], 16)
            if name == 'sp':
                for s, c in final_tokens:
                    e.wait_ge(self._semof(s), c)

        with nc.Block() as block:
            if prog['pe']:
                @block.tensor
                def _(e):
                    emit('pe', e)
            if prog['act']:
                @block.scalar
                def _(e):
                    emit('act', e)
            if prog['dve']:
                @block.vector
                def _(e):
                    emit('dve', e)
            if prog['pool']:
                @block.gpsimd
                def _(e):
                    emit('pool', e)
            if prog['sp'] or final_tokens:
                @block.sync
                def _(e):
                    emit('sp', e)
        for e in self.ENGS:
            for s in self.ENGS:
                self.known[e][s] = self.cnt[s]


class B:
    def __init__(self, S):
        self.S = S
        self.rr = 0

    def mm(self, items, reads, writes):
        its = list(items)

        def fn(e):
            ins = None
            for (o, l, r, st, sp) in its:
                ins = e.matmul(o, l, r, start=st, stop=sp)
            return ins
        return self.S.op('pe', fn, reads, writes)

    def act(self, out, in_, func, reads, writes, bias=None, scale=None, eng='act'):
        kw = {}
        if bias is not None:
            kw['bias'] = bias
        if scale is not None:
            kw['scale'] = scale
        return self.S.op('act', lambda e: e.activation(out=out, in_=in_, func=func, **kw), reads, writes)

    def tt(self, eng, out, in0, in1, op, reads, writes):
        return self.S.op(eng, lambda e: e.tensor_tensor(out, in0, in1, op), reads, writes)

    def ts(self, eng, out, in0, s1, s2, op0, op1, reads, writes):
        if op1 is None:
            return self.S.op(eng, lambda e: e.tensor_scalar(out, in0, s1, None, op0), reads, writes)
        return self.S.op(eng, lambda e: e.tensor_scalar(out, in0, s1, s2, op0, op1), reads, writes)

    def stt(self, eng, out, in0, scalar, in1, op0, op1, reads, writes):
        return self.S.op(eng, lambda e: e.scalar_tensor_tensor(out, in0, scalar, in1, op0, op1), reads, writes)

    def copy(self, eng, out, in_, reads, writes):
        if eng == 'act':
            return self.S.op('act', lambda e: e.activation(out=out, in_=in_, func=AF.Copy), reads, writes)
        return self.S.op(eng, lambda e: e.tensor_copy(out, in_), reads, writes)

    def recip(self, eng, out, in_, reads, writes):
        return self.S.op(eng, lambda e: e.reciprocal(out, in_), reads, writes)

    def memset(self, eng, ap, val, writes):
        return self.S.op(eng, lambda e: e.memset(ap, val), (), writes)


def _rs(ap2d, shape):
    dims = shape[1:]
    if len(dims) > 1:
        names = ["a%d" % i for i in range(len(dims))]
        kw = {names[i]: int(dims[i]) for i in range(len(dims) - 1)}
        ap2d = ap2d.rearrange("p (%s) -> p %s" % (" ".join(names), " ".join(names)), **kw)
    return ap2d[:shape[0]]


class Arena:
    def __init__(self, flat_f32):
        self.f = flat_f32
        self.off = 0
        self.W = flat_f32.shape[1]

    def f32(self, shape):
        n = int(np.prod(shape[1:]))
        v = self.f[:, self.off:self.off + n]
        self.off += n
        assert self.off <= self.W, (self.off, self.W)
        return _rs(v, shape)

    def bf16(self, shape):
        n = int(np.prod(shape[1:]))
        nw = (n + 1) // 2
        v = self.f[:, self.off:self.off + nw].bitcast(BF16)[:, :n]
        self.off += nw
        assert self.off <= self.W, (self.off, self.W)
        return _rs(v, shape)


def build_program(debug=False, stop_after=None):
    nc = bass.Bass("TRN2", target_bir_lowering=False)
    dt = nc.dram_tensor

    def din(name, shape, dtype=F32):
        return dt(name, list(shape), dtype, kind="ExternalInput").ap()

    def dout(name, shape, dtype=F32):
        return dt(name, list(shape), dtype, kind="ExternalOutput").ap()

    xall = din("xall", [128, KC, NALL])
    cosk = din("cosk", [64, NALL])
    sink = din("sink", [64, NALL])
    cvec = din("cvec", [128, KC, 2])
    ada_w = din("ada_w", [2, D, 6 * D])
    adab = din("adab", [128, 2, 48])
    lng = din("lng", [128, 4, KC])
    lnb = din("lnb", [128, 4, KC])
    wdown = din("wdown", [128, KC, 768])
    gq = din("gq", [128, 3])
    gkv = din("gkv", [128, 2])
    wuq = din("wuq", [128, 3, 8 * 256])
    wuk = din("wuk", [128, 2, 1024])
    wuv = din("wuv", [128, 2, 1024])
    wo = din("wo", [128, 8, 1024])
    ffn_w1 = din("ffn_w1", [D, DFF])
    ffn_w3 = din("ffn_w3", [D, DFF])
    ffn_w2 = din("ffn_w2", [DFF, D])
    s5a = din("s5a", [128, 2, 2, 32])
    s5ls = din("s5ls", [128, 2, 32])
    s5b = din("s5b", [128, 2, 2, 32, 16])
    s5c = din("s5c", [128, 2, 2, 32, 16])
    s5d = din("s5d", [128, KC])
    pmask = din("pmask", [128, 2])
    cc_in = nc.dram_tensor("cc_in", [128, 128], F32)
    cc_out = nc.dram_tensor("cc_out", [128, 128], F32)
    ident = din("ident", [128, 128])
    identf = din("identf", [128, 128])
    glu_a = din("glu_a", [D, D])
    glu_b = din("glu_b", [D, D])
    wrt = din("wrt", [128, KC, 8])
    sel = din("sel", [8, 8, 128])
    moe_w1 = din("moe_w1", [NEXP, D, DFE])
    moe_w3 = din("moe_w3", [NEXP, D, DFE])
    moe_w2 = din("moe_w2", [NEXP, DFE, D])
    yout = dout("yout", [128, KC, NOWN])
    dbg = dout("dbg", [128, KC, NQ]) if debug else None

    with ExitStack() as es:
        S = Sched(nc, es)
        b = B(S)
        sb = lambda name, shape, dtype=F32, stack=es: stack.enter_context(nc.sbuf_tensor(name, list(shape), dtype))
        PS = [es.enter_context(nc.psum_tensor("ps%d" % i, [128, 512], F32)) for i in range(8)]
        pk = lambda i: ('ps', i)

        hT = sb("hT", [128, KC, NQ], F32)
        ubuf = sb("ubuf", [128, KC, NQ], BF16)
        hT_flat = hT[:].rearrange("p k n -> p (k n)")
        ub_flat32 = ubuf[:].rearrange("p k n -> p (k n)").bitcast(F32)
        modT = sb("modT", [128, 2, 48, 2], F32)
        pv = sb("pv", [128, 512], F32)
        lng_s = sb("lng_s", [128, 4, KC], F32)
        lnb_s = sb("lnb_s", [128, 4, KC], F32)
        ones_d = sb("ones_d", [128, 128], F32)
        ones_q = sb("ones_q", [128, 128], F32)
        ones_kv = sb("ones_kv", [128, 128], F32)
        ones_bf = sb("ones_bf", [128, 128], BF16)

        pv_off = [0]

        def pvslot(n):
            o = pv_off[0]
            pv_off[0] += n
            assert pv_off[0] <= 512
            return o

        SL = {}
        for l in range(2):
            SL[('s1p', l)] = pvslot(16)
            SL[('gam', l)] = pvslot(16)
            SL[('s2p', l)] = pvslot(16)
            SL[('gaf', l)] = pvslot(16)
            SL[('gU', l, 0)] = pvslot(16)
            SL[('bU', l, 0)] = pvslot(16)
        SL[('gU', 0, 1)] = pvslot(16)
        SL[('bU', 0, 1)] = pvslot(16)

        def pvs(key, kc, s):
            o = SL[key] + kc * 2 + s
            return pv[:, o:o + 1]

        def modv(l, m, kc, s):
            return modT[:, l, m * 8 + kc, s:s + 1]

        def ln_norm(r_ap_fn, N, s, rkeys, g1, b1, out1_fn, out1_keys, g2=None, b2=None, out2_fn=None, out2_keys=(),
                    tmp=None, psA=6, psB=7, in2_fn=None):
            sqb, mean_sb, m2, sd, rstd, nmr = tmp
            for kc in range(KC):
                b.mm([(PS[psA][:, :N], ones_d[:], r_ap_fn(kc), kc == 0, kc == KC - 1)], [rkeys[kc], 'ones'], [pk(psA)])
            for kc in range(KC):
                q = kc % 2
                b.act(sqb[:, q, :N], r_ap_fn(kc), AF.Square, [rkeys[kc]], [('sqb', q)])
                b.mm([(PS[psB][:, :N], ones_d[:], sqb[:, q, :N], kc == 0, kc == KC - 1)], [('sqb', q), 'ones'], [pk(psB)])
            b.copy('act', mean_sb[:, :N], PS[psA][:, :N], [pk(psA)], ['mean_sb'])
            b.tt('dve', m2[:, :N], mean_sb[:, :N], mean_sb[:, :N], ALU.mult, ['mean_sb'], ['m2'])
            b.tt('dve', m2[:, :N], PS[psB][:, :N], m2[:, :N], ALU.subtract, [pk(psB), 'm2'], ['m2'])
            b.act(sd[:, :N], m2[:, :N], AF.Sqrt, ['m2'], ['sd'], bias=eps_ln[:, 0:1])
            b.recip('dve', rstd[:, :N], sd[:, :N], ['sd'], ['rstd'])
            b.stt('dve', nmr[:, :N], mean_sb[:, :N], -1.0, rstd[:, :N], ALU.mult, ALU.mult, ['mean_sb', 'rstd'], ['nmr'])
            for kc in range(KC):
                ra = r_ap_fn(kc)
                eng = 'dve' if kc % 2 == 0 else 'pool'
                b.tt(eng, ra, ra, rstd[:, :N], ALU.mult, [rkeys[kc], 'rstd'], [rkeys[kc]])
                b.tt(eng, ra, ra, nmr[:, :N], ALU.add, [rkeys[kc], 'nmr'], [rkeys[kc]])
                if out2_fn is not None:
                    ok2 = list(out2_keys) if in2_fn is not None else [out2_keys[kc]]
                    b.act(out2_fn(kc), (in2_fn(kc) if in2_fn is not None else ra), AF.Identity, [rkeys[kc]], ok2, bias=b2(kc), scale=g2(kc))
                b.act(out1_fn(kc), ra, AF.Identity, [rkeys[kc]], [out1_keys[kc]], bias=b1(kc), scale=g1(kc))

        eps_ln = sb("eps_ln", [128, 2], F32)

        def ln_tmps(stack, tag):
            return (sb("sqb" + tag, [128, 2, 512], F32, stack), sb("mean" + tag, [128, 512], F32, stack),
                    sb("m2" + tag, [128, 512], F32, stack), sb("sd" + tag, [128, 512], F32, stack),
                    sb("rstd" + tag, [128, 512], F32, stack), sb("nmr" + tag, [128, 512], F32, stack))

        with ExitStack() as ps0:
            aw = sb("aw", [128, 2, KC, 1024], F32, ps0)
            cv = sb("cv", [128, KC, 2], F32, ps0)
            scT = sb("scT", [128, KC, 2], F32, ps0)
            adab_s = sb("adab_s", [128, 2, 48], F32, ps0)
            sig = sb("sig", [128, KC, 2], F32, ps0)
            b.memset('dve', ones_d[:], 1.0 / 1024, ['ones'])
            b.memset('dve', ones_q[:], 1.0 / 384, ['ones_q'])
            b.memset('dve', ones_kv[:], 1.0 / 256, ['ones_kv'])
            b.memset('dve', ones_bf[:], 1.0, ['ones_bf'])
            b.memset('dve', eps_ln[:, 0:1], EPS_LN, ['eps_ln'])
            b.memset('dve', eps_ln[:, 1:2], EPS, ['eps_ln1'])
            S.dma('sp', cv[:], cvec[:], [], ['cv'])
            S.dma('sp', adab_s[:], adab[:], [], ['adab'])
            S.dma('sp', lng_s[:], lng[:], [], ['lng'])
            S.dma('sp', lnb_s[:], lnb[:], [], ['lnb'])
            b.act(sig[:], cv[:], AF.Sigmoid, ['cv'], ['sig'])
            b.tt('dve', scT[:], cv[:], sig[:], ALU.mult, ['cv', 'sig'], ['scT'])
            i = 0
            for l in range(2):
                for m in range(6):
                    q = i % 2
                    i += 1
                    src = ada_w[l, :, m * 1024:(m + 1) * 1024].rearrange("(kc p) n -> p kc n", p=128)
                    S.dma('sp', aw[:, q, :, :], src, [], [('aw', q)])
                    for oc in range(KC):
                        col = (m * 8 + oc) * 2
                        pb = l
                        b.mm([(PS[pb][:, col:col + 2], aw[:, q, kc, oc * 128:(oc + 1) * 128], scT[:, kc, :], kc == 0, kc == KC - 1)
                              for kc in range(KC)], [('aw', q), 'scT'], [pk(pb)])
                for s in range(2):
                    b.tt('dve', modT[:, l, :, s], PS[l][:, 0:96].rearrange("p (j s) -> p j s", s=2)[:, :, s], adab_s[:, l, :],
                         ALU.add, [pk(l), 'adab'], [('modT', l)])
            def pvw(key):
                o = SL[key]
                return pv[:, o:o + 16].rearrange("p (k s) -> p k s", s=2)

            def modw(l, m):
                return modT[:, l, m * 8:(m + 1) * 8, :]

            def bc2(ap):
                return ap.unsqueeze(2).to_broadcast([128, KC, 2])
            for l in range(2):
                rd = [('modT', l), 'lng', 'lnb']
                b.ts('dve', pvw(('s1p', l)), modw(l, 1), 1.0, None, ALU.add, None, rd, ['pv'])
                b.ts('dve', pvw(('gam', l)), modw(l, 2), 1.0 / ALPHA, None, ALU.mult, None, rd, ['pv'])
                b.ts('dve', pvw(('s2p', l)), modw(l, 4), 1.0, None, ALU.add, None, rd, ['pv'])
                b.ts('dve', pvw(('gaf', l)), modw(l, 5), 1.0 / ALPHA, None, ALU.mult, None, rd, ['pv'])
                b.tt('dve', pvw(('gU', l, 0)), pvw(('s2p', l)), bc2(lng_s[:, l * 2 + 0, :]), ALU.mult, rd + ['pv'], ['pv'])
                b.tt('dve', pvw(('bU', l, 0)), pvw(('s2p', l)), bc2(lnb_s[:, l * 2 + 0, :]), ALU.mult, rd + ['pv'], ['pv'])
                b.tt('dve', pvw(('bU', l, 0)), pvw(('bU', l, 0)), modw(l, 3), ALU.add, rd + ['pv'], ['pv'])
            rd = [('modT', 1), 'lng', 'lnb', 'pv']
            b.tt('dve', pvw(('gU', 0, 1)), pvw(('s1p', 1)), bc2(lng_s[:, 1, :]), ALU.mult, rd, ['pv'])
            b.tt('dve', pvw(('bU', 0, 1)), pvw(('s1p', 1)), bc2(lnb_s[:, 1, :]), ALU.mult, rd, ['pv'])
            b.tt('dve', pvw(('bU', 0, 1)), pvw(('bU', 0, 1)), modw(1, 0), ALU.add, rd, ['pv'])
            S.flush()

        groups = [(0, 256, 1)] + [(256 + 512 * i, 512, 0) for i in range(8)]
        qgroups = groups[:5]

        pl0 = ExitStack()
        if True:
            cqT = sb("cqT", [128, 3, NQ], BF16, pl0)
            ckvT = sb("ckvT", [128, 2, NALL], BF16, pl0)
            krT = sb("krT", [64, NALL], BF16, pl0)

            with ExitStack() as p1:
                wd = sb("wd", [128, KC, 768], BF16, p1)
                gq_s = sb("gq_s", [128, 3], F32, p1)
                gkv_s = sb("gkv_s", [128, 2], F32, p1)
                hA = Arena(hT_flat)
                uA = Arena(ub_flat32)
                xg = hA.f32([128, 2, KC, 512])
                raw = hA.f32([128, 7, 512])
                sqr = hA.f32([128, 5, 512])
                rq = hA.f32([128, 2, 512])
                rs = hA.f32([128, 2, 512])
                kt1 = hA.f32([64, 2, 512])
                ug = uA.bf16([128, 2, KC, 512])
                csk = uA.f32([64, 2, 2, 512])
                S.dma('pool', wd[:], wdown[:], [], ['wd'])
                S.dma('sp', gq_s[:], gq[:], [], ['gq'])
                S.dma('sp', gkv_s[:], gkv[:], [], ['gkv'])
                psr = 0
                for gi, (c0, N, s) in enumerate(groups):
                    q = gi % 2
                    own = c0 < NQ
                    S.dma('sp', xg[:, q, :, :N], xall[:, :, c0:c0 + N], [], [('xg', q)])
                    S.dma('sp', csk[:, q, 0, :N], cosk[:, c0:c0 + N], [], [('csk', q)])
                    S.dma('sp', csk[:, q, 1, :N], sink[:, c0:c0 + N], [], [('csk', q)])
                    for kc in range(KC):
                        if kc % 2 == 0:
                            b.ts('dve', ug[:, q, kc, :N], xg[:, q, kc, :N], pvs(('s1p', 0), kc, s), modv(0, 0, kc, s),
                                 ALU.mult, ALU.add, [('xg', q), 'pv', ('modT', 0)], [('ug', q, kc)])
                        else:
                            b.act(ug[:, q, kc, :N], xg[:, q, kc, :N], AF.Identity, [('xg', q), 'pv', ('modT', 0)], [('ug', q, kc)],
                                  bias=modv(0, 0, kc, s), scale=pvs(('s1p', 0), kc, s))
                    chunks = ([(0, 0, 128), (1, 128, 128), (2, 256, 128)] if own else []) + \
                             [(3, 384, 128), (4, 512, 128), (5, 640, 64), (6, 704, 64)]
                    for (ri, m0, M) in chunks:
                        pb = psr % 4
                        psr += 1
                        b.mm([(PS[pb][:M, :N], wd[:, kc, m0:m0 + M], ug[:, q, kc, :N], kc == 0, kc == KC - 1) for kc in range(KC)],
                             ['wd'] + [('ug', q, kc) for kc in range(KC)], [pk(pb)])
                        b.copy('act', raw[:M, ri, :N], PS[pb][:M, :N], [pk(pb)], [('raw', ri)])
                    qs = [0, 1, 2] if own else []
                    for ri in qs + [3, 4]:
                        eng = 'dve' if ri % 2 == 0 else 'pool'
                        b.tt(eng, sqr[:, ri, :N], raw[:, ri, :N], raw[:, ri, :N], ALU.mult, [('raw', ri)], [('sqr', ri)])
                    if own:
                        b.mm([(PS[4][:, :N], ones_q[:], sqr[:, ri, :N], ri == 0, ri == 2) for ri in range(3)],
                             ['ones_q'] + [('sqr', ri) for ri in range(3)], [pk(4)])
                        b.act(rq[:, 0, :N], PS[4][:, :N], AF.Sqrt, [pk(4), 'eps_ln1'], [('rq', 0)], bias=eps_ln[:, 1:2])
                        b.recip('dve', rs[:, 0, :N], rq[:, 0, :N], [('rq', 0)], [('rs', 0)])
                        for ri in range(3):
                            b.stt('dve', cqT[:, ri, c0:c0 + N], raw[:, ri, :N], gq_s[:, ri:ri + 1], rs[:, 0, :N], ALU.mult, ALU.mult,
                                  [('raw', ri), 'gq', ('rs', 0)], [('cqT', gi)])
                    b.mm([(PS[5][:, :N], ones_kv[:], sqr[:, ri, :N], ri == 3, ri == 4) for ri in (3, 4)],
                         ['ones_kv', ('sqr', 3), ('sqr', 4)], [pk(5)])
                    b.act(rq[:, 1, :N], PS[5][:, :N], AF.Sqrt, [pk(5), 'eps_ln1'], [('rq', 1)], bias=eps_ln[:, 1:2])
                    b.recip('dve', rs[:, 1, :N], rq[:, 1, :N], [('rq', 1)], [('rs', 1)])
                    for ri in (3, 4):
                        b.stt('dve', ckvT[:, ri - 3, c0:c0 + N], raw[:, ri, :N], gkv_s[:, ri - 3:ri - 2], rs[:, 1, :N], ALU.mult, ALU.mult,
                              [('raw', ri), 'gkv', ('rs', 1)], [('ckvT', gi)])
                    b.tt('pool', kt1[:, 0, :N], raw[:64, 5, :N], csk[:, q, 0, :N], ALU.mult, [('raw', 5), ('csk', q)], [('kt1', 0)])
                    b.tt('pool', kt1[:, 1, :N], raw[:64, 6, :N], csk[:, q, 1, :N], ALU.mult, [('raw', 6), ('csk', q)], [('kt1', 1)])
                    b.tt('pool', krT[:, c0:c0 + N], kt1[:, 0, :N], kt1[:, 1, :N], ALU.add, [('kt1', 0), ('kt1', 1)], [('krT', gi)])
                S.flush()

            if stop_after == 'p1':
                pass

            if True:
                aoT = ubuf
                with ExitStack() as p2:
                    hA = Arena(hT_flat)
                    KnT = hA.bf16([128, 2, NALL])
                    Vt = hA.bf16([128, 2, 34, 128])
                    QnT = hA.bf16([128, 2, NQ])
                    QrT = hA.bf16([64, 2, NQ])
                    qt1 = hA.f32([64, 2, 512])
                    rD = hA.f32([128, 2, 512])
                    csq = hA.f32([64, 2, 2, 512])
                    wq_h = sb("wq_h", [128, 2, 3, 256], BF16, p2)
                    wk_h = sb("wk_h", [128, 2, 2, 128], BF16, p2)
                    wv_h = sb("wv_h", [128, 2, 2, 128], BF16, p2)
                    Pt = sb("Pt", [128, 3, 512], BF16, p2)
                    allck = [('ckvT', gi) for gi in range(9)]
                    allcq = [('cqT', gi) for gi in range(5)]
                    allkr = [('krT', gi) for gi in range(9)]
                    cp = 0
                    pcount = 0
                    csn = 0
                    for h in range(8):
                        hq = h % 2
                        S.dma('pool', wq_h[:, hq, :, :], wuq[:, :, h * 256:(h + 1) * 256], [], [('wq_h', hq)])
                        S.dma('pool', wk_h[:, hq, :, :], wuk[:, :, h * 128:(h + 1) * 128], [], [('wk_h', hq)])
                        S.dma('pool', wv_h[:, hq, :, :], wuv[:, :, h * 128:(h + 1) * 128], [], [('wv_h', hq)])
                        for gi, (c0, N, s) in enumerate(groups):
                            pb = 6 + (cp % 2)
                            cp += 1
                            b.mm([(PS[pb][:, :N], wk_h[:, hq, kc, :], ckvT[:, kc, c0:c0 + N], kc == 0, kc == 1) for kc in range(2)],
                                 [('wk_h', hq), ('ckvT', gi)], [pk(pb)])
                            b.copy('act' if gi % 2 == 0 else 'dve', KnT[:, hq, c0:c0 + N], PS[pb][:, :N], [pk(pb)], [('KnT', hq, gi)])
                        for t0 in range(0, 34, 4):
                            nt = min(4, 34 - t0)
                            pb = 6 + (cp % 2)
                            cp += 1
                            items = []
                            for j in range(nt):
                                kt = t0 + j
                                for kc in range(2):
                                    items.append((PS[pb][:, j * 128:(j + 1) * 128], ckvT[:, kc, kt * 128:(kt + 1) * 128],
                                                  wv_h[:, hq, kc, :], kc == 0, kc == 1))
                            b.mm(items, [('wv_h', hq)] + allck, [pk(pb)])
                            b.copy('dve' if (t0 // 4) % 2 == 0 else 'act', Vt[:, hq, t0:t0 + nt, :],
                                   PS[pb][:, :nt * 128].rearrange("p (j d) -> p j d", d=128), [pk(pb)], [('Vt', hq, t0)])
                        for gi, (c0, N, s) in enumerate(qgroups):
                            cq_ = csn % 2
                            csn += 1
                            S.dma('sp', csq[:, cq_, 0, :N], cosk[:, c0:c0 + N], [], [('csq', cq_)])
                            S.dma('sp', csq[:, cq_, 1, :N], sink[:, c0:c0 + N], [], [('csq', cq_)])
                            pb = 6 + (cp % 2)
                            cp += 1
                            b.mm([(PS[pb][:, :N], wq_h[:, hq, kc, 0:128], cqT[:, kc, c0:c0 + N], kc == 0, kc == 2) for kc in range(3)],
                                 [('wq_h', hq), ('cqT', gi)], [pk(pb)])
                            b.copy('act', QnT[:, hq, c0:c0 + N], PS[pb][:, :N], [pk(pb)], [('QnT', hq, gi)])
                            pb = 6 + (cp % 2)
                            cp += 1
                            b.mm([(PS[pb][:64, :N], wq_h[:, hq, kc, 128:192], cqT[:, kc, c0:c0 + N], kc == 0, kc == 2) for kc in range(3)],
                                 [('wq_h', hq), ('cqT', gi)], [pk(pb)])
                            b.tt('dve', qt1[:, 0, :N], PS[pb][:64, :N], csq[:, cq_, 0, :N], ALU.mult, [pk(pb), ('csq', cq_)], [('qt1', 0)])
                            pb = 6 + (cp % 2)
                            cp += 1
                            b.mm([(PS[pb][:64, :N], wq_h[:, hq, kc, 192:256], cqT[:, kc, c0:c0 + N], kc == 0, kc == 2) for kc in range(3)],
                                 [('wq_h', hq), ('cqT', gi)], [pk(pb)])
                            b.tt('dve', qt1[:, 1, :N], PS[pb][:64, :N], csq[:, cq_, 1, :N], ALU.mult, [pk(pb), ('csq', cq_)], [('qt1', 1)])
                            b.tt('pool', QrT[:, hq, c0:c0 + N], qt1[:, 0, :N], qt1[:, 1, :N], ALU.add, [('qt1', 0), ('qt1', 1)], [('QrT', hq, gi)])
                        kn_keys = [('KnT', hq, gi) for gi in range(9)]
                        vt_keys = [('Vt', hq, t0) for t0 in range(0, 34, 4)]
                        for gi, (c0, N, s) in enumerate(qgroups):
                            kts = [0, 1] if s == 1 else list(range(34))
                            ob = 2 + (pcount % 2) * 2
                            pcount += 1

                            def issue_S(j, c0=c0, N=N, gi=gi):
                                kt = kts[j]
                                sbk = j % 2
                                b.mm([(PS[sbk][:, :N], KnT[:, hq, kt * 128:(kt + 1) * 128], QnT[:, hq, c0:c0 + N], True, False),
                                      (PS[sbk][:, :N], krT[:, kt * 128:(kt + 1) * 128], QrT[:, hq, c0:c0 + N], False, True)],
                                     kn_keys + allkr + [('QnT', hq, gi), ('QrT', hq, gi)], [pk(sbk)])
                            issue_S(0)
                            for j in range(len(kts)):
                                kt = kts[j]
                                if j + 1 < len(kts):
                                    issue_S(j + 1)
                                pq = j % 3
                                b.act(Pt[:, pq, :N], PS[j % 2][:, :N], AF.Exp, [pk(j % 2)], [('Pt', pq)], scale=QK_SCALE)
                                b.mm([(PS[ob][:, :N], Vt[:, hq, kt, :], Pt[:, pq, :N], j == 0, j == len(kts) - 1),
                                      (PS[ob + 1][:, :N], ones_bf[:], Pt[:, pq, :N], j == 0, j == len(kts) - 1)],
                                     vt_keys + [('Pt', pq), 'ones_bf'], [pk(ob), pk(ob + 1)])
                            rq_ = (pcount % 2)
                            b.recip('dve', rD[:, rq_, :N], PS[ob + 1][:, :N], [pk(ob + 1)], [('rD', rq_)])
                            b.tt('dve', aoT[:, h, c0:c0 + N], PS[ob][:, :N], rD[:, rq_, :N], ALU.mult, [pk(ob), ('rD', rq_)], [('aoT', h, gi)])
                    S.flush()
                pl0.close()

                with ExitStack() as p3:
                    wo_s = sb("wo_s", [128, 8, 1024], BF16, p3)
                    lntmp = ln_tmps(p3, "b3")
                    S.dma('pool', wo_s[:], wo[:], [], ['wo'])
                    def oproj3(gi):
                        (c0, N, s) = qgroups[gi]
                        S.dma('sp', hT[:, :, c0:c0 + N], xall[:, :, c0:c0 + N], [], [('hT', gi, kc) for kc in range(KC)])
                        for oc in range(KC):
                            pb = oc % 4
                            b.mm([(PS[pb][:, :N], wo_s[:, h, oc * 128:(oc + 1) * 128], aoT[:, h, c0:c0 + N], h == 0, h == 7) for h in range(8)],
                                 ['wo'] + [('aoT', h, gi) for h in range(8)], [pk(pb)])
                            b.stt('dve', hT[:, oc, c0:c0 + N], PS[pb][:, :N], pvs(('gam', 0), oc, s), hT[:, oc, c0:c0 + N], ALU.mult, ALU.add,
                                  [pk(pb), 'pv', ('hT', gi, oc)], [('hT', gi, oc)])
                    oproj3(0)
                    for gi, (c0, N, s) in enumerate(qgroups):
                        if gi + 1 < len(qgroups):
                            oproj3(gi + 1)
                        ln_norm(lambda kc, c0=c0, N=N: hT[:, kc, c0:c0 + N], N, s, [('hT', gi, kc) for kc in range(KC)],
                                g1=lambda kc: lng_s[:, 0, kc:kc + 1], b1=lambda kc: lnb_s[:, 0, kc:kc + 1],
                                out1_fn=lambda kc, c0=c0, N=N: hT[:, kc, c0:c0 + N], out1_keys=[('hT', gi, kc) for kc in range(KC)],
                                g2=lambda kc, s=s: pvs(('gU', 0, 0), kc, s), b2=lambda kc, s=s: pvs(('bU', 0, 0), kc, s),
                                out2_fn=lambda kc, c0=c0, N=N: ubuf[:, kc, c0:c0 + N], out2_keys=[('ubuf', gi, kc) for kc in range(KC)],
                                tmp=lntmp)
                    S.flush()

            with ExitStack() as p4:
                w1s = sb("w1s", [128, 2, KC, 512], BF16, p4)
                w3s = sb("w3s", [128, 2, KC, 512], BF16, p4)
                w2s = sb("w2s", [128, 2, 4, 1024], BF16, p4)
                s1b = sb("s1b", [128, 2, 512], F32, p4)
                gT = sb("gT", [128, 2, 4, 512], BF16, p4)
                swiglu_stages(nc, S, b, PS, pk, qgroups, list(range(5)), ubuf, hT, [(ffn_w1, ffn_w3, ffn_w2, DFF, None)],
                              (w1s, w3s, w2s, s1b, gT, None), lambda oc, s: pvs(('gaf', 0), oc, s), NF=4, NWB=2)
                S.flush()
            with ExitStack() as p4b:
                lntmp = ln_tmps(p4b, "b4")
                for gi, (c0, N, s) in enumerate(qgroups):
                    ln_norm(lambda kc, c0=c0, N=N: hT[:, kc, c0:c0 + N], N, s, [('hT', gi, kc) for kc in range(KC)],
                            g1=lambda kc: lng_s[:, 1, kc:kc + 1], b1=lambda kc: lnb_s[:, 1, kc:kc + 1],
                            out1_fn=lambda kc, c0=c0, N=N: hT[:, kc, c0:c0 + N], out1_keys=[('hT', gi, kc) for kc in range(KC)],
                            g2=lambda kc, s=s: pvs(('gU', 0, 1), kc, s), b2=lambda kc, s=s: pvs(('bU', 0, 1), kc, s),
                            out2_fn=lambda kc, c0=c0, N=N: ubuf[:, kc, :].rearrange("p (j c) -> p c j", j=8)[:, c0 // 8:(c0 + N) // 8, :],
                            out2_keys=[('ubuf', g_, kc) for kc in range(KC) for g_ in range(5)],
                            in2_fn=lambda kc, c0=c0, N=N: hT[:, kc, c0:c0 + N].rearrange("p (c j) -> p c j", j=8),
                            tmp=lntmp)
                S.flush()

        lvl = {'l0': 0, 'p5': 1, 'p6': 1, 'p7a': 2, 'p7b': 2, None: 2}[stop_after]
        if lvl >= 1:
            own_groups = [(256 + 512 * i, 512, 0) for i in range(4)]
            own_gidx = [1, 2, 3, 4]
            allh = [('hT', gi, kc) for gi in range(5) for kc in range(KC)]
            with ExitStack() as p5:
                s5a_s = sb("s5a_s", [128, 2, 2, 32], F32, p5)
                s5ls_s = sb("s5ls_s", [128, 2, 32], F32, p5)
                s5b_k = sb("s5b_k", [128, 2, 2, 2, 4, 16], F32, p5)
                s5c_k = sb("s5c_k", [128, 2, 2, 2, 4, 16], F32, p5)
                s5d_s = sb("s5d_s", [128, KC], F32, p5)
                dAB = sb("dAB", [128, 2, KC], F32, p5)
                rin_s = sb("rin_s", [128, 32, 2], F32, p5)
                fx_s = sb("fx_s", [128, 32, 2], F32, p5)
                idb = sb("idb", [128, 128], BF16, p5)
                Pw = sb("Pw", [128, 2, 9, 2, 32], F32, p5)
                cS = sb("cS", [128, 2, 2, 8, 32], F32, p5)
                rho = sb("rho", [128, 2, 32], F32, p5)
                Arot = sb("Arot", [128, 2, 2, 32, 18], F32, p5)
                Brot = sb("Brot", [128, 2, 2, 32, 16], F32, p5)
                tmp = sb("s5tmp", [128, 16, 32], F32, p5)
                wpw = sb("wpw", [128, 9, 2, 32], F32, p5)
                Vc = sb("Vc", [128, 2, 4, 8, 16], F32, p5)
                Vt1 = sb("Vt1", [128, 2, 4, 8, 16], F32, p5)
                tmp8 = Vt1[:].rearrange("p a b c d -> p a (b c d)")[:, :, 0:256]
                Vbd = sb("Vbd", [128, 2, 4, 8, 2, 32], BF16, p5)
                Ebd = sb("Ebd", [128, 2, 4, 8, 2, 32], BF16, p5)
                Cbd = sb("Cbd", [128, 2, 4, 2, 32], BF16, p5)
                Wst = sb("Wst", [128, 2, 8, 2, 128], BF16, p5)
                rot = sb("rot", [128, 2, 2, 288], F32, p5)
                trti = sb("trti", [128, 1, 2, 288], F32, p5)
                rt2 = sb("rt2", [128, 2, 2, 288], F32, p5)
                ccb2 = sb("ccb2", [128, 2, 64], F32, p5)
                gg = sb("gg", [128, 2, 2, 288], F32, p5)
                Hp = sb("Hp", [128, 2, 2, 2, 288], BF16, p5)
                t32z = sb("t32z", [128, 2, 2, 256], F32, p5)
                S.dma('sp', s5a_s[:], s5a[:], [], ['s5a'])
                S.dma('sp', s5ls_s[:], s5ls[:], [], ['s5ls'])
                S.dma('sp', s5d_s[:], s5d[:], [], ['s5d'])
                pm_s = sb("pm_s", [128, 2], F32, p5)
                ccb = sb("ccb", [128, 2, 64], F32, p5)
                S.dma('sp', pm_s[:], pmask[:], [], ['pmask'])
                S.dma('pool', idb[:], ident[:], [], ['idb'])
                b.memset('pool', Vbd[:], 0.0, [('Vbd', 0), ('Vbd', 1)])
                b.memset('pool', Ebd[:], 0.0, [('Ebd', 0), ('Ebd', 1)])
                b.memset('pool', Cbd[:], 0.0, [('Cbd', 0), ('Cbd', 1)])
                b.memset('pool', Hp[:], 0.0, [('Hp', d_, h_) for d_ in range(2) for h_ in range(2)])
                for kc in range(KC):
                    b.tt('dve', dAB[:, 0, kc:kc + 1], s5d_s[:, kc:kc + 1], pvs(('s1p', 1), kc, 0), ALU.mult, ['s5d', 'pv'], ['dAB'])
                    b.tt('dve', dAB[:, 1, kc:kc + 1], s5d_s[:, kc:kc + 1], modv(1, 0, kc, 0), ALU.mult, ['s5d', ('modT', 1)], ['dAB'])

                def T(i):
                    return tmp[:, i, :]

                def tk(i):
                    return ('s5t', i)

                def cmul(eng, o_r, o_i, a_r, a_i, b_r, b_i, t1, t2, k1, k2, rk, wk, conj_b=False):
                    b.tt(eng, t1, a_r, b_r, ALU.mult, rk, [k1])
                    b.tt(eng, t2, a_i, b_i, ALU.mult, rk, [k2])
                    b.tt(eng, o_r, t1, t2, ALU.add if conj_b else ALU.subtract, [k1, k2], wk)
                    b.tt(eng, t1, a_i, b_r, ALU.mult, rk, [k1])
                    b.tt(eng, t2, a_r, b_i, ALU.mult, rk, [k2])
                    b.tt(eng, o_i, t1, t2, ALU.subtract if conj_b else ALU.add, [k1, k2], wk)

                def discretize(d):
                    ar = s5a_s[:, d, 0, :]
                    ai = s5a_s[:, d, 1, :]
                    kin = ['s5a', 's5ls']
                    b.act(T(0), s5ls_s[:, d, :], AF.Exp, kin, [tk(0)])
                    b.tt('dve', T(1), T(0), ar, ALU.mult, [tk(0)] + kin, [tk(1)])
                    b.tt('dve', T(2), T(0), ai, ALU.mult, [tk(0)] + kin, [tk(2)])
                    b.ts('dve', T(3), T(1), 1.0 / 6, 1.0, ALU.mult, ALU.add, [tk(1)], [tk(3)])
                    for k in (5, 4, 3, 2, 1):
                        b.tt('dve', T(3), T(3), T(1), ALU.mult, [tk(3), tk(1)], [tk(3)])
                        b.ts('dve', T(3), T(3), 1.0 / k, 1.0, ALU.mult, ALU.add, [tk(3)], [tk(3)])
                    b.act(T(4), T(2), AF.Sin, [tk(2)], [tk(4)], scale=1.0 / 16)
                    b.act(T(5), T(2), AF.Sin, [tk(2)], [tk(5)], scale=1.0 / 32)
                    b.tt('dve', T(5), T(5), T(5), ALU.mult, [tk(5)], [tk(5)])
                    b.ts('dve', T(5), T(5), -2.0, 1.0, ALU.mult, ALU.add, [tk(5)], [tk(5)])
                    for _ in range(4):
                        b.tt('dve', T(6), T(5), T(5), ALU.mult, [tk(5)], [tk(6)])
                        b.tt('dve', T(7), T(4), T(4), ALU.mult, [tk(4)], [tk(7)])
                        b.stt('dve', T(8), T(5), 2.0, T(4), ALU.mult, ALU.mult, [tk(5), tk(4)], [tk(8)])
                        b.tt('dve', T(5), T(6), T(7), ALU.subtract, [tk(6), tk(7)], [tk(5)])
                        b.copy('dve', T(4), T(8), [tk(8)], [tk(4)])
                    pk_ = ('Pw', d)

                    def P(m, ri):
                        return Pw[:, d, m, ri, :]
                    b.memset('dve', P(0, 0), 1.0, [pk_])
                    b.memset('dve', P(0, 1), 0.0, [pk_])
                    b.tt('dve', P(1, 0), T(3), T(5), ALU.mult, [tk(3), tk(5)], [pk_])
                    b.tt('dve', P(1, 1), T(3), T(4), ALU.mult, [tk(3), tk(4)], [pk_])
                    for m in range(2, 9):
                        cmul('dve', P(m, 0), P(m, 1), P(m - 1, 0), P(m - 1, 1), P(1, 0), P(1, 1), T(6), T(7), tk(6), tk(7), [pk_], [pk_])
                    b.ts('dve', T(6), P(1, 0), -1.0, None, ALU.add, None, [pk_], [tk(6)])
                    b.tt('dve', T(7), ar, ar, ALU.mult, kin, [tk(7)])
                    b.tt('dve', T(8), ai, ai, ALU.mult, kin, [tk(8)])
                    b.tt('dve', T(7), T(7), T(8), ALU.add, [tk(7), tk(8)], [tk(7)])
                    b.recip('dve', T(8), T(7), [tk(7)], [tk(8)])
                    b.tt('dve', T(9), T(6), ar, ALU.mult, [tk(6)] + kin, [tk(9)])
                    b.tt('dve', T(10), P(1, 1), ai, ALU.mult, [pk_] + kin, [tk(10)])
                    b.tt('dve', T(9), T(9), T(10), ALU.add, [tk(9), tk(10)], [tk(9)])
                    b.tt('dve', T(11), T(9), T(8), ALU.mult, [tk(9), tk(8)], [tk(11)])
                    b.tt('dve', T(9), P(1, 1), ar, ALU.mult, [pk_] + kin, [tk(9)])
                    b.tt('dve', T(10), T(6), ai, ALU.mult, [tk(6)] + kin, [tk(10)])
                    b.tt('dve', T(9), T(9), T(10), ALU.subtract, [tk(9), tk(10)], [tk(9)])
                    b.tt('dve', T(12), T(9), T(8), ALU.mult, [tk(9), tk(8)], [tk(12)])
                    t8a = tmp8[:, 0, :].rearrange("p (a b) -> p a b", a=8)
                    t8b = tmp8[:, 1, :].rearrange("p (a b) -> p a b", a=8)
                    cmul('dve', cS[:, d, 0, :, :], cS[:, d, 1, :, :], Pw[:, d, 0:8, 0, :], Pw[:, d, 0:8, 1, :],
                         T(11).unsqueeze(1).to_broadcast([128, 8, 32]), T(12).unsqueeze(1).to_broadcast([128, 8, 32]),
                         t8a, t8b, 'Vt1a', 'Vt1b', [pk_, tk(11), tk(12)], [('cS', d)])
                    b.tt('dve', T(6), T(3), T(3), ALU.mult, [tk(3)], [tk(6)])
                    b.tt('dve', T(6), T(6), T(6), ALU.mult, [tk(6)], [tk(6)])
                    b.tt('dve', rho[:, d, :], T(6), T(6), ALU.mult, [tk(6)], [('rho', d)])
                    b.recip('dve', T(7), rho[:, d, :], [('rho', d)], [tk(7)])

                    def W(k, ri):
                        return wpw[:, k, ri, :]
                    b.tt('dve', W(0, 0), P(8, 0), T(7), ALU.mult, [pk_, tk(7)], ['wpw'])
                    b.stt('dve', W(0, 1), P(8, 1), -1.0, T(7), ALU.mult, ALU.mult, [pk_, tk(7)], ['wpw'])
                    for k in range(8):
                        b.tt('dve', T(8), W(k, 0), W(k, 0), ALU.mult, ['wpw'], [tk(8)])
                        b.tt('dve', T(9), W(k, 1), W(k, 1), ALU.mult, ['wpw'], [tk(9)])
                        b.tt('dve', W(k + 1, 0), T(8), T(9), ALU.subtract, [tk(8), tk(9)], ['wpw'])
                        b.stt('dve', W(k + 1, 1), W(k, 0), 2.0, W(k, 1), ALU.mult, ALU.mult, ['wpw'], ['wpw'])
                    bk = ('Brot', d)
                    b.copy('dve', Brot[:, d, 0, :, 0:1], W(0, 0).unsqueeze(2), ['wpw'], [bk])
                    b.copy('dve', Brot[:, d, 1, :, 0:1], W(0, 1).unsqueeze(2), ['wpw'], [bk])
                    for kk, k in enumerate((1, 2, 4, 8)):
                        ta = tmp8[:, 0, :32 * k].rearrange("p (a b) -> p a b", a=32)
                        tb = tmp8[:, 1, :32 * k].rearrange("p (a b) -> p a b", a=32)
                        cmul('dve', Brot[:, d, 0, :, k:2 * k], Brot[:, d, 1, :, k:2 * k], Brot[:, d, 0, :, 0:k], Brot[:, d, 1, :, 0:k],
                             W(kk, 0).unsqueeze(2).to_broadcast([128, 32, k]), W(kk, 1).unsqueeze(2).to_broadcast([128, 32, k]),
                             ta, tb, 'Vt1a', 'Vt1b', [bk, 'wpw'], [bk])
                    ak = ('Arot', d)
                    b.memset('dve', Arot[:, d, 0, :, 0:1], 1.0, [ak])
                    b.memset('dve', Arot[:, d, 1, :, 0:1], 0.0, [ak])
                    for kk, (k, n) in enumerate(((1, 1), (2, 2), (4, 4), (8, 8), (16, 2))):
                        ta = tmp8[:, 0, :32 * n].rearrange("p (a b) -> p a b", a=32)
                        tb = tmp8[:, 1, :32 * n].rearrange("p (a b) -> p a b", a=32)
                        cmul('dve', Arot[:, d, 0, :, k:k + n], Arot[:, d, 1, :, k:k + n], Arot[:, d, 0, :, 0:n], Arot[:, d, 1, :, 0:n],
                             W(4 + kk, 0).unsqueeze(2).to_broadcast([128, 32, n]), W(4 + kk, 1).unsqueeze(2).to_broadcast([128, 32, n]),
                             ta, tb, 'Vt1a', 'Vt1b', [ak, 'wpw'], [ak])

                discretize(0)
                bg_ops = []
                real_op = S.op
                S.op = lambda eng, fn, reads=(), writes=(): bg_ops.append((eng, fn, reads, writes))
                discretize(1)
                S.op = real_op

                def bg_emit(n):
                    for _ in range(n):
                        if bg_ops:
                            real_op(*bg_ops.pop(0))

                def bd_scatter(dst_fn, src, negate, rk, wk):
                    for half in range(2):
                        ps_ = slice(half * 64, half * 64 + 64)
                        o = dst_fn(ps_, slice(half * 16, half * 16 + 16))
                        if negate:
                            b.act(o, src[ps_], AF.Copy, rk, wk, scale=-1.0)
                        else:
                            b.act(o, src[ps_], AF.Copy, rk, wk)

                def load_bc(kc):
                    kb = kc % 2
                    S.dma('sp', s5b_k[:, kb], s5b[:, :, :, 4 * kc:4 * kc + 4, :], [], [('s5b', kb)])
                    S.dma('sp', s5c_k[:, kb], s5c[:, :, :, 4 * kc:4 * kc + 4, :], [], [('s5c', kb)])

                def gen_tables(d, kc, only_v=False):
                    kb = kc % 2
                    pr = slice(4 * kc, 4 * kc + 4)
                    vr = Vc[:, 0]
                    vi = Vc[:, 1]
                    t1 = Vt1[:, 0]
                    t2 = Vt1[:, 1]
                    crb = cS[:, d, 0, :, pr].rearrange("p d q -> p q d").unsqueeze(3).to_broadcast([128, 4, 8, 16])
                    cib = cS[:, d, 1, :, pr].rearrange("p d q -> p q d").unsqueeze(3).to_broadcast([128, 4, 8, 16])
                    Brb = s5b_k[:, kb, d, 0, :, :].unsqueeze(2).to_broadcast([128, 4, 8, 16])
                    Bib = s5b_k[:, kb, d, 1, :, :].unsqueeze(2).to_broadcast([128, 4, 8, 16])
                    cmul('dve', vr, vi, crb, cib, Brb, Bib, t1, t2, 'Vt1a', 'Vt1b', [('cS', d), ('s5b', kb)], ['Vc'])
                    bd_scatter(lambda p_, c_: Vbd[p_, d, :, :, 0, c_], vr, False, ['Vc'], [('Vbd', d)])
                    bd_scatter(lambda p_, c_: Vbd[p_, d, :, :, 1, c_], vi, False, ['Vc'], [('Vbd', d)])
                    if only_v:
                        return
                    bd_scatter(lambda p_, c_: Cbd[p_, d, :, 0, c_], s5c_k[:, kb, d, 0, :, :], False, [('s5c', kb)], [('Cbd', d)])
                    bd_scatter(lambda p_, c_: Cbd[p_, d, :, 1, c_], s5c_k[:, kb, d, 1, :, :], True, [('s5c', kb)], [('Cbd', d)])

                def gen_E(d, kc):
                    kb = kc % 2
                    pr = slice(4 * kc, 4 * kc + 4)
                    vr = Vc[:, 0]
                    vi = Vc[:, 1]
                    t1 = Vt1[:, 0]
                    t2 = Vt1[:, 1]
                    prb = Pw[:, d, 1:9, 0, pr].rearrange("p m q -> p q m").unsqueeze(3).to_broadcast([128, 4, 8, 16])
                    pib = Pw[:, d, 1:9, 1, pr].rearrange("p m q -> p q m").unsqueeze(3).to_broadcast([128, 4, 8, 16])
                    Crb = s5c_k[:, kb, d, 0, :, :].unsqueeze(2).to_broadcast([128, 4, 8, 16])
                    Cib = s5c_k[:, kb, d, 1, :, :].unsqueeze(2).to_broadcast([128, 4, 8, 16])
                    cmul('dve', vr, vi, prb, pib, Crb, Cib, t1, t2, 'Vt1a', 'Vt1b', [('Pw', d), ('s5c', kb)], ['Vc'])
                    bd_scatter(lambda p_, c_: Ebd[p_, d, :, :, 0, c_], vr, False, ['Vc'], [('Ebd', d)])
                    bd_scatter(lambda p_, c_: Ebd[p_, d, :, :, 1, c_], vi, True, ['Vc'], [('Ebd', d)])

                PTb = PS[6][:].bitcast(BF16)
                pcnt = [0]

                kin4_p = [hT[:, p_, 0:256].bitcast(BF16).rearrange("p (a n) -> p a n", a=4) for p_ in range(4)]
                for p_ in range(4):
                    b.memset('pool', hT[:, p_, 0:256], 0.0, [('hT', 0, p_), ('Kin4', p_ // 2)])

                def kin4(d, dl):
                    return kin4_p[d * 2 + dl // 4][:, dl % 4, :]

                def kin_prologue(kc):
                    for d in range(2):
                        for q in range(4):
                            rows = slice(32 * q, 32 * q + 32)

                            def fnk(e, d=d, q=q, rows=rows):
                                ins = None
                                for dl in range(8):
                                    o = PS[7][rows, dl * 32:(dl + 1) * 32]
                                    e.matmul(o, Vbd[:, d, q, dl, 0, :], Cbd[:, d, q, 0, :], start=True, stop=False, tile_position=(0, 32 * q))
                                    ins = e.matmul(o, Vbd[:, d, q, dl, 1, :], Cbd[:, d, q, 1, :], start=False, stop=True, tile_position=(0, 32 * q))
                                return ins
                            S.op('pe', fnk, [('Vbd', d), ('Cbd', d)], [pk(7)])
                        for q in range(4):
                            rows = slice(32 * q, 32 * q + 32)
                            for hf in range(2):
                                b.copy('act', kin4_p[d * 2 + hf][rows, :, 32 * q:32 * q + 32],
                                       PS[7][rows, hf * 128:hf * 128 + 128].rearrange("p (a n) -> p a n", a=4), [pk(7)], [('Kin4', d)])

                def intra_prologue(kc):
                    ukeys = [('ubuf', gi, kc) for gi in range(5)]

                    def fni(e):
                        ins = None
                        for d in range(2):
                            for j in range(8):
                                o = PS[j // 2][:, (j % 2) * 256:(j % 2) * 256 + 256]
                                js = list(range(0, j + 1)) if d == 0 else list(range(j, 8))
                                for n_, jp in enumerate(js):
                                    ins = e.matmul(o, kin4(d, abs(j - jp)), ubuf[:, kc, jp * 288 + 32:jp * 288 + 288],
                                                   start=(d == 0 and j % 2 == 0 and n_ == 0), stop=False)
                        return ins
                    S.op('pe', fni, [('Kin4', 0), ('Kin4', 1)] + ukeys, [pk(0), pk(1), pk(2), pk(3)])

                def u_geom(d, kc, q):
                    return 4 * kc + q, slice(32 * q, 32 * q + 32), (288 if d == 0 else 256), (0 if d == 0 else NCTX)

                def u_front(d, kc, q, iu, need_out):
                    pair, rows, C, col0 = u_geom(d, kc, q)
                    wb = iu % 2
                    for half in range(2):
                        def fn(e, half=half):
                            ins = None
                            for dl in range(4):
                                for ri in range(2):
                                    idx = dl * 2 + ri
                                    ins = e.transpose(PTb[rows, idx * 128:(idx + 1) * 128], Vbd[:, d, q, half * 4 + dl, ri, :], idb[:],
                                                      tile_position=(0, 32 * q))
                            return ins
                        S.op('pe', fn, [('Vbd', d), 'idb'], [pk(6)])
                        b.copy('act', Wst[rows, wb, half * 4:half * 4 + 4, :, :],
                               PTb[rows, :].rearrange("p (a r n) -> p a r n", a=4, r=2), [pk(6)], [('Wst', wb)])

                def u_rot(d, kc, q, iu):
                    pair, rows, C, col0 = u_geom(d, kc, q)
                    rb = iu % 2
                    na = C // 16
                    rv = lambda ri: rot[:, rb, ri, :C].rearrange("p (a b) -> p a b", b=16)
                    cmul('pool', rv(0), rv(1),
                         Arot[:, d, 0, pair, 0:na].unsqueeze(2).to_broadcast([128, na, 16]), Arot[:, d, 1, pair, 0:na].unsqueeze(2).to_broadcast([128, na, 16]),
                         Brot[:, d, 0, pair, :].unsqueeze(1).to_broadcast([128, na, 16]), Brot[:, d, 1, pair, :].unsqueeze(1).to_broadcast([128, na, 16]),
                         rt2[:, 1, 0, :C].rearrange("p (a b) -> p a b", b=16), rt2[:, 1, 1, :C].rearrange("p (a b) -> p a b", b=16),
                         ('rt2a', 'pool'), ('rt2b', 'pool'), [('Arot', d), ('Brot', d)], [('rot', rb)])

                def u_states(d, kc, q, iu):
                    pair, rows, C, col0 = u_geom(d, kc, q)
                    wb = iu % 2
                    ukeys = [('ubuf', gi, kc) for gi in range(5)]
                    for ri in range(2):
                        def fns(e, ri=ri):
                            ins = None
                            for j in range(8):
                                dl = (7 - j) if d == 0 else j
                                ins = e.matmul(PS[4 + ri][:, :C], Wst[rows, wb, dl, ri, :], ubuf[rows, kc, j * 288 + col0 // 8:j * 288 + col0 // 8 + C],
                                               start=(j == 0), stop=(j == 7), tile_position=(32 * q, 0))
                            return ins
                        S.op('pe', fns, [('Wst', wb)] + ukeys, [pk(4 + ri)])

                def u_scan(d, kc, q, iu):
                    pair, rows, C, col0 = u_geom(d, kc, q)
                    wb = iu % 2
                    if d == 0:
                        Sr, Si = PS[4][:, :C], PS[5][:, :C]
                    else:
                        Sr, Si = PS[4][:, C - 1::-1], PS[5][:, C - 1::-1]
                    R0, R1 = rot[:, wb, 0, :C], rot[:, wb, 1, :C]
                    tr, ti = trti[:, 0, 0, :C], trti[:, 0, 1, :C]
                    ta, tb = gg[:, wb, 0, :C], gg[:, wb, 1, :C]
                    kr_ = [pk(4), pk(5), ('rot', wb)]
                    b.tt('dve', ta, Sr, R0, ALU.mult, kr_, [('gg', wb, 0)])
                    b.tt('dve', tb, Si, R1, ALU.mult, kr_, [('gg', wb, 1)])
                    b.tt('dve', tr, ta, tb, ALU.subtract, [('gg', wb, 0), ('gg', wb, 1)], [('trti', 0)])
                    b.tt('dve', ta, Si, R0, ALU.mult, kr_, [('gg', wb, 0)])
                    b.tt('dve', tb, Sr, R1, ALU.mult, kr_, [('gg', wb, 1)])
                    b.tt('dve', ti, ta, tb, ALU.add, [('gg', wb, 0), ('gg', wb, 1)], [('trti', 1)])
                    rho_b = rho[:, d, pair:pair + 1].to_broadcast([128, C])
                    if d == 0:
                        i_r, i_i, ik = 0.0, 0.0, []
                    else:
                        i_r, i_i, ik = rin_s[:, pair, 0:1], rin_s[:, pair, 1:2], ['rin']
                    gr, gi_ = gg[:, wb, 0, :C], gg[:, wb, 1, :C]
                    S.op('dve', lambda e: e.tensor_tensor_scan(gr, rho_b, tr, i_r, ALU.mult, ALU.add),
                         [('rho', d), ('trti', 0)] + ik, [('gg', wb, 0)])
                    S.op('dve', lambda e: e.tensor_tensor_scan(gi_, rho_b, ti, i_i, ALU.mult, ALU.add),
                         [('rho', d), ('trti', 1)] + ik, [('gg', wb, 1)])

                def u_final(d, kc, q, iu):
                    pair, rows, C, col0 = u_geom(d, kc, q)
                    wb = iu % 2
                    R0, R1 = rot[:, wb, 0, :C], rot[:, wb, 1, :C]
                    gr, gi_ = gg[:, wb, 0, :C], gg[:, wb, 1, :C]
                    L = slice(C - 1, C)
                    b.tt('dve', T(13)[:, 0:1], R0[:, L], gr[:, L], ALU.mult, [('rot', wb), ('gg', wb, 0)], [tk(13)])
                    b.tt('dve', T(13)[:, 1:2], R1[:, L], gi_[:, L], ALU.mult, [('rot', wb), ('gg', wb, 1)], [tk(13)])
                    b.tt('dve', fx_s[:, pair, 0:1], T(13)[:, 0:1], T(13)[:, 1:2], ALU.add, [tk(13)], ['fx'])
                    b.tt('dve', T(13)[:, 2:3], R0[:, L], gi_[:, L], ALU.mult, [('rot', wb), ('gg', wb, 1)], [tk(13)])
                    b.tt('dve', T(13)[:, 3:4], R1[:, L], gr[:, L], ALU.mult, [('rot', wb), ('gg', wb, 0)], [tk(13)])
                    b.tt('dve', fx_s[:, pair, 1:2], T(13)[:, 2:3], T(13)[:, 3:4], ALU.subtract, [tk(13)], ['fx'])

                def u_unrot(d, kc, q, iu):
                    pair, rows, C, col0 = u_geom(d, kc, q)
                    wb = iu % 2
                    hb = (iu // 2) % 2
                    R0, R1 = rot[:, wb, 0, :C], rot[:, wb, 1, :C]
                    gr, gi_ = gg[:, wb, 0, :C], gg[:, wb, 1, :C]
                    if d == 0:
                        o_r, o_i = Hp[:, d, hb, 0, 1:C], Hp[:, d, hb, 1, 1:C]
                    else:
                        o_r, o_i = Hp[:, d, hb, 0, C - 2::-1], Hp[:, d, hb, 1, C - 2::-1]
                    n1 = C - 1
                    hk = ('Hp', d, hb)
                    e2 = 'dve' if d == 0 else 'pool'
                    ka, kb_ = ('rt2a', e2), ('rt2b', e2)
                    r2 = rt2[:, 0 if d == 0 else 1]
                    b.tt(e2, r2[:, 0, :n1], R0[:, :n1], gr[:, :n1], ALU.mult, [('rot', wb), ('gg', wb, 0)], [ka])
                    b.tt(e2, r2[:, 1, :n1], R1[:, :n1], gi_[:, :n1], ALU.mult, [('rot', wb), ('gg', wb, 1)], [kb_])
                    b.tt(e2, o_r, r2[:, 0, :n1], r2[:, 1, :n1], ALU.add, [ka, kb_], [hk])
                    b.tt(e2, r2[:, 0, :n1], R0[:, :n1], gi_[:, :n1], ALU.mult, [('rot', wb), ('gg', wb, 1)], [ka])
                    b.tt(e2, r2[:, 1, :n1], R1[:, :n1], gr[:, :n1], ALU.mult, [('rot', wb), ('gg', wb, 0)], [kb_])
                    b.tt(e2, o_i, r2[:, 0, :n1], r2[:, 1, :n1], ALU.subtract, [ka, kb_], [hk])
                    if d == 1:
                        b.copy('pool', Hp[:, d, hb, 0, C - 1:C], rin_s[:, pair, 0:1], ['rin'], [hk])
                        b.copy('pool', Hp[:, d, hb, 1, C - 1:C], rin_s[:, pair, 1:2], ['rin'], [hk])

                def u_out(d, kc, q, iu, first_dir, last_dir):
                    pair, rows, C, col0 = u_geom(d, kc, q)
                    wb = iu % 2
                    hb = (iu // 2) % 2
                    hk = ('Hp', d, hb)
                    hoff = 32 if d == 0 else 0
                    ukeys = [('ubuf', gi, kc) for gi in range(5)]

                    def fno(e):
                        ins = None
                        for j in range(8):
                            o = PS[j // 2][rows, (j % 2) * 256:(j % 2) * 256 + 256]
                            mi = j if d == 0 else 7 - j
                            e.matmul(o, Ebd[:, d, q, mi, 0, :], Hp[:, d, hb, 0, hoff:hoff + 256], start=False, stop=False, tile_position=(0, 32 * q))
                            ins = e.matmul(o, Ebd[:, d, q, mi, 1, :], Hp[:, d, hb, 1, hoff:hoff + 256], start=False,
                                           stop=(last_dir and q == 3), tile_position=(0, 32 * q))
                        return ins
                    S.op('pe', fno, [('Ebd', d), hk] + ukeys, [pk(0), pk(1), pk(2), pk(3)])

                unitsA = [(0, kc, q) for kc in range(KC) for q in range(4)]
                load_bc(0)
                gen_tables(0, 0, only_v=True)
                u_front(*unitsA[0], 0, False)
                u_rot(*unitsA[0], 0)
                u_states(*unitsA[0], 0)
                for i, u in enumerate(unitsA):
                    if i + 1 < len(unitsA):
                        un = unitsA[i + 1]
                        if un[1] != u[1]:
                            load_bc(un[1])
                            gen_tables(0, un[1], only_v=True)
                        u_front(*un, i + 1, False)
                        u_rot(*un, i + 1)
                    u_scan(*u, i)
                    bg_emit(4)
                    if i + 1 < len(unitsA):
                        u_states(*unitsA[i + 1], i + 1)
                    u_final(*u, i)
                    bg_emit(4)
                bg_emit(len(bg_ops))
                fxf = fx_s[:].rearrange("p a b -> p (a b)")
                for sl in range(2):
                    b.ts('dve', ccb[:, sl, :], fxf, pm_s[:, sl:sl + 1], None, ALU.mult, None, ['fx', 'pmask'], ['ccb'])
                S.dma('pool', cc_in.ap().opt() if False else cc_in[:, :], ccb[:].rearrange("p a b -> p (a b)"), ['ccb'], ['cc_in'])
                S.collective(lambda e: e.collective_compute("AllReduce", ALU.add, replica_groups=[[0, 1], [2, 3], [4, 5], [6, 7]],
                                                            ins=[cc_in.ap().opt()], outs=[cc_out.ap().opt()]), ['cc_in'], ['cc_out'])
                S.dma('sp', ccb2[:].rearrange("p a b -> p (a b)"), cc_out[:, :], ['cc_out'], ['ccb2'])

                def recv_rin():
                    rinf = rin_s[:].rearrange("p a b -> p (a b)")
                    b.ts('dve', rinf, ccb2[:, 0, :], pm_s[:, 1:2], None, ALU.mult, None, ['ccb2', 'pmask'], ['rin'])
                    b.stt('dve', rinf, ccb2[:, 1, :], pm_s[:, 0:1], rinf, ALU.mult, ALU.add, ['ccb2', 'pmask', 'rin'], ['rin'])

                unitsB = [(d, kc, q) for kc in range(KC) for q in range(4) for d in range(2)]

                def z_step(kc):
                    hv = hT[:, kc, NCTX:NQ].rearrange("p (c j) -> p j c", j=8)
                    zv = ubuf[:, kc, NCTX:NQ].rearrange("p (c j) -> p j c", j=8)
                    for bk_ in range(4):
                        zb = bk_ % 2
                        b.stt('dve', t32z[:, zb, :, :], hv[:, 2 * bk_:2 * bk_ + 2, :], dAB[:, 0, kc:kc + 1],
                              PS[bk_][:, :].rearrange("p (j c) -> p j c", j=2), ALU.mult, ALU.add,
                              [pk(bk_), 'dAB'] + [('hT', gi, kc) for gi in range(1, 5)], [('t32z', zb)])
                        if stop_after == 'p5':
                            b.act(hv[:, 2 * bk_:2 * bk_ + 2, :], t32z[:, zb, :, :], AF.Identity, [('t32z', zb), 'dAB'], [('hT', gi, kc) for gi in range(1, 5)],
                                  bias=dAB[:, 1, kc:kc + 1])
                        else:
                            b.act(zv[:, 2 * bk_:2 * bk_ + 2, :], t32z[:, zb, :, :], AF.Gelu_apprx_tanh, [('t32z', zb), 'dAB'], [('ubuf', gi, kc) for gi in range(1, 5)],
                                  bias=dAB[:, 1, kc:kc + 1])

                load_bc(0)
                for d_ in range(2):
                    gen_tables(d_, 0)
                    gen_E(d_, 0)
                kin_prologue(0)
                intra_prologue(0)
                u_front(*unitsB[0], 0, True)
                u_rot(*unitsB[0], 0)
                u_states(*unitsB[0], 0)
                for i, u in enumerate(unitsB):
                    nxt_kc = None
                    if i + 1 < len(unitsB):
                        un = unitsB[i + 1]
                        if un[1] != u[1]:
                            nxt_kc = un[1]
                            load_bc(nxt_kc)
                            gen_tables(0, nxt_kc)
                            gen_tables(1, nxt_kc)
                            kin_prologue(nxt_kc)
                        u_front(*un, i + 1, True)
                        u_rot(*un, i + 1)
                    if i == 1:
                        recv_rin()
                    u_scan(*u, i)
                    if i + 1 < len(unitsB):
                        u_states(*unitsB[i + 1], i + 1)
                    u_unrot(*u, i)
                    u_out(*u, i, u[0] == 0, u[0] == 1)
                    if u[0] == 1 and u[2] == 3:
                        z_step(u[1])
                    if nxt_kc is not None:
                        intra_prologue(nxt_kc)
                        gen_E(0, nxt_kc)
                        gen_E(1, nxt_kc)
                S.flush()

            p6 = ExitStack()
            if stop_after != 'p5':
                wga = sb("wga", [128, KC, 1024], BF16, p6)
                wgb = sb("wgb", [128, KC, 1024], BF16, p6)
                sgb = sb("sgb", [128, 2, 512], F32, p6)
                ogb = sb("ogb", [128, 2, 512], F32, p6)
                lntmp = ln_tmps(p6, "b6")
                S.dma('pool', wga[:], glu_a.rearrange("(kc p) n -> p kc n", p=128), [], ['wga'])
                S.dma('pool', wgb[:], glu_b.rearrange("(kc p) n -> p kc n", p=128), [], ['wgb'])
                cnt6 = [0]

                def glu6(ti):
                    (c0, N, s) = own_groups[ti]
                    gi = own_gidx[ti]
                    zkeys = [('ubuf', gi, kc) for kc in range(KC)]
                    for oc in range(KC):
                        pa = (cnt6[0] % 2) * 2
                        q6 = cnt6[0] % 2
                        cnt6[0] += 1
                        b.mm([(PS[pa][:, :N], wga[:, kc, oc * 128:(oc + 1) * 128], ubuf[:, kc, c0:c0 + N], kc == 0, kc == KC - 1) for kc in range(KC)],
                             ['wga'] + zkeys, [pk(pa)])
                        b.mm([(PS[pa + 1][:, :N], wgb[:, kc, oc * 128:(oc + 1) * 128], ubuf[:, kc, c0:c0 + N], kc == 0, kc == KC - 1) for kc in range(KC)],
                             ['wgb'] + zkeys, [pk(pa + 1)])
                        b.act(sgb[:, q6, :N], PS[pa + 1][:, :N], AF.Sigmoid, [pk(pa + 1)], [('sgb', q6)])
                        b.stt('dve', ogb[:, q6, :N], PS[pa][:, :N], pvs(('gam', 1), oc, 0), sgb[:, q6, :N], ALU.mult, ALU.mult,
                              [pk(pa), 'pv', ('sgb', q6)], [('ogb', q6)])
                        b.tt('pool', hT[:, oc, c0:c0 + N], ogb[:, q6, :N], hT[:, oc, c0:c0 + N], ALU.add, [('ogb', q6), ('hT', gi, oc)], [('hT', gi, oc)])
                glu6(0)
                for ti, (c0, N, s) in enumerate(own_groups):
                    gi = own_gidx[ti]
                    if ti + 1 < len(own_groups):
                        glu6(ti + 1)
                    ln_norm(lambda kc, c0=c0, N=N: hT[:, kc, c0:c0 + N], N, 0, [('hT', gi, kc) for kc in range(KC)],
                            g1=lambda kc: lng_s[:, 2, kc:kc + 1], b1=lambda kc: lnb_s[:, 2, kc:kc + 1],
                            out1_fn=lambda kc, c0=c0, N=N: hT[:, kc, c0:c0 + N], out1_keys=[('hT', gi, kc) for kc in range(KC)],
                            g2=lambda kc: pvs(('gU', 1, 0), kc, 0), b2=lambda kc: pvs(('bU', 1, 0), kc, 0),
                            out2_fn=lambda kc, c0=c0, N=N: ubuf[:, kc, c0:c0 + N], out2_keys=[('ubuf', gi, kc) for kc in range(KC)],
                            tmp=lntmp, psA=4, psB=5)
                S.flush()
            p6.close()

        if lvl >= 2:
            with ExitStack() as p7:
                combT = sb("combT", [8, NOWN], F32, p7)
                cbt = sb("cbt", [128, NOWN], F32, p7)
                sel_s = sb("sel_s", [8, 8, 128], F32, p7)
                S.dma('sp', sel_s[:], sel[:], [], ['sel'])
                with ExitStack() as p7a:
                    wr_s = sb("wr_s", [128, KC, 8], F32, p7a)
                    idf = sb("idf", [128, 128], F32, p7a)
                    u32 = sb("u32", [128, 2, KC, 128], F32, p7a)
                    lg = sb("lg", [128, 2, 8], F32, p7a)
                    mx = sb("mx", [128, 2, 8], F32, p7a)
                    msk = sb("msk", [128, 2, 8], F32, p7a)
                    ex = sb("ex", [128, 2, 8], F32, p7a)
                    den = sb("den", [128, 2, 2], F32, p7a)
                    cmb = sb("cmb", [128, 2, 8], F32, p7a)
                    S.dma('sp', wr_s[:], wrt[:], [], ['wr'])
                    S.dma('sp', idf[:], identf[:], [], ['idf'])
                    for tp in range(8):
                        tiles = [(2 * tp + tb, tb) for tb in range(2)]
                        for (t, tb) in tiles:
                            c0 = NCTX + 128 * t
                            gi = 1 + t // 4
                            for kc in range(KC):
                                eng = 'dve' if kc % 2 == 0 else 'pool'
                                b.ts(eng, u32[:, tb, kc, :], hT[:, kc, c0:c0 + 128], pvs(('s2p', 1), kc, 0), modv(1, 3, kc, 0), ALU.mult, ALU.add,
                                     [('hT', gi, kc), 'pv', ('modT', 1)], [('u32', tb, kc)])
                        for (t, tb) in tiles:
                            b.mm([(PS[tb][:, 0:8], u32[:, tb, kc, :], wr_s[:, kc, :], kc == 0, kc == KC - 1) for kc in range(KC)],
                                 ['wr'] + [('u32', tb, kc) for kc in range(KC)], [pk(tb)])
                        for (t, tb) in tiles:
                            b.copy('act', lg[:, tb, :], PS[tb][:, 0:8], [pk(tb)], [('lg', tb)])
                        for (t, tb) in tiles:
                            S.op('dve', lambda e, tb=tb: e.max(mx[:, tb, :], lg[:, tb, :]), [('lg', tb)], [('mx', tb)])
                        for (t, tb) in tiles:
                            b.ts('dve', msk[:, tb, :], lg[:, tb, :], mx[:, tb, 1:2], None, ALU.is_ge, None, [('lg', tb), ('mx', tb)], [('msk', tb)])
                        for (t, tb) in tiles:
                            b.ts('dve', ex[:, tb, :], lg[:, tb, :], mx[:, tb, 0:1], None, ALU.subtract, None, [('lg', tb), ('mx', tb)], [('ex', tb)])
                        for (t, tb) in tiles:
                            b.act(ex[:, tb, :], ex[:, tb, :], AF.Exp, [('ex', tb)], [('ex', tb)])
                        for (t, tb) in tiles:
                            b.tt('dve', ex[:, tb, :], ex[:, tb, :], msk[:, tb, :], ALU.mult, [('ex', tb), ('msk', tb)], [('ex', tb)])
                        for (t, tb) in tiles:
                            S.op('dve', lambda e, tb=tb: e.reduce_sum(den[:, tb, 0:1], ex[:, tb, :], axis=mybir.AxisListType.X), [('ex', tb)], [('den', tb)])
                        for (t, tb) in tiles:
                            b.recip('dve', den[:, tb, 1:2], den[:, tb, 0:1], [('den', tb)], [('den2', tb)])
                        for (t, tb) in tiles:
                            b.ts('dve', cmb[:, tb, :], ex[:, tb, :], den[:, tb, 1:2], None, ALU.mult, None, [('ex', tb), ('den2', tb)], [('cmb', tb)])
                        for (t, tb) in tiles:
                            S.op('pe', lambda e, tb=tb: e.transpose(PS[2 + tb][0:8, 0:128], cmb[:, tb, :], idf[:]), [('cmb', tb), 'idf'], [pk(2 + tb)])
                        for (t, tb) in tiles:
                            b.copy('act', combT[:, t * 128:(t + 1) * 128], PS[2 + tb][0:8, 0:128], [pk(2 + tb)], ['combT'])
                    S.flush()
                p7b = ExitStack()
                if stop_after != 'p7a':
                    w1s = sb("w1m", [128, 3, KC, 256], BF16, p7b)
                    w3s = sb("w3m", [128, 3, KC, 256], BF16, p7b)
                    w2s = sb("w2m", [128, 3, 2, 1024], BF16, p7b)
                    s1b = sb("s1m", [128, 2, 512], F32, p7b)
                    t32 = sb("t32m", [128, 2, 512], F32, p7b)
                    gT = sb("gTm", [128, 2, 2, 512], BF16, p7b)

                    def cb_prepare(e):
                        for tg in range(4):
                            pb = 4 + tg
                            b.mm([(PS[pb][:, :512], sel_s[:, e, :], combT[:, tg * 512:(tg + 1) * 512], True, True)], ['sel', 'combT'], [pk(pb)])
                            b.copy('act', cbt[:, tg * 512:(tg + 1) * 512], PS[pb][:, :512], [pk(pb)], ['cbt'])
                    experts = [(moe_w1[e], moe_w3[e], moe_w2[e], DFE, e) for e in range(NEXP)]
                    swiglu_stages(nc, S, b, PS, pk, own_groups, own_gidx, ubuf, hT, experts, (w1s, w3s, w2s, s1b, gT, t32),
                                  lambda oc, s: pvs(('gaf', 1), oc, 0), NF=2, cb=(cb_prepare, cbt), NWB=3)
                    S.flush()
                p7b.close()
                p7c = ExitStack()
                if stop_after not in ('p7a', 'p7b'):
                    lntmp = ln_tmps(p7c, "b7")
                    for ti, (c0, N, s) in enumerate(own_groups):
                        gi = own_gidx[ti]
                        ln_norm(lambda kc, c0=c0, N=N: hT[:, kc, c0:c0 + N], N, 0, [('hT', gi, kc) for kc in range(KC)],
                                g1=lambda kc: lng_s[:, 3, kc:kc + 1], b1=lambda kc: lnb_s[:, 3, kc:kc + 1],
                                out1_fn=lambda kc, c0=c0, N=N: hT[:, kc, c0:c0 + N], out1_keys=[('hT', gi, kc) for kc in range(KC)],
                                tmp=lntmp)
                    S.flush()
                p7c.close()


        toks = []
        if debug:
            toks.append(S.dma('sp', dbg[:], hT[:], [('hT', gi, kc) for gi in range(5) for kc in range(KC)], []))
        toks.append(S.dma('sp', yout[:], hT[:, :, NCTX:NQ], [('hT', gi, kc) for gi in range(5) for kc in range(KC)], []))
        S.flush(final_tokens=toks)
    return nc


def swiglu_stages(nc, S, b, PS, pk, tgroups, gidx, ubuf, hT, experts, bufs, gate_fn, NF=4, cb=None, NWB=2):
    w1s, w3s, w2s, s1b, gT, t32 = bufs
    stages = []
    for (w1, w3, w2, F, e) in experts:
        nfc = F // 128
        f0 = 0
        first = True
        while f0 < nfc:
            nf = min(NF, nfc - f0)
            stages.append((w1, w3, w2, f0, nf, e, first))
            first = False
            f0 += nf

    def load(si):
        (w1, w3, w2, f0, nf, e, first) = stages[si]
        q = si % NWB
        c0f = f0 * 128
        S.dma('pool', w1s[:, q, :, :nf * 128], w1[:, c0f:c0f + nf * 128].rearrange("(kc p) n -> p kc n", p=128), [], [('w1s', q)])
        S.dma('pool', w3s[:, q, :, :nf * 128], w3[:, c0f:c0f + nf * 128].rearrange("(kc p) n -> p kc n", p=128), [], [('w3s', q)])
        S.dma('pool', w2s[:, q, :nf, :], w2[c0f:c0f + nf * 128, :].rearrange("(fc p) n -> p fc n", p=128), [], [('w2s', q)])

    items = [(si, ti) for si in range(len(stages)) for ti in range(len(tgroups))]
    hc = [0]

    def H(i):
        si, ti = items[i]
        (w1, w3, w2, f0, nf, e, first) = stages[si]
        q = si % NWB
        if ti == 0:
            if si == 0:
                for k in range(min(NWB - 1, len(stages))):
                    load(k)
            if cb is not None and first:
                cb[0](e)
        (c0, N, s) = tgroups[ti]
        gi = gidx[ti]
        gq_ = i % 2
        ukeys = [('ubuf', gi, kc) for kc in range(KC)]
        for fc in range(nf):
            p1 = (hc[0] % 2) * 2
            sq_ = hc[0] % 2
            hc[0] += 1
            b.mm([(PS[p1][:, :N], w1s[:, q, kc, fc * 128:(fc + 1) * 128], ubuf[:, kc, c0:c0 + N], kc == 0, kc == KC - 1) for kc in range(KC)],
                 [('w1s', q)] + ukeys, [pk(p1)])
            b.mm([(PS[p1 + 1][:, :N], w3s[:, q, kc, fc * 128:(fc + 1) * 128], ubuf[:, kc, c0:c0 + N], kc == 0, kc == KC - 1) for kc in range(KC)],
                 [('w3s', q)] + ukeys, [pk(p1 + 1)])
            b.act(s1b[:, sq_, :N], PS[p1][:, :N], AF.Silu, [pk(p1)], [('s1b', sq_)])
            if cb is None:
                b.tt('dve', gT[:, gq_, fc, :N], PS[p1 + 1][:, :N], s1b[:, sq_, :N], ALU.mult, [pk(p1 + 1), ('s1b', sq_)], [('gT', gq_, fc)])
            else:
                b.tt('dve', t32[:, sq_, :N], PS[p1 + 1][:, :N], s1b[:, sq_, :N], ALU.mult, [pk(p1 + 1), ('s1b', sq_)], [('t32', sq_)])
                b.tt('pool', gT[:, gq_, fc, :N], t32[:, sq_, :N], cb[1][:, ti * 512:ti * 512 + N], ALU.mult,
                     [('t32', sq_), 'cbt'], [('gT', gq_, fc)])

    def W2(i):
        si, ti = items[i]
        (w1, w3, w2, f0, nf, e, first) = stages[si]
        q = si % NWB
        (c0, N, s) = tgroups[ti]
        gi = gidx[ti]
        gq_ = i % 2
        if ti == 0 and si + NWB - 1 < len(stages):
            load(si + NWB - 1)
        for oc in range(KC):
            pb = 4 + (oc % 4)
            b.mm([(PS[pb][:, :N], w2s[:, q, fc, oc * 128:(oc + 1) * 128], gT[:, gq_, fc, :N], fc == 0, fc == nf - 1) for fc in range(nf)],
                 [('w2s', q)] + [('gT', gq_, fc) for fc in range(nf)], [pk(pb)])
            b.stt('dve', hT[:, oc, c0:c0 + N], PS[pb][:, :N], gate_fn(oc, s), hT[:, oc, c0:c0 + N], ALU.mult, ALU.add,
                  [pk(pb), 'pv', ('hT', gi, oc)], [('hT', gi, oc)])

    H(0)
    for i in range(len(items)):
        if i + 1 < len(items):
            H(i + 1)
        W2(i)


def _fm(a):
    n, d = a.shape
    return np.ascontiguousarray(a.reshape(n, d // 128, 128).transpose(2, 1, 0))


def _vec(a):
    return np.ascontiguousarray(a.reshape(-1, 128).T)


def _rope_tables(tok_idx):
    pairs = 16
    freqs = (10000.0 ** (-np.arange(pairs, dtype=np.float32) / pairs)).astype(np.float32)
    row = (tok_idx // 64).astype(np.float32)
    col = (tok_idx % 64).astype(np.float32)
    ang = np.stack([row[:, None] * freqs, col[:, None] * freqs], axis=1).astype(np.float32)
    c = np.cos(ang).astype(np.float32)
    s = np.sin(ang).astype(np.float32)
    n = len(tok_idx)
    cos_t = np.zeros((64, n), np.float32)
    sin_t = np.zeros((64, n), np.float32)
    for ax in range(2):
        for half in range(2):
            d0 = ax * 32 + half * 16
            cos_t[d0:d0 + 16] = c[:, ax, :].T
            sin_t[d0:d0 + 16] = (-s[:, ax, :].T) if half == 0 else s[:, ax, :].T
    return cos_t, sin_t


_SWAP = np.array([ax * 32 + (1 - half) * 16 + p for ax in range(2) for half in range(2) for p in range(16)])


def make_in_maps(inp):
    f = lambda k: np.asarray(inp[k], dtype=np.float32)
    x, c, ctx, c_ctx = f("x"), f("c"), f("ctx"), f("c_ctx")
    shared = {}
    shared["ada_w"] = np.ascontiguousarray(f("ada_w"))
    ab = f("ada_b")
    shared["adab"] = np.ascontiguousarray(ab.reshape(2, 48, 128).transpose(2, 0, 1))
    shared["lng"] = np.ascontiguousarray(f("ln_g").reshape(4, KC, 128).transpose(2, 0, 1))
    shared["lnb"] = np.ascontiguousarray(f("ln_b").reshape(4, KC, 128).transpose(2, 0, 1))
    wd = f("mla_w_down")[0]
    wd_ext = np.concatenate([wd, wd[:, 640 + _SWAP]], axis=1)
    shared["wdown"] = np.ascontiguousarray(wd_ext.reshape(KC, 128, 768).transpose(1, 0, 2))
    shared["gq"] = _vec(f("mla_g_q")[0])
    shared["gkv"] = _vec(f("mla_g_kv")[0])
    wq = f("mla_w_uq")[0]
    wq_ext = np.concatenate([wq, wq[:, :, 128 + _SWAP]], axis=2)
    shared["wuq"] = np.ascontiguousarray(wq_ext.reshape(3, 128, 8 * 256).transpose(1, 0, 2))
    shared["wuk"] = np.ascontiguousarray(f("mla_w_uk")[0].reshape(2, 128, 1024).transpose(1, 0, 2))
    shared["wuv"] = np.ascontiguousarray(f("mla_w_uv")[0].reshape(2, 128, 1024).transpose(1, 0, 2))
    shared["wo"] = np.ascontiguousarray(f("mla_w_o")[0].reshape(8, 128, 1024).transpose(1, 0, 2))
    shared["ffn_w1"] = np.ascontiguousarray(f("ffn_w1")[0])
    shared["ffn_w3"] = np.ascontiguousarray(f("ffn_w3")[0])
    shared["ffn_w2"] = np.ascontiguousarray(f("ffn_w2")[0])
    shared["glu_a"] = np.ascontiguousarray(f("s5_w_glu_a")[0])
    shared["glu_b"] = np.ascontiguousarray(f("s5_w_glu_b")[0])
    shared["s5d"] = _vec(f("s5_d")[0])
    shared["ident"] = np.eye(128, dtype=np.float32)
    shared["identf"] = np.eye(128, dtype=np.float32)
    shared["wrt"] = np.ascontiguousarray(f("moe_w_router")[0].reshape(KC, 128, NEXP).transpose(1, 0, 2))
    selm = np.zeros((8, 8, 128), np.float32)
    for e in range(8):
        selm[e, e, :] = 1.0
    shared["sel"] = selm
    shared["moe_w1"] = np.ascontiguousarray(f("moe_w1")[0])
    shared["moe_w3"] = np.ascontiguousarray(f("moe_w3")[0])
    shared["moe_w2"] = np.ascontiguousarray(f("moe_w2")[0])
    a_re, a_im, lstep = f("s5_a_re")[0], f("s5_a_im")[0], f("s5_log_step")[0]
    b_re, b_im, c_re, c_im = f("s5_b_re")[0], f("s5_b_im")[0], f("s5_c_re")[0], f("s5_c_im")[0]

    def gp(a2):
        return a2.reshape(32, 2, 64).transpose(1, 2, 0).reshape(128, 32)

    def gpb(b4):
        return b4.reshape(32, 2, 64, 16).transpose(1, 2, 0, 3).reshape(128, 32, 16)

    def gpc(c4):
        return c4.reshape(32, 2, 16, 64).transpose(1, 3, 0, 2).reshape(128, 32, 16)
    s5_dir = []
    for d in range(2):
        ls = np.broadcast_to(lstep[d].reshape(32, 2).T[:, None, :], (2, 64, 32)).reshape(128, 32)
        s5_dir.append(dict(a=np.stack([gp(a_re[d]), gp(a_im[d])], 0), ls=ls,
                           b=np.stack([gpb(b_re[d]), gpb(b_im[d])], 0), c=np.stack([gpc(c_re[d]), gpc(c_im[d])], 0)))
    in_maps = []
    orders = []
    for k in range(8):
        bi, half = k // 2, k % 2
        if half == 0:
            own = np.arange(0, 2048)
            partner = np.arange(2048, 4096)
            cidx = np.arange(0, 256)
        else:
            own = np.arange(4095, 2047, -1)
            partner = np.arange(2047, -1, -1)
            cidx = np.arange(255, -1, -1)
        X = np.concatenate([ctx[bi][cidx], x[bi][own], x[bi][partner]], axis=0)
        m = dict(shared)
        m["xall"] = _fm(X)
        ck, sk = _rope_tables(np.concatenate([own, partner]))
        cos_t = np.concatenate([np.ones((64, 256), np.float32), ck], axis=1)
        sin_t = np.concatenate([np.zeros((64, 256), np.float32), sk], axis=1)
        m["cosk"] = np.ascontiguousarray(cos_t)
        m["sink"] = np.ascontiguousarray(sin_t)
        m["cvec"] = np.ascontiguousarray(np.stack([_vec(c[bi]), _vec(c_ctx)], axis=2))
        dd = [s5_dir[half], s5_dir[1 - half]]
        m["s5a"] = np.ascontiguousarray(np.stack([dd[0]["a"], dd[1]["a"]], 0).transpose(2, 0, 1, 3))
        m["s5ls"] = np.ascontiguousarray(np.stack([dd[0]["ls"], dd[1]["ls"]], 0).transpose(1, 0, 2))
        m["s5b"] = np.ascontiguousarray(np.stack([dd[0]["b"], dd[1]["b"]], 0).transpose(2, 0, 1, 3, 4))
        m["s5c"] = np.ascontiguousarray(np.stack([dd[0]["c"], dd[1]["c"]], 0).transpose(2, 0, 1, 3, 4))
        pm = np.zeros((128, 2), np.float32)
        pm[:, half] = 1.0
        m["pmask"] = pm
        in_maps.append(m)
        orders.append(own)
    return in_maps, orders


_NC_CACHE = {}


def kernel(**inputs):
    in_maps, orders = make_in_maps(inputs)
    if "nc" not in _NC_CACHE:
        _NC_CACHE["nc"] = build_program()
    nc = _NC_CACHE["nc"]
    res = run_bass_kernel_spmd(nc, in_maps, core_ids=list(range(8)))
    out = np.zeros((4, 4096, D), np.float32)
    for k in range(8):
        y = np.asarray(res.results[k]["yout"])
        out[k // 2, orders[k], :] = y.transpose(2, 1, 0).reshape(NOWN, D)
    return out
```

```python
import numpy as np
from contextlib import ExitStack
import concourse.bass as bass
import concourse.mybir as mybir
from concourse.bass_utils import run_bass_kernel_spmd

F32 = mybir.dt.float32
BF16 = mybir.dt.bfloat16
AF = mybir.ActivationFunctionType
ALU = mybir.AluOpType

D = 1024
KC = 8
NCTX = 256
NOWN = 2048
NQ = NCTX + NOWN
NALL = NQ + NOWN
ALPHA = 4.0 ** 0.25
EPS = 1e-6
EPS_LN = EPS / (ALPHA * ALPHA)
DFF = 2816
NEXP = 8
DFE = 3584
QK_SCALE = 192.0 ** -0.5


class Sched:
    ENGS = ('pe', 'act', 'dve', 'pool', 'sp')

    def __init__(self, nc, es, ndma=20):
        self.nc = nc
        self.sem = {e: es.enter_context(nc.semaphore("sem_" + e)) for e in self.ENGS}
        self.dsem = [es.enter_context(nc.semaphore("dsem%d" % i)) for i in range(ndma)]
        self.dcnt = [0] * ndma
        self.dpool = {'pool': list(range(0, ndma // 2)), 'sp': list(range(ndma // 2, ndma))}
        self.dnext = {'pool': 0, 'sp': 0}
        self.ccsem = es.enter_context(nc.semaphore("ccsem"))
        self.cccnt = 0
        self.cnt = {e: 0 for e in self.ENGS}
        self.prog = {e: [] for e in self.ENGS}
        self.lastw = {}
        self.readers = {}
        self.known = {e: {} for e in self.ENGS}
        self.snap = {}
        self.nops = 0

    def _semof(self, s):
        if isinstance(s, str):
            return self.sem[s]
        return self.ccsem if s[0] == 'c' else self.dsem[s[1]]

    def collective(self, fn, reads=(), writes=()):
        waits = self._deps('pool', reads, writes)
        self.cccnt += 1
        tok = (('c', 0), self.cccnt)
        self.prog['pool'].append((waits, fn, 'cc'))
        self.snap[tok] = {s: c for s, c in self.known['pool'].items() if isinstance(s, str)}
        self._record(tok, reads, writes)
        return tok

    def _deps(self, eng, reads, writes):
        deps = {}
        for k in reads:
            t = self.lastw.get(k)
            if t is not None and deps.get(t[0], 0) < t[1]:
                deps[t[0]] = t[1]
        for k in writes:
            t = self.lastw.get(k)
            if t is not None and deps.get(t[0], 0) < t[1]:
                deps[t[0]] = t[1]
            for s, c in self.readers.get(k, {}).items():
                if deps.get(s, 0) < c:
                    deps[s] = c
        kn = self.known[eng]
        waits = []
        for s, c in deps.items():
            if s == 'pe' and eng == 'pe':
                continue
            if kn.get(s, 0) >= c:
                continue
            waits.append((s, c))
        for s, c in waits:
            kn[s] = c
            sn = self.snap.get((s, c))
            if sn:
                for s2, c2 in sn.items():
                    if kn.get(s2, 0) < c2:
                        kn[s2] = c2
        return waits

    def _record(self, tok, reads, writes):
        for k in writes:
            self.lastw[k] = tok
            self.readers[k] = {}
        for k in reads:
            r = self.readers.setdefault(k, {})
            if r.get(tok[0], 0) < tok[1]:
                r[tok[0]] = tok[1]

    def op(self, eng, fn, reads=(), writes=()):
        waits = self._deps(eng, reads, writes)
        self.cnt[eng] += 1
        tok = (eng, self.cnt[eng])
        self.prog[eng].append((waits, fn, None))
        self.snap[tok] = {s: c for s, c in self.known[eng].items() if isinstance(s, str)}
        self._record(tok, reads, writes)
        self.nops += 1
        return tok

    def dma(self, q, out, in_, reads=(), writes=()):
        pool_ = self.dpool[q]
        i = pool_[self.dnext[q] % len(pool_)]
        self.dnext[q] += 1
        waits = self._deps(q, reads, writes)
        src = ('d', i)
        prev = 16 * self.dcnt[i]
        if prev > 0 and self.known[q].get(src, 0) < prev:
            waits.append((src, prev))
            self.known[q][src] = prev
        self.dcnt[i] += 1
        tok = (src, 16 * self.dcnt[i])
        self.prog[q].append((waits, (lambda e, o=out, a=in_: e.dma_start(out=o, in_=a)), i))
        self.snap[tok] = {s: c for s, c in self.known[q].items() if isinstance(s, str)}
        self._record(tok, reads, writes)
        self.nops += 1
        return tok

    def flush(self, final_tokens=()):
        nc = self.nc
        prog = self.prog
        self.prog = {e: [] for e in self.ENGS}

        def emit(name, e):
            for waits, fn, di in prog[name]:
                for s, c in waits:
                    e.wait_ge(self._semof(s), c)
                ins = fn(e)
                if di is None:
                    ins.then_inc(self.sem[name], 1)
                elif di == 'cc':
                    ins.then_inc(self.ccsem)
                else:
                    ins.then_inc(self.dsem[di], 16)
            if name == 'sp':
                for s, c in final_tokens:
                    e.wait_ge(self._semof(s), c)

        with nc.Block() as block:
            if prog['pe']:
                @block.tensor
                def _(e):
                    emit('pe', e)
            if prog['act']:
                @block.scalar
                def _(e):
                    emit('act', e)
            if prog['dve']:
                @block.vector
                def _(e):
                    emit('dve', e)
            if prog['pool']:
                @block.gpsimd
                def _(e):
                    emit('pool', e)
            if prog['sp'] or final_tokens:
                @block.sync
                def _(e):
                    emit('sp', e)
        for e in self.ENGS:
            for s in self.ENGS:
                self.known[e][s] = self.cnt[s]


class B:
    def __init__(self, S):
        self.S = S
        self.rr = 0

    def mm(self, items, reads, writes):
        its = list(items)

        def fn(e):
            ins = None
            for (o, l, r, st, sp) in its:
                ins = e.matmul(o, l, r, start=st, stop=sp)
            return ins
        return self.S.op('pe', fn, reads, writes)

    def act(self, out, in_, func, reads, writes, bias=None, scale=None, eng='act'):
        kw = {}
        if bias is not None:
            kw['bias'] = bias
        if scale is not None:
            kw['scale'] = scale
        return self.S.op('act', lambda e: e.activation(out=out, in_=in_, func=func, **kw), reads, writes)

    def tt(self, eng, out, in0, in1, op, reads, writes):
        return self.S.op(eng, lambda e: e.tensor_tensor(out, in0, in1, op), reads, writes)

    def ts(self, eng, out, in0, s1, s2, op0, op1, reads, writes):
        if op1 is None:
            return self.S.op(eng, lambda e: e.tensor_scalar(out, in0, s1, None, op0), reads, writes)
        return self.S.op(eng, lambda e: e.tensor_scalar(out, in0, s1, s2, op0, op1), reads, writes)

    def stt(self, eng, out, in0, scalar, in1, op0, op1, reads, writes):
        return self.S.op(eng, lambda e: e.scalar_tensor_tensor(out, in0, scalar, in1, op0, op1), reads, writes)

    def copy(self, eng, out, in_, reads, writes):
        if eng == 'act':
            return self.S.op('act', lambda e: e.activation(out=out, in_=in_, func=AF.Copy), reads, writes)
        return self.S.op(eng, lambda e: e.tensor_copy(out, in_), reads, writes)

    def recip(self, eng, out, in_, reads, writes):
        return self.S.op(eng, lambda e: e.reciprocal(out, in_), reads, writes)

    def memset(self, eng, ap, val, writes):
        return self.S.op(eng, lambda e: e.memset(ap, val), (), writes)


def _rs(ap2d, shape):
    dims = shape[1:]
    if len(dims) > 1:
        names = ["a%d" % i for i in range(len(dims))]
        kw = {names[i]: int(dims[i]) for i in range(len(dims) - 1)}
        ap2d = ap2d.rearrange("p (%s) -> p %s" % (" ".join(names), " ".join(names)), **kw)
    return ap2d[:shape[0]]


class Arena:
    def __init__(self, flat_f32):
        self.f = flat_f32
        self.off = 0
        self.W = flat_f32.shape[1]

    def f32(self, shape):
        n = int(np.prod(shape[1:]))
        v = self.f[:, self.off:self.off + n]
        self.off += n
        assert self.off <= self.W, (self.off, self.W)
        return _rs(v, shape)

    def bf16(self, shape):
        n = int(np.prod(shape[1:]))
        nw = (n + 1) // 2
        v = self.f[:, self.off:self.off + nw].bitcast(BF16)[:, :n]
        self.off += nw
        assert self.off <= self.W, (self.off, self.W)
        return _rs(v, shape)


def build_program(debug=False, stop_after=None):
    nc = bass.Bass("TRN2", target_bir_lowering=False)
    dt = nc.dram_tensor

    def din(name, shape, dtype=F32):
        return dt(name, list(shape), dtype, kind="ExternalInput").ap()

    def dout(name, shape, dtype=F32):
        return dt(name, list(shape), dtype, kind="ExternalOutput").ap()

    xall = din("xall", [128, KC, NALL])
    cosk = din("cosk", [64, NALL])
    sink = din("sink", [64, NALL])
    cvec = din("cvec", [128, KC, 2])
    ada_w = din("ada_w", [2, D, 6 * D])
    adab = din("adab", [128, 2, 48])
    lng = din("lng", [128, 4, KC])
    lnb = din("lnb", [128, 4, KC])
    wdown = din("wdown", [128, KC, 768])
    gq = din("gq", [128, 3])
    gkv = din("gkv", [128, 2])
    wuq = din("wuq", [128, 3, 8 * 256])
    wuk = din("wuk", [128, 2, 1024])
    wuv = din("wuv", [128, 2, 1024])
    wo = din("wo", [128, 8, 1024])
    ffn_w1 = din("ffn_w1", [D, DFF])
    ffn_w3 = din("ffn_w3", [D, DFF])
    ffn_w2 = din("ffn_w2", [DFF, D])
    s5a = din("s5a", [128, 2, 2, 32])
    s5ls = din("s5ls", [128, 2, 32])
    s5b = din("s5b", [128, 2, 2, 32, 16])
    s5c = din("s5c", [128, 2, 2, 32, 16])
    s5d = din("s5d", [128, KC])
    pmask = din("pmask", [128, 2])
    cc_in = nc.dram_tensor("cc_in", [128, 128], F32)
    cc_out = nc.dram_tensor("cc_out", [128, 128], F32)
    ident = din("ident", [128, 128])
    identf = din("identf", [128, 128])
    glu_a = din("glu_a", [D, D])
    glu_b = din("glu_b", [D, D])
    wrt = din("wrt", [128, KC, 8])
    sel = din("sel", [8, 8, 128])
    moe_w1 = din("moe_w1", [NEXP, D, DFE])
    moe_w3 = din("moe_w3", [NEXP, D, DFE])
    moe_w2 = din("moe_w2", [NEXP, DFE, D])
    yout = dout("yout", [128, KC, NOWN])
    dbg = dout("dbg", [128, KC, NQ]) if debug else None

    with ExitStack() as es:
        S = Sched(nc, es)
        b = B(S)
        sb = lambda name, shape, dtype=F32, stack=es: stack.enter_context(nc.sbuf_tensor(name, list(shape), dtype))
        PS = [es.enter_context(nc.psum_tensor("ps%d" % i, [128, 512], F32)) for i in range(8)]
        pk = lambda i: ('ps', i)

        hT = sb("hT", [128, KC, NQ], F32)
        ubuf = sb("ubuf", [128, KC, NQ], BF16)
        hT_flat = hT[:].rearrange("p k n -> p (k n)")
        ub_flat32 = ubuf[:].rearrange("p k n -> p (k n)").bitcast(F32)
        modT = sb("modT", [128, 2, 48, 2], F32)
        pv = sb("pv", [128, 512], F32)
        lng_s = sb("lng_s", [128, 4, KC], F32)
        lnb_s = sb("lnb_s", [128, 4, KC], F32)
        ones_d = sb("ones_d", [128, 128], F32)
        ones_q = sb("ones_q", [128, 128], F32)
        ones_kv = sb("ones_kv", [128, 128], F32)
        ones_bf = sb("ones_bf", [128, 128], BF16)

        pv_off = [0]

        def pvslot(n):
            o = pv_off[0]
            pv_off[0] += n
            assert pv_off[0] <= 512
            return o

        SL = {}
        for l in range(2):
            SL[('s1p', l)] = pvslot(16)
            SL[('gam', l)] = pvslot(16)
            SL[('s2p', l)] = pvslot(16)
            SL[('gaf', l)] = pvslot(16)
            SL[('gU', l, 0)] = pvslot(16)
            SL[('bU', l, 0)] = pvslot(16)
        SL[('gU', 0, 1)] = pvslot(16)
        SL[('bU', 0, 1)] = pvslot(16)

        def pvs(key, kc, s):
            o = SL[key] + kc * 2 + s
            return pv[:, o:o + 1]

        def modv(l, m, kc, s):
            return modT[:, l, m * 8 + kc, s:s + 1]

        def ln_norm(r_ap_fn, N, s, rkeys, g1, b1, out1_fn, out1_keys, g2=None, b2=None, out2_fn=None, out2_keys=(),
                    tmp=None, psA=6, psB=7, in2_fn=None):
            sqb, mean_sb, m2, sd, rstd, nmr = tmp
            for kc in range(KC):
                b.mm([(PS[psA][:, :N], ones_d[:], r_ap_fn(kc), kc == 0, kc == KC - 1)], [rkeys[kc], 'ones'], [pk(psA)])
            for kc in range(KC):
                q = kc % 2
                b.act(sqb[:, q, :N], r_ap_fn(kc), AF.Square, [rkeys[kc]], [('sqb', q)])
                b.mm([(PS[psB][:, :N], ones_d[:], sqb[:, q, :N], kc == 0, kc == KC - 1)], [('sqb', q), 'ones'], [pk(psB)])
            b.copy('act', mean_sb[:, :N], PS[psA][:, :N], [pk(psA)], ['mean_sb'])
            b.tt('dve', m2[:, :N], mean_sb[:, :N], mean_sb[:, :N], ALU.mult, ['mean_sb'], ['m2'])
            b.tt('dve', m2[:, :N], PS[psB][:, :N], m2[:, :N], ALU.subtract, [pk(psB), 'm2'], ['m2'])
            b.act(sd[:, :N], m2[:, :N], AF.Sqrt, ['m2'], ['sd'], bias=eps_ln[:, 0:1])
            b.recip('dve', rstd[:, :N], sd[:, :N], ['sd'], ['rstd'])
            b.stt('dve', nmr[:, :N], mean_sb[:, :N], -1.0, rstd[:, :N], ALU.mult, ALU.mult, ['mean_sb', 'rstd'], ['nmr'])
            for kc in range(KC):
                ra = r_ap_fn(kc)
                eng = 'dve' if kc % 2 == 0 else 'pool'
                b.tt(eng, ra, ra, rstd[:, :N], ALU.mult, [rkeys[kc], 'rstd'], [rkeys[kc]])
                b.tt(eng, ra, ra, nmr[:, :N], ALU.add, [rkeys[kc], 'nmr'], [rkeys[kc]])
                if out2_fn is not None:
                    ok2 = list(out2_keys) if in2_fn is not None else [out2_keys[kc]]
                    b.act(out2_fn(kc), (in2_fn(kc) if in2_fn is not None else ra), AF.Identity, [rkeys[kc]], ok2, bias=b2(kc), scale=g2(kc))
                b.act(out1_fn(kc), ra, AF.Identity, [rkeys[kc]], [out1_keys[kc]], bias=b1(kc), scale=g1(kc))

        eps_ln = sb("eps_ln", [128, 2], F32)

        def ln_tmps(stack, tag):
            return (sb("sqb" + tag, [128, 2, 512], F32, stack), sb("mean" + tag, [128, 512], F32, stack),
                    sb("m2" + tag, [128, 512], F32, stack), sb("sd" + tag, [128, 512], F32, stack),
                    sb("rstd" + tag, [128, 512], F32, stack), sb("nmr" + tag, [128, 512], F32, stack))

        with ExitStack() as ps0:
            aw = sb("aw", [128, 2, KC, 1024], F32, ps0)
            cv = sb("cv", [128, KC, 2], F32, ps0)
            scT = sb("scT", [128, KC, 2], F32, ps0)
            adab_s = sb("adab_s", [128, 2, 48], F32, ps0)
            sig = sb("sig", [128, KC, 2], F32, ps0)
            b.memset('dve', ones_d[:], 1.0 / 1024, ['ones'])
            b.memset('dve', ones_q[:], 1.0 / 384, ['ones_q'])
            b.memset('dve', ones_kv[:], 1.0 / 256, ['ones_kv'])
            b.memset('dve', ones_bf[:], 1.0, ['ones_bf'])
            b.memset('dve', eps_ln[:, 0:1], EPS_LN, ['eps_ln'])
            b.memset('dve', eps_ln[:, 1:2], EPS, ['eps_ln1'])
            S.dma('sp', cv[:], cvec[:], [], ['cv'])
            S.dma('sp', adab_s[:], adab[:], [], ['adab'])
            S.dma('sp', lng_s[:], lng[:], [], ['lng'])
            S.dma('sp', lnb_s[:], lnb[:], [], ['lnb'])
            b.act(sig[:], cv[:], AF.Sigmoid, ['cv'], ['sig'])
            b.tt('dve', scT[:], cv[:], sig[:], ALU.mult, ['cv', 'sig'], ['scT'])
            i = 0
            for l in range(2):
                for m in range(6):
                    q = i % 2
                    i += 1
                    src = ada_w[l, :, m * 1024:(m + 1) * 1024].rearrange("(kc p) n -> p kc n", p=128)
                    S.dma('sp', aw[:, q, :, :], src, [], [('aw', q)])
                    for oc in range(KC):
                        col = (m * 8 + oc) * 2
                        pb = l
                        b.mm([(PS[pb][:, col:col + 2], aw[:, q, kc, oc * 128:(oc + 1) * 128], scT[:, kc, :], kc == 0, kc == KC - 1)
                              for kc in range(KC)], [('aw', q), 'scT'], [pk(pb)])
                for s in range(2):
                    b.tt('dve', modT[:, l, :, s], PS[l][:, 0:96].rearrange("p (j s) -> p j s", s=2)[:, :, s], adab_s[:, l, :],
                         ALU.add, [pk(l), 'adab'], [('modT', l)])
            def pvw(key):
                o = SL[key]
                return pv[:, o:o + 16].rearrange("p (k s) -> p k s", s=2)

            def modw(l, m):
                return modT[:, l, m * 8:(m + 1) * 8, :]

            def bc2(ap):
                return ap.unsqueeze(2).to_broadcast([128, KC, 2])
            for l in range(2):
                rd = [('modT', l), 'lng', 'lnb']
                b.ts('dve', pvw(('s1p', l)), modw(l, 1), 1.0, None, ALU.add, None, rd, ['pv'])
                b.ts('dve', pvw(('gam', l)), modw(l, 2), 1.0 / ALPHA, None, ALU.mult, None, rd, ['pv'])
                b.ts('dve', pvw(('s2p', l)), modw(l, 4), 1.0, None, ALU.add, None, rd, ['pv'])
                b.ts('dve', pvw(('gaf', l)), modw(l, 5), 1.0 / ALPHA, None, ALU.mult, None, rd, ['pv'])
                b.tt('dve', pvw(('gU', l, 0)), pvw(('s2p', l)), bc2(lng_s[:, l * 2 + 0, :]), ALU.mult, rd + ['pv'], ['pv'])
                b.tt('dve', pvw(('bU', l, 0)), pvw(('s2p', l)), bc2(lnb_s[:, l * 2 + 0, :]), ALU.mult, rd + ['pv'], ['pv'])
                b.tt('dve', pvw(('bU', l, 0)), pvw(('bU', l, 0)), modw(l, 3), ALU.add, rd + ['pv'], ['pv'])
            rd = [('modT', 1), 'lng', 'lnb', 'pv']
            b.tt('dve', pvw(('gU', 0, 1)), pvw(('s1p', 1)), bc2(lng_s[:, 1, :]), ALU.mult, rd, ['pv'])
            b.tt('dve', pvw(('bU', 0, 1)), pvw(('s1p', 1)), bc2(lnb_s[:, 1, :]), ALU.mult, rd, ['pv'])
            b.tt('dve', pvw(('bU', 0, 1)), pvw(('bU', 0, 1)), modw(1, 0), ALU.add, rd, ['pv'])
            S.flush()

        groups = [(0, 256, 1)] + [(256 + 512 * i, 512, 0) for i in range(8)]
        qgroups = groups[:5]

        pl0 = ExitStack()
        if True:
            cqT = sb("cqT", [128, 3, NQ], BF16, pl0)
            ckvT = sb("ckvT", [128, 2, NALL], BF16, pl0)
            krT = sb("krT", [64, NALL], BF16, pl0)

            with ExitStack() as p1:
                wd = sb("wd", [128, KC, 768], BF16, p1)
                gq_s = sb("gq_s", [128, 3], F32, p1)
                gkv_s = sb("gkv_s", [128, 2], F32, p1)
                hA = Arena(hT_flat)
                uA = Arena(ub_flat32)
                xg = hA.f32([128, 2, KC, 512])
                raw = hA.f32([128, 7, 512])
                sqr = hA.f32([128, 5, 512])
                rq = hA.f32([128, 2, 512])
                rs = hA.f32([128, 2, 512])
                kt1 = hA.f32([64, 2, 512])
                ug = uA.bf16([128, 2, KC, 512])
                csk = uA.f32([64, 2, 2, 512])
                S.dma('pool', wd[:], wdown[:], [], ['wd'])
                S.dma('sp', gq_s[:], gq[:], [], ['gq'])
                S.dma('sp', gkv_s[:], gkv[:], [], ['gkv'])
                psr = 0
                for gi, (c0, N, s) in enumerate(groups):
                    q = gi % 2
                    own = c0 < NQ
                    S.dma('sp', xg[:, q, :, :N], xall[:, :, c0:c0 + N], [], [('xg', q)])
                    S.dma('sp', csk[:, q, 0, :N], cosk[:, c0:c0 + N], [], [('csk', q)])
                    S.dma('sp', csk[:, q, 1, :N], sink[:, c0:c0 + N], [], [('csk', q)])
                    for kc in range(KC):
                        if kc % 2 == 0:
                            b.ts('dve', ug[:, q, kc, :N], xg[:, q, kc, :N], pvs(('s1p', 0), kc, s), modv(0, 0, kc, s),
                                 ALU.mult, ALU.add, [('xg', q), 'pv', ('modT', 0)], [('ug', q, kc)])
                        else:
                            b.act(ug[:, q, kc, :N], xg[:, q, kc, :N], AF.Identity, [('xg', q), 'pv', ('modT', 0)], [('ug', q, kc)],
                                  bias=modv(0, 0, kc, s), scale=pvs(('s1p', 0), kc, s))
                    chunks = ([(0, 0, 128), (1, 128, 128), (2, 256, 128)] if own else []) + \
                             [(3, 384, 128), (4, 512, 128), (5, 640, 64), (6, 704, 64)]
                    for (ri, m0, M) in chunks:
                        pb = psr % 4
                        psr += 1
                        b.mm([(PS[pb][:M, :N], wd[:, kc, m0:m0 + M], ug[:, q, kc, :N], kc == 0, kc == KC - 1) for kc in range(KC)],
                             ['wd'] + [('ug', q, kc) for kc in range(KC)], [pk(pb)])
                        b.copy('act', raw[:M, ri, :N], PS[pb][:M, :N], [pk(pb)], [('raw', ri)])
                    qs = [0, 1, 2] if own else []
                    for ri in qs + [3, 4]:
                        eng = 'dve' if ri % 2 == 0 else 'pool'
                        b.tt(eng, sqr[:, ri, :N], raw[:, ri, :N], raw[:, ri, :N], ALU.mult, [('raw', ri)], [('sqr', ri)])
                    if own:
                        b.mm([(PS[4][:, :N], ones_q[:], sqr[:, ri, :N], ri == 0, ri == 2) for ri in range(3)],
                             ['ones_q'] + [('sqr', ri) for ri in range(3)], [pk(4)])
                        b.act(rq[:, 0, :N], PS[4][:, :N], AF.Sqrt, [pk(4), 'eps_ln1'], [('rq', 0)], bias=eps_ln[:, 1:2])
                        b.recip('dve', rs[:, 0, :N], rq[:, 0, :N], [('rq', 0)], [('rs', 0)])
                        for ri in range(3):
                            b.stt('dve', cqT[:, ri, c0:c0 + N], raw[:, ri, :N], gq_s[:, ri:ri + 1], rs[:, 0, :N], ALU.mult, ALU.mult,
                                  [('raw', ri), 'gq', ('rs', 0)], [('cqT', gi)])
                    b.mm([(PS[5][:, :N], ones_kv[:], sqr[:, ri, :N], ri == 3, ri == 4) for ri in (3, 4)],
                         ['ones_kv', ('sqr', 3), ('sqr', 4)], [pk(5)])
                    b.act(rq[:, 1, :N], PS[5][:, :N], AF.Sqrt, [pk(5), 'eps_ln1'], [('rq', 1)], bias=eps_ln[:, 1:2])
                    b.recip('dve', rs[:, 1, :N], rq[:, 1, :N], [('rq', 1)], [('rs', 1)])
                    for ri in (3, 4):
                        b.stt('dve', ckvT[:, ri - 3, c0:c0 + N], raw[:, ri, :N], gkv_s[:, ri - 3:ri - 2], rs[:, 1, :N], ALU.mult, ALU.mult,
                              [('raw', ri), 'gkv', ('rs', 1)], [('ckvT', gi)])
                    b.tt('pool', kt1[:, 0, :N], raw[:64, 5, :N], csk[:, q, 0, :N], ALU.mult, [('raw', 5), ('csk', q)], [('kt1', 0)])
                    b.tt('pool', kt1[:, 1, :N], raw[:64, 6, :N], csk[:, q, 1, :N], ALU.mult, [('raw', 6), ('csk', q)], [('kt1', 1)])
                    b.tt('pool', krT[:, c0:c0 + N], kt1[:, 0, :N], kt1[:, 1, :N], ALU.add, [('kt1', 0), ('kt1', 1)], [('krT', gi)])
                S.flush()

            if stop_after == 'p1':
                pass

            if True:
                aoT = ubuf
                with ExitStack() as p2:
                    hA = Arena(hT_flat)
                    KnT = hA.bf16([128, 2, NALL])
                    Vt = hA.bf16([128, 2, 34, 128])
                    QnT = hA.bf16([128, 2, NQ])
                    QrT = hA.bf16([64, 2, NQ])
                    qt1 = hA.f32([64, 2, 512])
                    rD = hA.f32([128, 2, 512])
                    csq = hA.f32([64, 2, 2, 512])
                    wq_h = sb("wq_h", [128, 2, 3, 256], BF16, p2)
                    wk_h = sb("wk_h", [128, 2, 2, 128], BF16, p2)
                    wv_h = sb("wv_h", [128, 2, 2, 128], BF16, p2)
                    Pt = sb("Pt", [128, 3, 512], BF16, p2)
                    allck = [('ckvT', gi) for gi in range(9)]
                    allcq = [('cqT', gi) for gi in range(5)]
                    allkr = [('krT', gi) for gi in range(9)]
                    cp = 0
                    pcount = 0
                    csn = 0
                    for h in range(8):
                        hq = h % 2
                        S.dma('pool', wq_h[:, hq, :, :], wuq[:, :, h * 256:(h + 1) * 256], [], [('wq_h', hq)])
                        S.dma('pool', wk_h[:, hq, :, :], wuk[:, :, h * 128:(h + 1) * 128], [], [('wk_h', hq)])
                        S.dma('pool', wv_h[:, hq, :, :], wuv[:, :, h * 128:(h + 1) * 128], [], [('wv_h', hq)])
                        for gi, (c0, N, s) in enumerate(groups):
                            pb = 6 + (cp % 2)
                            cp += 1
                            b.mm([(PS[pb][:, :N], wk_h[:, hq, kc, :], ckvT[:, kc, c0:c0 + N], kc == 0, kc == 1) for kc in range(2)],
                                 [('wk_h', hq), ('ckvT', gi)], [pk(pb)])
                            b.copy('act' if gi % 2 == 0 else 'dve', KnT[:, hq, c0:c0 + N], PS[pb][:, :N], [pk(pb)], [('KnT', hq, gi)])
                        for t0 in range(0, 34, 4):
                            nt = min(4, 34 - t0)
                            pb = 6 + (cp % 2)
                            cp += 1
                            items = []
                            for j in range(nt):
                                kt = t0 + j
                                for kc in range(2):
                                    items.append((PS[pb][:, j * 128:(j + 1) * 128], ckvT[:, kc, kt * 128:(kt + 1) * 128],
                                                  wv_h[:, hq, kc, :], kc == 0, kc == 1))
                            b.mm(items, [('wv_h', hq)] + allck, [pk(pb)])
                            b.copy('dve' if (t0 // 4) % 2 == 0 else 'act', Vt[:, hq, t0:t0 + nt, :],
                                   PS[pb][:, :nt * 128].rearrange("p (j d) -> p j d", d=128), [pk(pb)], [('Vt', hq, t0)])
                        for gi, (c0, N, s) in enumerate(qgroups):
                            cq_ = csn % 2
                            csn += 1
                            S.dma('sp', csq[:, cq_, 0, :N], cosk[:, c0:c0 + N], [], [('csq', cq_)])
                            S.dma('sp', csq[:, cq_, 1, :N], sink[:, c0:c0 + N], [], [('csq', cq_)])
                            pb = 6 + (cp % 2)
                            cp += 1
                            b.mm([(PS[pb][:, :N], wq_h[:, hq, kc, 0:128], cqT[:, kc, c0:c0 + N], kc == 0, kc == 2) for kc in range(3)],
                                 [('wq_h', hq), ('cqT', gi)], [pk(pb)])
                            b.copy('act', QnT[:, hq, c0:c0 + N], PS[pb][:, :N], [pk(pb)], [('QnT', hq, gi)])
                            pb = 6 + (cp % 2)
                            cp += 1
                            b.mm([(PS[pb][:64, :N], wq_h[:, hq, kc, 128:192], cqT[:, kc, c0:c0 + N], kc == 0, kc == 2) for kc in range(3)],
                                 [('wq_h', hq), ('cqT', gi)], [pk(pb)])
                            b.tt('dve', qt1[:, 0, :N], PS[pb][:64, :N], csq[:, cq_, 0, :N], ALU.mult, [pk(pb), ('csq', cq_)], [('qt1', 0)])
                            pb = 6 + (cp % 2)
                            cp += 1
                            b.mm([(PS[pb][:64, :N], wq_h[:, hq, kc, 192:256], cqT[:, kc, c0:c0 + N], kc == 0, kc == 2) for kc in range(3)],
                                 [('wq_h', hq), ('cqT', gi)], [pk(pb)])
                            b.tt('dve', qt1[:, 1, :N], PS[pb][:64, :N], csq[:, cq_, 1, :N], ALU.mult, [pk(pb), ('csq', cq_)], [('qt1', 1)])
                            b.tt('pool', QrT[:, hq, c0:c0 + N], qt1[:, 0, :N], qt1[:, 1, :N], ALU.add, [('qt1', 0), ('qt1', 1)], [('QrT', hq, gi)])
                        kn_keys = [('KnT', hq, gi) for gi in range(9)]
                        vt_keys = [('Vt', hq, t0) for t0 in range(0, 34, 4)]
                        for gi, (c0, N, s) in enumerate(qgroups):
                            kts = [0, 1] if s == 1 else list(range(34))
                            ob = 2 + (pcount % 2) * 2
                            pcount += 1

                            def issue_S(j, c0=c0, N=N, gi=gi):
                                kt = kts[j]
                                sbk = j % 2
                                b.mm([(PS[sbk][:, :N], KnT[:, hq, kt * 128:(kt + 1) * 128], QnT[:, hq, c0:c0 + N], True, False),
                                      (PS[sbk][:, :N], krT[:, kt * 128:(kt + 1) * 128], QrT[:, hq, c0:c0 + N], False, True)],
                                     kn_keys + allkr + [('QnT', hq, gi), ('QrT', hq, gi)], [pk(sbk)])
                            issue_S(0)
                            for j in range(len(kts)):
                                kt = kts[j]
                                if j + 1 < len(kts):
                                    issue_S(j + 1)
                                pq = j % 3
                                b.act(Pt[:, pq, :N], PS[j % 2][:, :N], AF.Exp, [pk(j % 2)], [('Pt', pq)], scale=QK_SCALE)
                                b.mm([(PS[ob][:, :N], Vt[:, hq, kt, :], Pt[:, pq, :N], j == 0, j == len(kts) - 1),
                                      (PS[ob + 1][:, :N], ones_bf[:], Pt[:, pq, :N], j == 0, j == len(kts) - 1)],
                                     vt_keys + [('Pt', pq), 'ones_bf'], [pk(ob), pk(ob + 1)])
                            rq_ = (pcount % 2)
                            b.recip('dve', rD[:, rq_, :N], PS[ob + 1][:, :N], [pk(ob + 1)], [('rD', rq_)])
                            b.tt('dve', aoT[:, h, c0:c0 + N], PS[ob][:, :N], rD[:, rq_, :N], ALU.mult, [pk(ob), ('rD', rq_)], [('aoT', h, gi)])
                    S.flush()
                pl0.close()

                with ExitStack() as p3:
                    wo_s = sb("wo_s", [128, 8, 1024], BF16, p3)
                    lntmp = ln_tmps(p3, "b3")
                    S.dma('pool', wo_s[:], wo[:], [], ['wo'])
                    def oproj3(gi):
                        (c0, N, s) = qgroups[gi]
                        S.dma('sp', hT[:, :, c0:c0 + N], xall[:, :, c0:c0 + N], [], [('hT', gi, kc) for kc in range(KC)])
                        for oc in range(KC):
                            pb = oc % 4
                            b.mm([(PS[pb][:, :N], wo_s[:, h, oc * 128:(oc + 1) * 128], aoT[:, h, c0:c0 + N], h == 0, h == 7) for h in range(8)],
                                 ['wo'] + [('aoT', h, gi) for h in range(8)], [pk(pb)])
                            b.stt('dve', hT[:, oc, c0:c0 + N], PS[pb][:, :N], pvs(('gam', 0), oc, s), hT[:, oc, c0:c0 + N], ALU.mult, ALU.add,
                                  [pk(pb), 'pv', ('hT', gi, oc)], [('hT', gi, oc)])
                    oproj3(0)
                    for gi, (c0, N, s) in enumerate(qgroups):
                        if gi + 1 < len(qgroups):
                            oproj3(gi + 1)
                        ln_norm(lambda kc, c0=c0, N=N: hT[:, kc, c0:c0 + N], N, s, [('hT', gi, kc) for kc in range(KC)],
                                g1=lambda kc: lng_s[:, 0, kc:kc + 1], b1=lambda kc: lnb_s[:, 0, kc:kc + 1],
                                out1_fn=lambda kc, c0=c0, N=N: hT[:, kc, c0:c0 + N], out1_keys=[('hT', gi, kc) for kc in range(KC)],
                                g2=lambda kc, s=s: pvs(('gU', 0, 0), kc, s), b2=lambda kc, s=s: pvs(('bU', 0, 0), kc, s),
                                out2_fn=lambda kc, c0=c0, N=N: ubuf[:, kc, c0:c0 + N], out2_keys=[('ubuf', gi, kc) for kc in range(KC)],
                                tmp=lntmp)
                    S.flush()

            with ExitStack() as p4:
                w1s = sb("w1s", [128, 2, KC, 512], BF16, p4)
                w3s = sb("w3s", [128, 2, KC, 512], BF16, p4)
                w2s = sb("w2s", [128, 2, 4, 1024], BF16, p4)
                s1b = sb("s1b", [128, 2, 512], F32, p4)
                gT = sb("gT", [128, 2, 4, 512], BF16, p4)
                swiglu_stages(nc, S, b, PS, pk, qgroups, list(range(5)), ubuf, hT, [(ffn_w1, ffn_w3, ffn_w2, DFF, None)],
                              (w1s, w3s, w2s, s1b, gT, None), lambda oc, s: pvs(('gaf', 0), oc, s), NF=4, NWB=2)
                S.flush()
            with ExitStack() as p4b:
                lntmp = ln_tmps(p4b, "b4")
                for gi, (c0, N, s) in enumerate(qgroups):
                    ln_norm(lambda kc, c0=c0, N=N: hT[:, kc, c0:c0 + N], N, s, [('hT', gi, kc) for kc in range(KC)],
                            g1=lambda kc: lng_s[:, 1, kc:kc + 1], b1=lambda kc: lnb_s[:, 1, kc:kc + 1],
                            out1_fn=lambda kc, c0=c0, N=N: hT[:, kc, c0:c0 + N], out1_keys=[('hT', gi, kc) for kc in range(KC)],
                            g2=lambda kc, s=s: pvs(('gU', 0, 1), kc, s), b2=lambda kc, s=s: pvs(('bU', 0, 1), kc, s),
                            out2_fn=lambda kc, c0=c0, N=N: ubuf[:, kc, :].rearrange("p (j c) -> p j c", j=8)[:, :, c0 // 8:(c0 + N) // 8],
                            out2_keys=[('ubuf', g_, kc) for kc in range(KC) for g_ in range(5)],
                            in2_fn=lambda kc, c0=c0, N=N: hT[:, kc, c0:c0 + N].rearrange("p (c j) -> p j c", j=8),
                            tmp=lntmp)
                S.flush()

        lvl = {'l0': 0, 'p5': 1, 'p6': 1, 'p7a': 2, 'p7b': 2, None: 2}[stop_after]
        if lvl >= 1:
            own_groups = [(256 + 512 * i, 512, 0) for i in range(4)]
            own_gidx = [1, 2, 3, 4]
            allh = [('hT', gi, kc) for gi in range(5) for kc in range(KC)]
            with ExitStack() as p5:
                s5a_s = sb("s5a_s", [128, 2, 2, 32], F32, p5)
                s5ls_s = sb("s5ls_s", [128, 2, 32], F32, p5)
                s5b_k = sb("s5b_k", [128, 2, 2, 2, 4, 16], F32, p5)
                s5c_k = sb("s5c_k", [128, 2, 2, 2, 4, 16], F32, p5)
                s5d_s = sb("s5d_s", [128, KC], F32, p5)
                dAB = sb("dAB", [128, 2, KC], F32, p5)
                rin_s = sb("rin_s", [128, 32, 2], F32, p5)
                fx_s = sb("fx_s", [128, 32, 2], F32, p5)
                idb = sb("idb", [128, 128], BF16, p5)
                Pw = sb("Pw", [128, 2, 9, 2, 32], F32, p5)
                cS = sb("cS", [128, 2, 2, 8, 32], F32, p5)
                rho = sb("rho", [128, 2, 32], F32, p5)
                Arot = sb("Arot", [128, 2, 2, 32, 18], F32, p5)
                Brot = sb("Brot", [128, 2, 2, 32, 16], F32, p5)
                tmp = sb("s5tmp", [128, 16, 32], F32, p5)
                wpw = sb("wpw", [128, 9, 2, 32], F32, p5)
                Vc = sb("Vc", [128, 2, 4, 8, 16], F32, p5)
                Vt1 = sb("Vt1", [128, 2, 4, 8, 16], F32, p5)
                tmp8 = Vt1[:].rearrange("p a b c d -> p a (b c d)")[:, :, 0:256]
                Vbd = sb("Vbd", [128, 2, 4, 8, 2, 32], BF16, p5)
                Ebd = sb("Ebd", [128, 2, 4, 8, 2, 32], BF16, p5)
                Cbd = sb("Cbd", [128, 2, 4, 2, 32], BF16, p5)
                Wst = sb("Wst", [128, 2, 8, 2, 128], BF16, p5)
                rot = sb("rot", [128, 2, 2, 288], F32, p5)
                trti = sb("trti", [128, 1, 2, 288], F32, p5)
                rt2 = sb("rt2", [128, 2, 2, 288], F32, p5)
                ccb2 = sb("ccb2", [128, 2, 64], F32, p5)
                gg = sb("gg", [128, 2, 2, 288], F32, p5)
                Hp = sb("Hp", [128, 2, 2, 2, 288], BF16, p5)
                t32z = sb("t32z", [128, 2, 2, 256], F32, p5)
                S.dma('sp', s5a_s[:], s5a[:], [], ['s5a'])
                S.dma('sp', s5ls_s[:], s5ls[:], [], ['s5ls'])
                S.dma('sp', s5d_s[:], s5d[:], [], ['s5d'])
                pm_s = sb("pm_s", [128, 2], F32, p5)
                ccb = sb("ccb", [128, 2, 64], F32, p5)
                S.dma('sp', pm_s[:], pmask[:], [], ['pmask'])
                S.dma('pool', idb[:], ident[:], [], ['idb'])
                b.memset('pool', Vbd[:], 0.0, [('Vbd', 0), ('Vbd', 1)])
                b.memset('pool', Ebd[:], 0.0, [('Ebd', 0), ('Ebd', 1)])
                b.memset('pool', Cbd[:], 0.0, [('Cbd', 0), ('Cbd', 1)])
                b.memset('pool', Hp[:], 0.0, [('Hp', d_, h_) for d_ in range(2) for h_ in range(2)])
                for kc in range(KC):
                    b.tt('dve', dAB[:, 0, kc:kc + 1], s5d_s[:, kc:kc + 1], pvs(('s1p', 1), kc, 0), ALU.mult, ['s5d', 'pv'], ['dAB'])
                    b.tt('dve', dAB[:, 1, kc:kc + 1], s5d_s[:, kc:kc + 1], modv(1, 0, kc, 0), ALU.mult, ['s5d', ('modT', 1)], ['dAB'])

                def T(i):
                    return tmp[:, i, :]

                def tk(i):
                    return ('s5t', i)

                def cmul(eng, o_r, o_i, a_r, a_i, b_r, b_i, t1, t2, k1, k2, rk, wk, conj_b=False):
                    b.tt(eng, t1, a_r, b_r, ALU.mult, rk, [k1])
                    b.tt(eng, t2, a_i, b_i, ALU.mult, rk, [k2])
                    b.tt(eng, o_r, t1, t2, ALU.add if conj_b else ALU.subtract, [k1, k2], wk)
                    b.tt(eng, t1, a_i, b_r, ALU.mult, rk, [k1])
                    b.tt(eng, t2, a_r, b_i, ALU.mult, rk, [k2])
                    b.tt(eng, o_i, t1, t2, ALU.subtract if conj_b else ALU.add, [k1, k2], wk)

                def discretize(d):
                    ar = s5a_s[:, d, 0, :]
                    ai = s5a_s[:, d, 1, :]
                    kin = ['s5a', 's5ls']
                    b.act(T(0), s5ls_s[:, d, :], AF.Exp, kin, [tk(0)])
                    b.tt('dve', T(1), T(0), ar, ALU.mult, [tk(0)] + kin, [tk(1)])
                    b.tt('dve', T(2), T(0), ai, ALU.mult, [tk(0)] + kin, [tk(2)])
                    b.ts('dve', T(3), T(1), 1.0 / 6, 1.0, ALU.mult, ALU.add, [tk(1)], [tk(3)])
                    for k in (5, 4, 3, 2, 1):
                        b.tt('dve', T(3), T(3), T(1), ALU.mult, [tk(3), tk(1)], [tk(3)])
                        b.ts('dve', T(3), T(3), 1.0 / k, 1.0, ALU.mult, ALU.add, [tk(3)], [tk(3)])
                    b.act(T(4), T(2), AF.Sin, [tk(2)], [tk(4)], scale=1.0 / 16)
                    b.act(T(5), T(2), AF.Sin, [tk(2)], [tk(5)], scale=1.0 / 32)
                    b.tt('dve', T(5), T(5), T(5), ALU.mult, [tk(5)], [tk(5)])
                    b.ts('dve', T(5), T(5), -2.0, 1.0, ALU.mult, ALU.add, [tk(5)], [tk(5)])
                    for _ in range(4):
                        b.tt('dve', T(6), T(5), T(5), ALU.mult, [tk(5)], [tk(6)])
                        b.tt('dve', T(7), T(4), T(4), ALU.mult, [tk(4)], [tk(7)])
                        b.stt('dve', T(8), T(5), 2.0, T(4), ALU.mult, ALU.mult, [tk(5), tk(4)], [tk(8)])
                        b.tt('dve', T(5), T(6), T(7), ALU.subtract, [tk(6), tk(7)], [tk(5)])
                        b.copy('dve', T(4), T(8), [tk(8)], [tk(4)])
                    pk_ = ('Pw', d)

                    def P(m, ri):
                        return Pw[:, d, m, ri, :]
                    b.memset('dve', P(0, 0), 1.0, [pk_])
                    b.memset('dve', P(0, 1), 0.0, [pk_])
                    b.tt('dve', P(1, 0), T(3), T(5), ALU.mult, [tk(3), tk(5)], [pk_])
                    b.tt('dve', P(1, 1), T(3), T(4), ALU.mult, [tk(3), tk(4)], [pk_])
                    for m in range(2, 9):
                        cmul('dve', P(m, 0), P(m, 1), P(m - 1, 0), P(m - 1, 1), P(1, 0), P(1, 1), T(6), T(7), tk(6), tk(7), [pk_], [pk_])
                    b.ts('dve', T(6), P(1, 0), -1.0, None, ALU.add, None, [pk_], [tk(6)])
                    b.tt('dve', T(7), ar, ar, ALU.mult, kin, [tk(7)])
                    b.tt('dve', T(8), ai, ai, ALU.mult, kin, [tk(8)])
                    b.tt('dve', T(7), T(7), T(8), ALU.add, [tk(7), tk(8)], [tk(7)])
                    b.recip('dve', T(8), T(7), [tk(7)], [tk(8)])
                    b.tt('dve', T(9), T(6), ar, ALU.mult, [tk(6)] + kin, [tk(9)])
                    b.tt('dve', T(10), P(1, 1), ai, ALU.mult, [pk_] + kin, [tk(10)])
                    b.tt('dve', T(9), T(9), T(10), ALU.add, [tk(9), tk(10)], [tk(9)])
                    b.tt('dve', T(11), T(9), T(8), ALU.mult, [tk(9), tk(8)], [tk(11)])
                    b.tt('dve', T(9), P(1, 1), ar, ALU.mult, [pk_] + kin, [tk(9)])
                    b.tt('dve', T(10), T(6), ai, ALU.mult, [tk(6)] + kin, [tk(10)])
                    b.tt('dve', T(9), T(9), T(10), ALU.subtract, [tk(9), tk(10)], [tk(9)])
                    b.tt('dve', T(12), T(9), T(8), ALU.mult, [tk(9), tk(8)], [tk(12)])
                    t8a = tmp8[:, 0, :].rearrange("p (a b) -> p a b", a=8)
                    t8b = tmp8[:, 1, :].rearrange("p (a b) -> p a b", a=8)
                    cmul('dve', cS[:, d, 0, :, :], cS[:, d, 1, :, :], Pw[:, d, 0:8, 0, :], Pw[:, d, 0:8, 1, :],
                         T(11).unsqueeze(1).to_broadcast([128, 8, 32]), T(12).unsqueeze(1).to_broadcast([128, 8, 32]),
                         t8a, t8b, 'Vt1a', 'Vt1b', [pk_, tk(11), tk(12)], [('cS', d)])
                    b.tt('dve', T(6), T(3), T(3), ALU.mult, [tk(3)], [tk(6)])
                    b.tt('dve', T(6), T(6), T(6), ALU.mult, [tk(6)], [tk(6)])
                    b.tt('dve', rho[:, d, :], T(6), T(6), ALU.mult, [tk(6)], [('rho', d)])
                    b.recip('dve', T(7), rho[:, d, :], [('rho', d)], [tk(7)])

                    def W(k, ri):
                        return wpw[:, k, ri, :]
                    b.tt('dve', W(0, 0), P(8, 0), T(7), ALU.mult, [pk_, tk(7)], ['wpw'])
                    b.stt('dve', W(0, 1), P(8, 1), -1.0, T(7), ALU.mult, ALU.mult, [pk_, tk(7)], ['wpw'])
                    for k in range(8):
                        b.tt('dve', T(8), W(k, 0), W(k, 0), ALU.mult, ['wpw'], [tk(8)])
                        b.tt('dve', T(9), W(k, 1), W(k, 1), ALU.mult, ['wpw'], [tk(9)])
                        b.tt('dve', W(k + 1, 0), T(8), T(9), ALU.subtract, [tk(8), tk(9)], ['wpw'])
                        b.stt('dve', W(k + 1, 1), W(k, 0), 2.0, W(k, 1), ALU.mult, ALU.mult, ['wpw'], ['wpw'])
                    bk = ('Brot', d)
                    b.copy('dve', Brot[:, d, 0, :, 0:1], W(0, 0).unsqueeze(2), ['wpw'], [bk])
                    b.copy('dve', Brot[:, d, 1, :, 0:1], W(0, 1).unsqueeze(2), ['wpw'], [bk])
                    for kk, k in enumerate((1, 2, 4, 8)):
                        ta = tmp8[:, 0, :32 * k].rearrange("p (a b) -> p a b", a=32)
                        tb = tmp8[:, 1, :32 * k].rearrange("p (a b) -> p a b", a=32)
                        cmul('dve', Brot[:, d, 0, :, k:2 * k], Brot[:, d, 1, :, k:2 * k], Brot[:, d, 0, :, 0:k], Brot[:, d, 1, :, 0:k],
                             W(kk, 0).unsqueeze(2).to_broadcast([128, 32, k]), W(kk, 1).unsqueeze(2).to_broadcast([128, 32, k]),
                             ta, tb, 'Vt1a', 'Vt1b', [bk, 'wpw'], [bk])
                    ak = ('Arot', d)
                    b.memset('dve', Arot[:, d, 0, :, 0:1], 1.0, [ak])
                    b.memset('dve', Arot[:, d, 1, :, 0:1], 0.0, [ak])
                    for kk, (k, n) in enumerate(((1, 1), (2, 2), (4, 4), (8, 8), (16, 2))):
                        ta = tmp8[:, 0, :32 * n].rearrange("p (a b) -> p a b", a=32)
                        tb = tmp8[:, 1, :32 * n].rearrange("p (a b) -> p a b", a=32)
                        cmul('dve', Arot[:, d, 0, :, k:k + n], Arot[:, d, 1, :, k:k + n], Arot[:, d, 0, :, 0:n], Arot[:, d, 1, :, 0:n],
                             W(4 + kk, 0).unsqueeze(2).to_broadcast([128, 32, n]), W(4 + kk, 1).unsqueeze(2).to_broadcast([128, 32, n]),
                             ta, tb, 'Vt1a', 'Vt1b', [ak, 'wpw'], [ak])

                discretize(0)
                bg_ops = []
                real_op = S.op
                S.op = lambda eng, fn, reads=(), writes=(): bg_ops.append((eng, fn, reads, writes))
                discretize(1)
                S.op = real_op

                def bg_emit(n):
                    for _ in range(n):
                        if bg_ops:
                            real_op(*bg_ops.pop(0))

                def bd_scatter(dst_fn, src, negate, rk, wk):
                    for half in range(2):
                        ps_ = slice(half * 64, half * 64 + 64)
                        o = dst_fn(ps_, slice(half * 16, half * 16 + 16))
                        if negate:
                            b.act(o, src[ps_], AF.Copy, rk, wk, scale=-1.0)
                        else:
                            b.act(o, src[ps_], AF.Copy, rk, wk)

                def load_bc(kc):
                    kb = kc % 2
                    S.dma('sp', s5b_k[:, kb], s5b[:, :, :, 4 * kc:4 * kc + 4, :], [], [('s5b', kb)])
                    S.dma('sp', s5c_k[:, kb], s5c[:, :, :, 4 * kc:4 * kc + 4, :], [], [('s5c', kb)])

                def gen_tables(d, kc, only_v=False):
                    kb = kc % 2
                    pr = slice(4 * kc, 4 * kc + 4)
                    vr = Vc[:, 0]
                    vi = Vc[:, 1]
                    t1 = Vt1[:, 0]
                    t2 = Vt1[:, 1]
                    crb = cS[:, d, 0, :, pr].rearrange("p d q -> p q d").unsqueeze(3).to_broadcast([128, 4, 8, 16])
                    cib = cS[:, d, 1, :, pr].rearrange("p d q -> p q d").unsqueeze(3).to_broadcast([128, 4, 8, 16])
                    Brb = s5b_k[:, kb, d, 0, :, :].unsqueeze(2).to_broadcast([128, 4, 8, 16])
                    Bib = s5b_k[:, kb, d, 1, :, :].unsqueeze(2).to_broadcast([128, 4, 8, 16])
                    cmul('dve', vr, vi, crb, cib, Brb, Bib, t1, t2, 'Vt1a', 'Vt1b', [('cS', d), ('s5b', kb)], ['Vc'])
                    bd_scatter(lambda p_, c_: Vbd[p_, d, :, :, 0, c_], vr, False, ['Vc'], [('Vbd', d)])
                    bd_scatter(lambda p_, c_: Vbd[p_, d, :, :, 1, c_], vi, False, ['Vc'], [('Vbd', d)])
                    if only_v:
                        return
                    bd_scatter(lambda p_, c_: Cbd[p_, d, :, 0, c_], s5c_k[:, kb, d, 0, :, :], False, [('s5c', kb)], [('Cbd', d)])
                    bd_scatter(lambda p_, c_: Cbd[p_, d, :, 1, c_], s5c_k[:, kb, d, 1, :, :], True, [('s5c', kb)], [('Cbd', d)])

                def gen_E(d, kc):
                    kb = kc % 2
                    pr = slice(4 * kc, 4 * kc + 4)
                    vr = Vc[:, 0]
                    vi = Vc[:, 1]
                    t1 = Vt1[:, 0]
                    t2 = Vt1[:, 1]
                    prb = Pw[:, d, 1:9, 0, pr].rearrange("p m q -> p q m").unsqueeze(3).to_broadcast([128, 4, 8, 16])
                    pib = Pw[:, d, 1:9, 1, pr].rearrange("p m q -> p q m").unsqueeze(3).to_broadcast([128, 4, 8, 16])
                    Crb = s5c_k[:, kb, d, 0, :, :].unsqueeze(2).to_broadcast([128, 4, 8, 16])
                    Cib = s5c_k[:, kb, d, 1, :, :].unsqueeze(2).to_broadcast([128, 4, 8, 16])
                    cmul('dve', vr, vi, prb, pib, Crb, Cib, t1, t2, 'Vt1a', 'Vt1b', [('Pw', d), ('s5c', kb)], ['Vc'])
                    bd_scatter(lambda p_, c_: Ebd[p_, d, :, :, 0, c_], vr, False, ['Vc'], [('Ebd', d)])
                    bd_scatter(lambda p_, c_: Ebd[p_, d, :, :, 1, c_], vi, True, ['Vc'], [('Ebd', d)])

                PTb = PS[6][:].bitcast(BF16)
                pcnt = [0]

                kin4_p = [hT[:, p_, 0:256].bitcast(BF16).rearrange("p (a n) -> p a n", a=4) for p_ in range(4)]
                for p_ in range(4):
                    b.memset('pool', hT[:, p_, 0:256], 0.0, [('hT', 0, p_), ('Kin4', p_ // 2)])

                def kin4(d, dl):
                    return kin4_p[d * 2 + dl // 4][:, dl % 4, :]

                def kin_prologue(kc):
                    for d in range(2):
                        for q in range(4):
                            rows = slice(32 * q, 32 * q + 32)

                            def fnk(e, d=d, q=q, rows=rows):
                                ins = None
                                for dl in range(8):
                                    o = PS[7][rows, dl * 32:(dl + 1) * 32]
                                    e.matmul(o, Vbd[:, d, q, dl, 0, :], Cbd[:, d, q, 0, :], start=True, stop=False, tile_position=(0, 32 * q))
                                    ins = e.matmul(o, Vbd[:, d, q, dl, 1, :], Cbd[:, d, q, 1, :], start=False, stop=True, tile_position=(0, 32 * q))
                                return ins
                            S.op('pe', fnk, [('Vbd', d), ('Cbd', d)], [pk(7)])
                        for q in range(4):
                            rows = slice(32 * q, 32 * q + 32)
                            for hf in range(2):
                                b.copy('act', kin4_p[d * 2 + hf][rows, :, 32 * q:32 * q + 32],
                                       PS[7][rows, hf * 128:hf * 128 + 128].rearrange("p (a n) -> p a n", a=4), [pk(7)], [('Kin4', d)])

                def intra_prologue(kc):
                    ukeys = [('ubuf', gi, kc) for gi in range(5)]

                    def fni(e):
                        ins = None
                        for d in range(2):
                            for j in range(8):
                                o = PS[j // 2][:, (j % 2) * 256:(j % 2) * 256 + 256]
                                js = list(range(0, j + 1)) if d == 0 else list(range(j, 8))
                                for n_, jp in enumerate(js):
                                    ins = e.matmul(o, kin4(d, abs(j - jp)), ubuf[:, kc, jp * 288 + 32:jp * 288 + 288],
                                                   start=(d == 0 and j % 2 == 0 and n_ == 0), stop=False)
                        return ins
                    S.op('pe', fni, [('Kin4', 0), ('Kin4', 1)] + ukeys, [pk(0), pk(1), pk(2), pk(3)])

                def u_geom(d, kc, q):
                    return 4 * kc + q, slice(32 * q, 32 * q + 32), (288 if d == 0 else 256), (0 if d == 0 else NCTX)

                def u_front(d, kc, q, iu, need_out):
                    pair, rows, C, col0 = u_geom(d, kc, q)
                    wb = iu % 2
                    for half in range(2):
                        def fn(e, half=half):
                            ins = None
                            for dl in range(4):
                                for ri in range(2):
                                    idx = dl * 2 + ri
                                    ins = e.transpose(PTb[rows, idx * 128:(idx + 1) * 128], Vbd[:, d, q, half * 4 + dl, ri, :], idb[:],
                                                      tile_position=(0, 32 * q))
                            return ins
                        S.op('pe', fn, [('Vbd', d), 'idb'], [pk(6)])
                        b.copy('act', Wst[rows, wb, half * 4:half * 4 + 4, :, :],
                               PTb[rows, :].rearrange("p (a r n) -> p a r n", a=4, r=2), [pk(6)], [('Wst', wb)])

                def u_rot(d, kc, q, iu):
                    pair, rows, C, col0 = u_geom(d, kc, q)
                    rb = iu % 2
                    na = C // 16
                    rv = lambda ri: rot[:, rb, ri, :C].rearrange("p (a b) -> p a b", b=16)
                    cmul('pool', rv(0), rv(1),
                         Arot[:, d, 0, pair, 0:na].unsqueeze(2).to_broadcast([128, na, 16]), Arot[:, d, 1, pair, 0:na].unsqueeze(2).to_broadcast([128, na, 16]),
                         Brot[:, d, 0, pair, :].unsqueeze(1).to_broadcast([128, na, 16]), Brot[:, d, 1, pair, :].unsqueeze(1).to_broadcast([128, na, 16]),
                         rt2[:, 1, 0, :C].rearrange("p (a b) -> p a b", b=16), rt2[:, 1, 1, :C].rearrange("p (a b) -> p a b", b=16),
                         ('rt2a', 'pool'), ('rt2b', 'pool'), [('Arot', d), ('Brot', d)], [('rot', rb)])

                def u_states(d, kc, q, iu):
                    pair, rows, C, col0 = u_geom(d, kc, q)
                    wb = iu % 2
                    ukeys = [('ubuf', gi, kc) for gi in range(5)]
                    for ri in range(2):
                        def fns(e, ri=ri):
                            ins = None
                            for j in range(8):
                                dl = (7 - j) if d == 0 else j
                                ins = e.matmul(PS[4 + ri][:, :C], Wst[rows, wb, dl, ri, :], ubuf[rows, kc, j * 288 + col0 // 8:j * 288 + col0 // 8 + C],
                                               start=(j == 0), stop=(j == 7), tile_position=(32 * q, 0))
                            return ins
                        S.op('pe', fns, [('Wst', wb)] + ukeys, [pk(4 + ri)])

                def u_scan(d, kc, q, iu):
                    pair, rows, C, col0 = u_geom(d, kc, q)
                    wb = iu % 2
                    if d == 0:
                        Sr, Si = PS[4][:, :C], PS[5][:, :C]
                    else:
                        Sr, Si = PS[4][:, C - 1::-1], PS[5][:, C - 1::-1]
                    R0, R1 = rot[:, wb, 0, :C], rot[:, wb, 1, :C]
                    tr, ti = trti[:, 0, 0, :C], trti[:, 0, 1, :C]
                    ta, tb = gg[:, wb, 0, :C], gg[:, wb, 1, :C]
                    kr_ = [pk(4), pk(5), ('rot', wb)]
                    b.tt('dve', ta, Sr, R0, ALU.mult, kr_, [('gg', wb, 0)])
                    b.tt('dve', tb, Si, R1, ALU.mult, kr_, [('gg', wb, 1)])
                    b.tt('dve', tr, ta, tb, ALU.subtract, [('gg', wb, 0), ('gg', wb, 1)], [('trti', 0)])
                    b.tt('dve', ta, Si, R0, ALU.mult, kr_, [('gg', wb, 0)])
                    b.tt('dve', tb, Sr, R1, ALU.mult, kr_, [('gg', wb, 1)])
                    b.tt('dve', ti, ta, tb, ALU.add, [('gg', wb, 0), ('gg', wb, 1)], [('trti', 1)])
                    rho_b = rho[:, d, pair:pair + 1].to_broadcast([128, C])
                    if d == 0:
                        i_r, i_i, ik = 0.0, 0.0, []
                    else:
                        i_r, i_i, ik = rin_s[:, pair, 0:1], rin_s[:, pair, 1:2], ['rin']
                    gr, gi_ = gg[:, wb, 0, :C], gg[:, wb, 1, :C]
                    S.op('dve', lambda e: e.tensor_tensor_scan(gr, rho_b, tr, i_r, ALU.mult, ALU.add),
                         [('rho', d), ('trti', 0)] + ik, [('gg', wb, 0)])
                    S.op('dve', lambda e: e.tensor_tensor_scan(gi_, rho_b, ti, i_i, ALU.mult, ALU.add),
                         [('rho', d), ('trti', 1)] + ik, [('gg', wb, 1)])

                def u_final(d, kc, q, iu):
                    pair, rows, C, col0 = u_geom(d, kc, q)
                    wb = iu % 2
                    R0, R1 = rot[:, wb, 0, :C], rot[:, wb, 1, :C]
                    gr, gi_ = gg[:, wb, 0, :C], gg[:, wb, 1, :C]
                    L = slice(C - 1, C)
                    b.tt('dve', T(13)[:, 0:1], R0[:, L], gr[:, L], ALU.mult, [('rot', wb), ('gg', wb, 0)], [tk(13)])
                    b.tt('dve', T(13)[:, 1:2], R1[:, L], gi_[:, L], ALU.mult, [('rot', wb), ('gg', wb, 1)], [tk(13)])
                    b.tt('dve', fx_s[:, pair, 0:1], T(13)[:, 0:1], T(13)[:, 1:2], ALU.add, [tk(13)], ['fx'])
                    b.tt('dve', T(13)[:, 2:3], R0[:, L], gi_[:, L], ALU.mult, [('rot', wb), ('gg', wb, 1)], [tk(13)])
                    b.tt('dve', T(13)[:, 3:4], R1[:, L], gr[:, L], ALU.mult, [('rot', wb), ('gg', wb, 0)], [tk(13)])
                    b.tt('dve', fx_s[:, pair, 1:2], T(13)[:, 2:3], T(13)[:, 3:4], ALU.subtract, [tk(13)], ['fx'])

                def u_unrot(d, kc, q, iu):
                    pair, rows, C, col0 = u_geom(d, kc, q)
                    wb = iu % 2
                    hb = (iu // 2) % 2
                    R0, R1 = rot[:, wb, 0, :C], rot[:, wb, 1, :C]
                    gr, gi_ = gg[:, wb, 0, :C], gg[:, wb, 1, :C]
                    if d == 0:
                        o_r, o_i = Hp[:, d, hb, 0, 1:C], Hp[:, d, hb, 1, 1:C]
                    else:
                        o_r, o_i = Hp[:, d, hb, 0, C - 2::-1], Hp[:, d, hb, 1, C - 2::-1]
                    n1 = C - 1
                    hk = ('Hp', d, hb)
                    e2 = 'dve' if d == 0 else 'pool'
                    ka, kb_ = ('rt2a', e2), ('rt2b', e2)
                    r2 = rt2[:, 0 if d == 0 else 1]
                    b.tt(e2, r2[:, 0, :n1], R0[:, :n1], gr[:, :n1], ALU.mult, [('rot', wb), ('gg', wb, 0)], [ka])
                    b.tt(e2, r2[:, 1, :n1], R1[:, :n1], gi_[:, :n1], ALU.mult, [('rot', wb), ('gg', wb, 1)], [kb_])
                    b.tt(e2, o_r, r2[:, 0, :n1], r2[:, 1, :n1], ALU.add, [ka, kb_], [hk])
                    b.tt(e2, r2[:, 0, :n1], R0[:, :n1], gi_[:, :n1], ALU.mult, [('rot', wb), ('gg', wb, 1)], [ka])
                    b.tt(e2, r2[:, 1, :n1], R1[:, :n1], gr[:, :n1], ALU.mult, [('rot', wb), ('gg', wb, 0)], [kb_])
                    b.tt(e2, o_i, r2[:, 0, :n1], r2[:, 1, :n1], ALU.subtract, [ka, kb_], [hk])
                    if d == 1:
                        b.copy('pool', Hp[:, d, hb, 0, C - 1:C], rin_s[:, pair, 0:1], ['rin'], [hk])
                        b.copy('pool', Hp[:, d, hb, 1, C - 1:C], rin_s[:, pair, 1:2], ['rin'], [hk])

                def u_out(d, kc, q, iu, first_dir, last_dir):
                    pair, rows, C, col0 = u_geom(d, kc, q)
                    wb = iu % 2
                    hb = (iu // 2) % 2
                    hk = ('Hp', d, hb)
                    hoff = 32 if d == 0 else 0
                    ukeys = [('ubuf', gi, kc) for gi in range(5)]

                    def fno(e):
                        ins = None
                        for j in range(8):
                            o = PS[j // 2][rows, (j % 2) * 256:(j % 2) * 256 + 256]
                            mi = j if d == 0 else 7 - j
                            e.matmul(o, Ebd[:, d, q, mi, 0, :], Hp[:, d, hb, 0, hoff:hoff + 256], start=False, stop=False, tile_position=(0, 32 * q))
                            ins = e.matmul(o, Ebd[:, d, q, mi, 1, :], Hp[:, d, hb, 1, hoff:hoff + 256], start=False,
                                           stop=(last_dir and q == 3), tile_position=(0, 32 * q))
                        return ins
                    S.op('pe', fno, [('Ebd', d), hk] + ukeys, [pk(0), pk(1), pk(2), pk(3)])

                unitsA = [(0, kc, q) for kc in range(KC) for q in range(4)]
                load_bc(0)
                gen_tables(0, 0, only_v=True)
                u_front(*unitsA[0], 0, False)
                u_rot(*unitsA[0], 0)
                u_states(*unitsA[0], 0)
                for i, u in enumerate(unitsA):
                    if i + 1 < len(unitsA):
                        un = unitsA[i + 1]
                        if un[1] != u[1]:
                            load_bc(un[1])
                            gen_tables(0, un[1], only_v=True)
                        u_front(*un, i + 1, False)
                        u_rot(*un, i + 1)
                    u_scan(*u, i)
                    bg_emit(4)
                    if i + 1 < len(unitsA):
                        u_states(*unitsA[i + 1], i + 1)
                    u_final(*u, i)
                    bg_emit(4)
                bg_emit(len(bg_ops))
                fxf = fx_s[:].rearrange("p a b -> p (a b)")
                for sl in range(2):
                    b.ts('dve', ccb[:, sl, :], fxf, pm_s[:, sl:sl + 1], None, ALU.mult, None, ['fx', 'pmask'], ['ccb'])
                S.dma('pool', cc_in.ap().opt() if False else cc_in[:, :], ccb[:].rearrange("p a b -> p (a b)"), ['ccb'], ['cc_in'])
                S.collective(lambda e: e.collective_compute("AllReduce", ALU.add, replica_groups=[[0, 1], [2, 3], [4, 5], [6, 7]],
                                                            ins=[cc_in.ap().opt()], outs=[cc_out.ap().opt()]), ['cc_in'], ['cc_out'])
                S.dma('sp', ccb2[:].rearrange("p a b -> p (a b)"), cc_out[:, :], ['cc_out'], ['ccb2'])

                def recv_rin():
                    rinf = rin_s[:].rearrange("p a b -> p (a b)")
                    b.ts('dve', rinf, ccb2[:, 0, :], pm_s[:, 1:2], None, ALU.mult, None, ['ccb2', 'pmask'], ['rin'])
                    b.stt('dve', rinf, ccb2[:, 1, :], pm_s[:, 0:1], rinf, ALU.mult, ALU.add, ['ccb2', 'pmask', 'rin'], ['rin'])

                unitsB = [(d, kc, q) for kc in range(KC) for q in range(4) for d in range(2)]

                def z_step(kc):
                    hv = hT[:, kc, NCTX:NQ].rearrange("p (c j) -> p j c", j=8)
                    zv = ubuf[:, kc, NCTX:NQ].rearrange("p (c j) -> p j c", j=8)
                    for bk_ in range(4):
                        zb = bk_ % 2
                        b.stt('dve', t32z[:, zb, :, :], hv[:, 2 * bk_:2 * bk_ + 2, :], dAB[:, 0, kc:kc + 1],
                              PS[bk_][:, :].rearrange("p (j c) -> p j c", j=2), ALU.mult, ALU.add,
                              [pk(bk_), 'dAB'] + [('hT', gi, kc) for gi in range(1, 5)], [('t32z', zb)])
                        if stop_after == 'p5':
                            b.act(hv[:, 2 * bk_:2 * bk_ + 2, :], t32z[:, zb, :, :], AF.Identity, [('t32z', zb), 'dAB'], [('hT', gi, kc) for gi in range(1, 5)],
                                  bias=dAB[:, 1, kc:kc + 1])
                        else:
                            b.act(zv[:, 2 * bk_:2 * bk_ + 2, :], t32z[:, zb, :, :], AF.Gelu_apprx_tanh, [('t32z', zb), 'dAB'], [('ubuf', gi, kc) for gi in range(1, 5)],
                                  bias=dAB[:, 1, kc:kc + 1])

                load_bc(0)
                for d_ in range(2):
                    gen_tables(d_, 0)
                    gen_E(d_, 0)
                kin_prologue(0)
                intra_prologue(0)
                u_front(*unitsB[0], 0, True)
                u_rot(*unitsB[0], 0)
                u_states(*unitsB[0], 0)
                for i, u in enumerate(unitsB):
                    nxt_kc = None
                    if i + 1 < len(unitsB):
                        un = unitsB[i + 1]
                        if un[1] != u[1]:
                            nxt_kc = un[1]
                            load_bc(nxt_kc)
                            gen_tables(0, nxt_kc)
                            gen_tables(1, nxt_kc)
                            kin_prologue(nxt_kc)
                        u_front(*un, i + 1, True)
                        u_rot(*un, i + 1)
                    if i == 1:
                        recv_rin()
                    u_scan(*u, i)
                    if i + 1 < len(unitsB):
                        u_states(*unitsB[i + 1], i + 1)
                    u_unrot(*u, i)
                    u_out(*u, i, u[0] == 0, u[0] == 1)
                    if u[0] == 1 and u[2] == 3:
                        z_step(u[1])
                    if nxt_kc is not None:
                        intra_prologue(nxt_kc)
                        gen_E(0, nxt_kc)
                        gen_E(1, nxt_kc)
                S.flush()

            p6 = ExitStack()
            if stop_after != 'p5':
                wga = sb("wga", [128, KC, 1024], BF16, p6)
                wgb = sb("wgb", [128, KC, 1024], BF16, p6)
                sgb = sb("sgb", [128, 2, 512], F32, p6)
                ogb = sb("ogb", [128, 2, 512], F32, p6)
                lntmp = ln_tmps(p6, "b6")
                S.dma('pool', wga[:], glu_a.rearrange("(kc p) n -> p kc n", p=128), [], ['wga'])
                S.dma('pool', wgb[:], glu_b.rearrange("(kc p) n -> p kc n", p=128), [], ['wgb'])
                cnt6 = [0]

                def glu6(ti):
                    (c0, N, s) = own_groups[ti]
                    gi = own_gidx[ti]
                    zkeys = [('ubuf', gi, kc) for kc in range(KC)]
                    for oc in range(KC):
                        pa = (cnt6[0] % 2) * 2
                        q6 = cnt6[0] % 2
                        cnt6[0] += 1
                        b.mm([(PS[pa][:, :N], wga[:, kc, oc * 128:(oc + 1) * 128], ubuf[:, kc, c0:c0 + N], kc == 0, kc == KC - 1) for kc in range(KC)],
                             ['wga'] + zkeys, [pk(pa)])
                        b.mm([(PS[pa + 1][:, :N], wgb[:, kc, oc * 128:(oc + 1) * 128], ubuf[:, kc, c0:c0 + N], kc == 0, kc == KC - 1) for kc in range(KC)],
                             ['wgb'] + zkeys, [pk(pa + 1)])
                        b.act(sgb[:, q6, :N], PS[pa + 1][:, :N], AF.Sigmoid, [pk(pa + 1)], [('sgb', q6)])
                        b.stt('dve', ogb[:, q6, :N], PS[pa][:, :N], pvs(('gam', 1), oc, 0), sgb[:, q6, :N], ALU.mult, ALU.mult,
                              [pk(pa), 'pv', ('sgb', q6)], [('ogb', q6)])
                        b.tt('pool', hT[:, oc, c0:c0 + N], ogb[:, q6, :N], hT[:, oc, c0:c0 + N], ALU.add, [('ogb', q6), ('hT', gi, oc)], [('hT', gi, oc)])
                glu6(0)
                for ti, (c0, N, s) in enumerate(own_groups):
                    gi = own_gidx[ti]
                    if ti + 1 < len(own_groups):
                        glu6(ti + 1)
                    ln_norm(lambda kc, c0=c0, N=N: hT[:, kc, c0:c0 + N], N, 0, [('hT', gi, kc) for kc in range(KC)],
                            g1=lambda kc: lng_s[:, 2, kc:kc + 1], b1=lambda kc: lnb_s[:, 2, kc:kc + 1],
                            out1_fn=lambda kc, c0=c0, N=N: hT[:, kc, c0:c0 + N], out1_keys=[('hT', gi, kc) for kc in range(KC)],
                            g2=lambda kc: pvs(('gU', 1, 0), kc, 0), b2=lambda kc: pvs(('bU', 1, 0), kc, 0),
                            out2_fn=lambda kc, c0=c0, N=N: ubuf[:, kc, c0:c0 + N], out2_keys=[('ubuf', gi, kc) for kc in range(KC)],
                            tmp=lntmp, psA=4, psB=5)
                S.flush()
            p6.close()

        if lvl >= 2:
            with ExitStack() as p7:
                combT = sb("combT", [8, NOWN], F32, p7)
                cbt = sb("cbt", [128, NOWN], F32, p7)
                sel_s = sb("sel_s", [8, 8, 128], F32, p7)
                S.dma('sp', sel_s[:], sel[:], [], ['sel'])
                with ExitStack() as p7a:
                    wr_s = sb("wr_s", [128, KC, 8], F32, p7a)
                    idf = sb("idf", [128, 128], F32, p7a)
                    u32 = sb("u32", [128, 2, KC, 128], F32, p7a)
                    lg = sb("lg", [128, 2, 8], F32, p7a)
                    mx = sb("mx", [128, 2, 8], F32, p7a)
                    msk = sb("msk", [128, 2, 8], F32, p7a)
                    ex = sb("ex", [128, 2, 8], F32, p7a)
                    den = sb("den", [128, 2, 2], F32, p7a)
                    cmb = sb("cmb", [128, 2, 8], F32, p7a)
                    S.dma('sp', wr_s[:], wrt[:], [], ['wr'])
                    S.dma('sp', idf[:], identf[:], [], ['idf'])
                    for tp in range(8):
                        tiles = [(2 * tp + tb, tb) for tb in range(2)]
                        for (t, tb) in tiles:
                            c0 = NCTX + 128 * t
                            gi = 1 + t // 4
                            for kc in range(KC):
                                eng = 'dve' if kc % 2 == 0 else 'pool'
                                b.ts(eng, u32[:, tb, kc, :], hT[:, kc, c0:c0 + 128], pvs(('s2p', 1), kc, 0), modv(1, 3, kc, 0), ALU.mult, ALU.add,
                                     [('hT', gi, kc), 'pv', ('modT', 1)], [('u32', tb, kc)])
                        for (t, tb) in tiles:
                            b.mm([(PS[tb][:, 0:8], u32[:, tb, kc, :], wr_s[:, kc, :], kc == 0, kc == KC - 1) for kc in range(KC)],
                                 ['wr'] + [('u32', tb, kc) for kc in range(KC)], [pk(tb)])
                        for (t, tb) in tiles:
                            b.copy('act', lg[:, tb, :], PS[tb][:, 0:8], [pk(tb)], [('lg', tb)])
                        for (t, tb) in tiles:
                            S.op('dve', lambda e, tb=tb: e.max(mx[:, tb, :], lg[:, tb, :]), [('lg', tb)], [('mx', tb)])
                        for (t, tb) in tiles:
                            b.ts('dve', msk[:, tb, :], lg[:, tb, :], mx[:, tb, 1:2], None, ALU.is_ge, None, [('lg', tb), ('mx', tb)], [('msk', tb)])
                        for (t, tb) in tiles:
                            b.ts('dve', ex[:, tb, :], lg[:, tb, :], mx[:, tb, 0:1], None, ALU.subtract, None, [('lg', tb), ('mx', tb)], [('ex', tb)])
                        for (t, tb) in tiles:
                            b.act(ex[:, tb, :], ex[:, tb, :], AF.Exp, [('ex', tb)], [('ex', tb)])
                        for (t, tb) in tiles:
                            b.tt('dve', ex[:, tb, :], ex[:, tb, :], msk[:, tb, :], ALU.mult, [('ex', tb), ('msk', tb)], [('ex', tb)])
                        for (t, tb) in tiles:
                            S.op('dve', lambda e, tb=tb: e.reduce_sum(den[:, tb, 0:1], ex[:, tb, :], axis=mybir.AxisListType.X), [('ex', tb)], [('den', tb)])
                        for (t, tb) in tiles:
                            b.recip('dve', den[:, tb, 1:2], den[:, tb, 0:1], [('den', tb)], [('den2', tb)])
                        for (t, tb) in tiles:
                            b.ts('dve', cmb[:, tb, :], ex[:, tb, :], den[:, tb, 1:2], None, ALU.mult, None, [('ex', tb), ('den2', tb)], [('cmb', tb)])
                        for (t, tb) in tiles:
                            S.op('pe', lambda e, tb=tb: e.transpose(PS[2 + tb][0:8, 0:128], cmb[:, tb, :], idf[:]), [('cmb', tb), 'idf'], [pk(2 + tb)])
                        for (t, tb) in tiles:
                            b.copy('act', combT[:, t * 128:(t + 1) * 128], PS[2 + tb][0:8, 0:128], [pk(2 + tb)], ['combT'])
                    S.flush()
                p7b = ExitStack()
                if stop_after != 'p7a':
                    w1s = sb("w1m", [128, 3, KC, 256], BF16, p7b)
                    w3s = sb("w3m", [128, 3, KC, 256], BF16, p7b)
                    w2s = sb("w2m", [128, 3, 2, 1024], BF16, p7b)
                    s1b = sb("s1m", [128, 2, 512], F32, p7b)
                    t32 = sb("t32m", [128, 2, 512], F32, p7b)
                    gT = sb("gTm", [128, 2, 2, 512], BF16, p7b)

                    def cb_prepare(e):
                        for tg in range(4):
                            pb = 4 + tg
                            b.mm([(PS[pb][:, :512], sel_s[:, e, :], combT[:, tg * 512:(tg + 1) * 512], True, True)], ['sel', 'combT'], [pk(pb)])
                            b.copy('act', cbt[:, tg * 512:(tg + 1) * 512], PS[pb][:, :512], [pk(pb)], ['cbt'])
                    experts = [(moe_w1[e], moe_w3[e], moe_w2[e], DFE, e) for e in range(NEXP)]
                    swiglu_stages(nc, S, b, PS, pk, own_groups, own_gidx, ubuf, hT, experts, (w1s, w3s, w2s, s1b, gT, t32),
                                  lambda oc, s: pvs(('gaf', 1), oc, 0), NF=2, cb=(cb_prepare, cbt), NWB=3)
                    S.flush()
                p7b.close()
                p7c = ExitStack()
                if stop_after not in ('p7a', 'p7b'):
                    lntmp = ln_tmps(p7c, "b7")
                    for ti, (c0, N, s) in enumerate(own_groups):
                        gi = own_gidx[ti]
                        ln_norm(lambda kc, c0=c0, N=N: hT[:, kc, c0:c0 + N], N, 0, [('hT', gi, kc) for kc in range(KC)],
                                g1=lambda kc: lng_s[:, 3, kc:kc + 1], b1=lambda kc: lnb_s[:, 3, kc:kc + 1],
                                out1_fn=lambda kc, c0=c0, N=N: hT[:, kc, c0:c0 + N], out1_keys=[('hT', gi, kc) for kc in range(KC)],
                                tmp=lntmp)
                    S.flush()
                p7c.close()


        toks = []
        if debug:
            toks.append(S.dma('sp', dbg[:], hT[:], [('hT', gi, kc) for gi in range(5) for kc in range(KC)], []))
        toks.append(S.dma('sp', yout[:], hT[:, :, NCTX:NQ], [('hT', gi, kc) for gi in range(5) for kc in range(KC)], []))
        S.flush(final_tokens=toks)
    return nc


def swiglu_stages(nc, S, b, PS, pk, tgroups, gidx, ubuf, hT, experts, bufs, gate_fn, NF=4, cb=None, NWB=2):
    w1s, w3s, w2s, s1b, gT, t32 = bufs
    stages = []
    for (w1, w3, w2, F, e) in experts:
        nfc = F // 128
        f0 = 0
        first = True
        while f0 < nfc:
            nf = min(NF, nfc - f0)
            stages.append((w1, w3, w2, f0, nf, e, first))
            first = False
            f0 += nf

    def load(si):
        (w1, w3, w2, f0, nf, e, first) = stages[si]
        q = si % NWB
        c0f = f0 * 128
        S.dma('pool', w1s[:, q, :, :nf * 128], w1[:, c0f:c0f + nf * 128].rearrange("(kc p) n -> p kc n", p=128), [], [('w1s', q)])
        S.dma('pool', w3s[:, q, :, :nf * 128], w3[:, c0f:c0f + nf * 128].rearrange("(kc p) n -> p kc n", p=128), [], [('w3s', q)])
        S.dma('pool', w2s[:, q, :nf, :], w2[c0f:c0f + nf * 128, :].rearrange("(fc p) n -> p fc n", p=128), [], [('w2s', q)])

    items = [(si, ti) for si in range(len(stages)) for ti in range(len(tgroups))]
    hc = [0]

    def H(i):
        si, ti = items[i]
        (w1, w3, w2, f0, nf, e, first) = stages[si]
        q = si % NWB
        if ti == 0:
            if si == 0:
                for k in range(min(NWB - 1, len(stages))):
                    load(k)
            if cb is not None and first:
                cb[0](e)
        (c0, N, s) = tgroups[ti]
        gi = gidx[ti]
        gq_ = i % 2
        ukeys = [('ubuf', gi, kc) for kc in range(KC)]
        for fc in range(nf):
            p1 = (hc[0] % 2) * 2
            sq_ = hc[0] % 2
            hc[0] += 1
            b.mm([(PS[p1][:, :N], w1s[:, q, kc, fc * 128:(fc + 1) * 128], ubuf[:, kc, c0:c0 + N], kc == 0, kc == KC - 1) for kc in range(KC)],
                 [('w1s', q)] + ukeys, [pk(p1)])
            b.mm([(PS[p1 + 1][:, :N], w3s[:, q, kc, fc * 128:(fc + 1) * 128], ubuf[:, kc, c0:c0 + N], kc == 0, kc == KC - 1) for kc in range(KC)],
                 [('w3s', q)] + ukeys, [pk(p1 + 1)])
            b.act(s1b[:, sq_, :N], PS[p1][:, :N], AF.Silu, [pk(p1)], [('s1b', sq_)])
            if cb is None:
                b.tt('dve', gT[:, gq_, fc, :N], PS[p1 + 1][:, :N], s1b[:, sq_, :N], ALU.mult, [pk(p1 + 1), ('s1b', sq_)], [('gT', gq_, fc)])
            else:
                b.tt('dve', t32[:, sq_, :N], PS[p1 + 1][:, :N], s1b[:, sq_, :N], ALU.mult, [pk(p1 + 1), ('s1b', sq_)], [('t32', sq_)])
                b.tt('pool', gT[:, gq_, fc, :N], t32[:, sq_, :N], cb[1][:, ti * 512:ti * 512 + N], ALU.mult,
                     [('t32', sq_), 'cbt'], [('gT', gq_, fc)])

    def W2(i):
        si, ti = items[i]
        (w1, w3, w2, f0, nf, e, first) = stages[si]
        q = si % NWB
        (c0, N, s) = tgroups[ti]
        gi = gidx[ti]
        gq_ = i % 2
        if ti == 0 and si + NWB - 1 < len(stages):
            load(si + NWB - 1)
        for oc in range(KC):
            pb = 4 + (oc % 4)
            b.mm([(PS[pb][:, :N], w2s[:, q, fc, oc * 128:(oc + 1) * 128], gT[:, gq_, fc, :N], fc == 0, fc == nf - 1) for fc in range(nf)],
                 [('w2s', q)] + [('gT', gq_, fc) for fc in range(nf)], [pk(pb)])
            b.stt('dve', hT[:, oc, c0:c0 + N], PS[pb][:, :N], gate_fn(oc, s), hT[:, oc, c0:c0 + N], ALU.mult, ALU.add,
                  [pk(pb), 'pv', ('hT', gi, oc)], [('hT', gi, oc)])

    H(0)
    for i in range(len(items)):
        if i + 1 < len(items):
            H(i + 1)
        W2(i)


def _fm(a):
    n, d = a.shape
    return np.ascontiguousarray(a.reshape(n, d // 128, 128).transpose(2, 1, 0))


def _vec(a):
    return np.ascontiguousarray(a.reshape(-1, 128).T)


def _rope_tables(tok_idx):
    pairs = 16
    freqs = (10000.0 ** (-np.arange(pairs, dtype=np.float32) / pairs)).astype(np.float32)
    row = (tok_idx // 64).astype(np.float32)
    col = (tok_idx % 64).astype(np.float32)
    ang = np.stack([row[:, None] * freqs, col[:, None] * freqs], axis=1).astype(np.float32)
    c = np.cos(ang).astype(np.float32)
    s = np.sin(ang).astype(np.float32)
    n = len(tok_idx)
    cos_t = np.zeros((64, n), np.float32)
    sin_t = np.zeros((64, n), np.float32)
    for ax in range(2):
        for half in range(2):
            d0 = ax * 32 + half * 16
            cos_t[d0:d0 + 16] = c[:, ax, :].T
            sin_t[d0:d0 + 16] = (-s[:, ax, :].T) if half == 0 else s[:, ax, :].T
    return cos_t, sin_t


_SWAP = np.array([ax * 32 + (1 - half) * 16 + p for ax in range(2) for half in range(2) for p in range(16)])


def make_in_maps(inp):
    f = lambda k: np.asarray(inp[k], dtype=np.float32)
    x, c, ctx, c_ctx = f("x"), f("c"), f("ctx"), f("c_ctx")
    shared = {}
    shared["ada_w"] = np.ascontiguousarray(f("ada_w"))
    ab = f("ada_b")
    shared["adab"] = np.ascontiguousarray(ab.reshape(2, 48, 128).transpose(2, 0, 1))
    shared["lng"] = np.ascontiguousarray(f("ln_g").reshape(4, KC, 128).transpose(2, 0, 1))
    shared["lnb"] = np.ascontiguousarray(f("ln_b").reshape(4, KC, 128).transpose(2, 0, 1))
    wd = f("mla_w_down")[0]
    wd_ext = np.concatenate([wd, wd[:, 640 + _SWAP]], axis=1)
    shared["wdown"] = np.ascontiguousarray(wd_ext.reshape(KC, 128, 768).transpose(1, 0, 2))
    shared["gq"] = _vec(f("mla_g_q")[0])
    shared["gkv"] = _vec(f("mla_g_kv")[0])
    wq = f("mla_w_uq")[0]
    wq_ext = np.concatenate([wq, wq[:, :, 128 + _SWAP]], axis=2)
    shared["wuq"] = np.ascontiguousarray(wq_ext.reshape(3, 128, 8 * 256).transpose(1, 0, 2))
    shared["wuk"] = np.ascontiguousarray(f("mla_w_uk")[0].reshape(2, 128, 1024).transpose(1, 0, 2))
    shared["wuv"] = np.ascontiguousarray(f("mla_w_uv")[0].reshape(2, 128, 1024).transpose(1, 0, 2))
    shared["wo"] = np.ascontiguousarray(f("mla_w_o")[0].reshape(8, 128, 1024).transpose(1, 0, 2))
    shared["ffn_w1"] = np.ascontiguousarray(f("ffn_w1")[0])
    shared["ffn_w3"] = np.ascontiguousarray(f("ffn_w3")[0])
    shared["ffn_w2"] = np.ascontiguousarray(f("ffn_w2")[0])
    shared["glu_a"] = np.ascontiguousarray(f("s5_w_glu_a")[0])
    shared["glu_b"] = np.ascontiguousarray(f("s5_w_glu_b")[0])
    shared["s5d"] = _vec(f("s5_d")[0])
    shared["ident"] = np.eye(128, dtype=np.float32)
    shared["identf"] = np.eye(128, dtype=np.float32)
    shared["wrt"] = np.ascontiguousarray(f("moe_w_router")[0].reshape(KC, 128, NEXP).transpose(1, 0, 2))
    selm = np.zeros((8, 8, 128), np.float32)
    for e in range(8):
        selm[e, e, :] = 1.0
    shared["sel"] = selm
    shared["moe_w1"] = np.ascontiguousarray(f("moe_w1")[0])
    shared["moe_w3"] = np.ascontiguousarray(f("moe_w3")[0])
    shared["moe_w2"] = np.ascontiguousarray(f("moe_w2")[0])
    a_re, a_im, lstep = f("s5_a_re")[0], f("s5_a_im")[0], f("s5_log_step")[0]
    b_re, b_im, c_re, c_im = f("s5_b_re")[0], f("s5_b_im")[0], f("s5_c_re")[0], f("s5_c_im")[0]

    def gp(a2):
        return a2.reshape(32, 2, 64).transpose(1, 2, 0).reshape(128, 32)

    def gpb(b4):
        return b4.reshape(32, 2, 64, 16).transpose(1, 2, 0, 3).reshape(128, 32, 16)

    def gpc(c4):
        return c4.reshape(32, 2, 16, 64).transpose(1, 3, 0, 2).reshape(128, 32, 16)
    s5_dir = []
    for d in range(2):
        ls = np.broadcast_to(lstep[d].reshape(32, 2).T[:, None, :], (2, 64, 32)).reshape(128, 32)
        s5_dir.append(dict(a=np.stack([gp(a_re[d]), gp(a_im[d])], 0), ls=ls,
                           b=np.stack([gpb(b_re[d]), gpb(b_im[d])], 0), c=np.stack([gpc(c_re[d]), gpc(c_im[d])], 0)))
    in_maps = []
    orders = []
    for k in range(8):
        bi, half = k // 2, k % 2
        if half == 0:
            own = np.arange(0, 2048)
            partner = np.arange(2048, 4096)
            cidx = np.arange(0, 256)
        else:
            own = np.arange(4095, 2047, -1)
            partner = np.arange(2047, -1, -1)
            cidx = np.arange(255, -1, -1)
        X = np.concatenate([ctx[bi][cidx], x[bi][own], x[bi][partner]], axis=0)
        m = dict(shared)
        m["xall"] = _fm(X)
        ck, sk = _rope_tables(np.concatenate([own, partner]))
        cos_t = np.concatenate([np.ones((64, 256), np.float32), ck], axis=1)
        sin_t = np.concatenate([np.zeros((64, 256), np.float32), sk], axis=1)
        m["cosk"] = np.ascontiguousarray(cos_t)
        m["sink"] = np.ascontiguousarray(sin_t)
        m["cvec"] = np.ascontiguousarray(np.stack([_vec(c[bi]), _vec(c_ctx)], axis=2))
        dd = [s5_dir[half], s5_dir[1 - half]]
        m["s5a"] = np.ascontiguousarray(np.stack([dd[0]["a"], dd[1]["a"]], 0).transpose(2, 0, 1, 3))
        m["s5ls"] = np.ascontiguousarray(np.stack([dd[0]["ls"], dd[1]["ls"]], 0).transpose(1, 0, 2))
        m["s5b"] = np.ascontiguousarray(np.stack([dd[0]["b"], dd[1]["b"]], 0).transpose(2, 0, 1, 3, 4))
        m["s5c"] = np.ascontiguousarray(np.stack([dd[0]["c"], dd[1]["c"]], 0).transpose(2, 0, 1, 3, 4))
        pm = np.zeros((128, 2), np.float32)
        pm[:, half] = 1.0
        m["pmask"] = pm
        in_maps.append(m)
        orders.append(own)
    return in_maps, orders


_NC_CACHE = {}


def kernel(**inputs):
    in_maps, orders = make_in_maps(inputs)
    if "nc" not in _NC_CACHE:
        _NC_CACHE["nc"] = build_program()
    nc = _NC_CACHE["nc"]
    res = run_bass_kernel_spmd(nc, in_maps, core_ids=list(range(8)))
    out = np.zeros((4, 4096, D), np.float32)
    for k in range(8):
        y = np.asarray(res.results[k]["yout"])
        out[k // 2, orders[k], :] = y.transpose(2, 1, 0).reshape(NOWN, D)
    return out
```
